# Optimizing a Trainium2 kernel written in Bass

```python
import math
import jax, jax.numpy as jnp
from jax import lax
import numpy as np

D_MODEL = 2048
BATCH = 1
SEQ = 16384
DEPTH = 1

S5_WIDTH = D_MODEL // 4
S5_GROUP = 16
S5_GROUPS = S5_WIDTH // S5_GROUP
S5_STATE = 64
LRU_WIDTH = 3 * D_MODEL // 4
LRU_HEADS = 12
LRU_HEAD_DIM = LRU_WIDTH // LRU_HEADS
CONV_WIDTH = 4
LRU_C = 8.0
IN_COLS = S5_WIDTH + 2 * LRU_WIDTH + 2 * D_MODEL
MOE_GROUPS = 4
EXPERTS_PER_GROUP = 8
N_EXPERTS = MOE_GROUPS * EXPERTS_PER_GROUP
TOP_K = 2
EXPERT_FF = D_MODEL // 8
NORM_EPS = 1e-6

kernel_name = 'hybrid_s5_rglru_hmoe_block'


def rms_norm(x, w):
    x32 = x.astype(jnp.float32)
    y = x32 * lax.rsqrt(jnp.mean(x32 * x32, axis=-1, keepdims=True) + NORM_EPS)
    return (y * w.astype(jnp.float32)).astype(x.dtype)


def _linear_combine(left, right):
    a_l, b_l = left
    a_r, b_r = right
    return a_r * a_l, a_r * b_l + b_r


def s5_branch(u, lam_re, lam_im, log_step, b_re, b_im, c_re, c_im, d_skip, w_glu, b_glu):
    bsz, seqlen, _ = u.shape
    f32 = jnp.float32
    u32 = u.astype(f32).reshape(bsz, seqlen, S5_GROUPS, S5_GROUP)
    lam = lax.complex(lam_re.astype(f32), lam_im.astype(f32))
    step = jnp.exp(log_step.astype(f32))[:, None]
    lam_bar = jnp.exp(lam * step)
    b_c = lax.complex(b_re.astype(f32), b_im.astype(f32))
    b_bar = ((lam_bar - 1.0) / lam)[..., None] * b_c
    bu = jnp.einsum('gpc,blgc->blgp', b_bar, u32.astype(jnp.complex64))
    a = jnp.broadcast_to(lam_bar, bu.shape)
    _, states = lax.associative_scan(_linear_combine, (a, bu), axis=1)
    c_c = lax.complex(c_re.astype(f32), c_im.astype(f32))
    y = jnp.real(jnp.einsum('gcp,blgp->blgc', c_c, states))
    y = y + d_skip.astype(f32).reshape(S5_GROUPS, S5_GROUP) * u32
    y = jax.nn.gelu(y.reshape(bsz, seqlen, S5_WIDTH))
    glu = jax.nn.sigmoid(y @ w_glu.astype(f32) + b_glu.astype(f32))
    return y * glu


def block_diag_linear(x, w, b):
    bsz, seqlen, _ = x.shape
    xh = x.reshape(bsz, seqlen, LRU_HEADS, LRU_HEAD_DIM)
    y = jnp.einsum('blhi,hij->blhj', xh, w.astype(x.dtype))
    return y.reshape(bsz, seqlen, LRU_WIDTH) + b.astype(x.dtype)


def rglru_branch(xb, yb, conv_w, conv_b, w_a, b_a, w_x, b_x, lam):
    f32 = jnp.float32
    seqlen = xb.shape[1]
    x32 = xb.astype(f32)
    xp = jnp.pad(x32, ((0, 0), (CONV_WIDTH - 1, 0), (0, 0)))
    xc = conv_b.astype(f32)
    for k in range(CONV_WIDTH):
        xc = xc + xp[:, k:k + seqlen] * conv_w[k].astype(f32)
    r = jax.nn.sigmoid(block_diag_linear(xc, w_a.astype(f32), b_a.astype(f32)))
    i = jax.nn.sigmoid(block_diag_linear(xc, w_x.astype(f32), b_x.astype(f32)))
    log_a = -LRU_C * r * jax.nn.softplus(-lam.astype(f32))
    a = jnp.exp(log_a)
    mult = jnp.sqrt(-jnp.expm1(2.0 * log_a))
    pos = jnp.arange(seqlen)[None, :, None]
    mult = jnp.where(pos == 0, 1.0, mult)
    _, h = lax.associative_scan(_linear_combine, (a, xc * i * mult), axis=1)
    return h * jax.nn.gelu(yb.astype(f32))


def mixer_sublayer(x, norm_w, w_in, b_gate, s5_lam_re, s5_lam_im, s5_log_step, s5_b_re, s5_b_im,
                   s5_c_re, s5_c_im, s5_d, s5_w_glu, s5_b_glu, lru_conv_w, lru_conv_b, lru_w_a,
                   lru_b_a, lru_w_x, lru_b_x, lru_lambda, w_proj_s5, w_proj_lru, w_out):
    h = rms_norm(x, norm_w)
    proj = h @ w_in
    o1 = S5_WIDTH
    o2 = o1 + LRU_WIDTH
    o3 = o2 + LRU_WIDTH
    u_s5 = proj[..., :o1]
    x_lru = proj[..., o1:o2]
    y_lru = proj[..., o2:o3]
    gates = jax.nn.sigmoid((proj[..., o3:] + b_gate).astype(jnp.float32))
    g_s5 = gates[..., :D_MODEL]
    g_lru = gates[..., D_MODEL:]
    out_s5 = s5_branch(u_s5, s5_lam_re, s5_lam_im, s5_log_step, s5_b_re, s5_b_im,
                       s5_c_re, s5_c_im, s5_d, s5_w_glu, s5_b_glu)
    out_lru = rglru_branch(x_lru, y_lru, lru_conv_w, lru_conv_b, lru_w_a, lru_b_a,
                           lru_w_x, lru_b_x, lru_lambda)
    br_s5 = out_s5.astype(x.dtype) @ w_proj_s5
    br_lru = out_lru.astype(x.dtype) @ w_proj_lru
    merged = (g_s5 * br_s5.astype(jnp.float32) + g_lru * br_lru.astype(jnp.float32)).astype(x.dtype)
    return x + merged @ w_out


def moe_sublayer(x, norm_w, w_rg, b_rg, w_re, b_re, w_e_gate, w_e_up, w_e_down):
    bsz, seqlen, d = x.shape
    f32 = jnp.float32
    ht = rms_norm(x, norm_w).reshape(bsz * seqlen, d)
    h32 = ht.astype(f32)
    g_prob = jax.nn.softmax(h32 @ w_rg.astype(f32) + b_rg.astype(f32), axis=-1)
    g_top, g_idx = lax.top_k(g_prob, 1)
    e_logits = (h32 @ w_re.astype(f32) + b_re.astype(f32)).reshape(-1, MOE_GROUPS, EXPERTS_PER_GROUP)
    e_sel = jnp.take_along_axis(e_logits, g_idx[:, :, None], axis=1)[:, 0]
    e_prob = jax.nn.softmax(e_sel, axis=-1)
    e_top, e_idx = lax.top_k(e_prob, TOP_K)
    e_top = e_top / jnp.sum(e_top, axis=-1, keepdims=True)
    weights = g_top * e_top
    expert_id = g_idx * EXPERTS_PER_GROUP + e_idx
    combine = jnp.sum(jax.nn.one_hot(expert_id, N_EXPERTS, dtype=f32) * weights[..., None], axis=1)
    gate = jnp.einsum('td,edf->tef', ht, w_e_gate)
    up = jnp.einsum('td,edf->tef', ht, w_e_up)
    act = jax.nn.silu(gate) * up * combine[:, :, None].astype(ht.dtype)
    out = jnp.einsum('tef,efd->td', act, w_e_down)
    return x + out.reshape(bsz, seqlen, d)


def setup_inputs(seed: int = 0) -> dict:
    key = jax.random.key(seed)
    ks = jax.random.split(key, 40)
    f32 = jnp.float32

    def nrm(k, shape, scale):
        return jax.random.normal(k, shape, f32) * scale

    G, P, GS = S5_GROUPS, S5_STATE, S5_GROUP
    x = nrm(ks[0], (BATCH, SEQ, D_MODEL), 1.0)
    norm_mix_w = 1.0 + nrm(ks[1], (DEPTH, D_MODEL), 0.02)
    w_in = nrm(ks[2], (DEPTH, D_MODEL, IN_COLS), D_MODEL ** -0.5)
    b_gate = nrm(ks[3], (DEPTH, 2 * D_MODEL), 0.02)
    s5_lam_re = -0.5 + nrm(ks[4], (DEPTH, G, P), 0.01)
    s5_lam_im = math.pi * jnp.arange(P, dtype=f32) + nrm(ks[5], (DEPTH, G, P), 0.01)
    s5_log_step = jax.random.uniform(ks[6], (DEPTH, G), f32, math.log(1e-3), math.log(1e-1))
    s5_b_re = nrm(ks[7], (DEPTH, G, P, GS), (2.0 * GS) ** -0.5)
    s5_b_im = nrm(ks[8], (DEPTH, G, P, GS), (2.0 * GS) ** -0.5)
    s5_c_re = nrm(ks[9], (DEPTH, G, GS, P), (2.0 * P) ** -0.5)
    s5_c_im = nrm(ks[10], (DEPTH, G, GS, P), (2.0 * P) ** -0.5)
    s5_d = nrm(ks[11], (DEPTH, S5_WIDTH), 1.0)
    s5_w_glu = nrm(ks[12], (DEPTH, S5_WIDTH, S5_WIDTH), S5_WIDTH ** -0.5)
    s5_b_glu = nrm(ks[13], (DEPTH, S5_WIDTH), 0.02)
    lru_conv_w = nrm(ks[14], (DEPTH, CONV_WIDTH, LRU_WIDTH), CONV_WIDTH ** -0.5)
    lru_conv_b = nrm(ks[15], (DEPTH, LRU_WIDTH), 0.02)
    lru_w_a = nrm(ks[16], (DEPTH, LRU_HEADS, LRU_HEAD_DIM, LRU_HEAD_DIM), LRU_HEAD_DIM ** -0.5)
    lru_b_a = nrm(ks[17], (DEPTH, LRU_WIDTH), 0.02)
    lru_w_x = nrm(ks[18], (DEPTH, LRU_HEADS, LRU_HEAD_DIM, LRU_HEAD_DIM), LRU_HEAD_DIM ** -0.5)
    lru_b_x = nrm(ks[19], (DEPTH, LRU_WIDTH), 0.02)
    a0 = jax.random.uniform(ks[20], (DEPTH, LRU_WIDTH), f32, 0.9, 0.999)
    s = a0 ** (1.0 / LRU_C)
    lru_lambda = jnp.log(s) - jnp.log1p(-s)
    w_proj_s5 = nrm(ks[21], (DEPTH, S5_WIDTH, D_MODEL), S5_WIDTH ** -0.5)
    w_proj_lru = nrm(ks[22], (DEPTH, LRU_WIDTH, D_MODEL), LRU_WIDTH ** -0.5)
    w_out = nrm(ks[23], (DEPTH, D_MODEL, D_MODEL), D_MODEL ** -0.5)
    norm_ffn_w = 1.0 + nrm(ks[24], (DEPTH, D_MODEL), 0.02)
    w_router_group = nrm(ks[25], (DEPTH, D_MODEL, MOE_GROUPS), D_MODEL ** -0.5)
    b_router_group = nrm(ks[26], (DEPTH, MOE_GROUPS), 0.01)
    w_router_expert = nrm(ks[27], (DEPTH, D_MODEL, N_EXPERTS), D_MODEL ** -0.5)
    b_router_expert = nrm(ks[28], (DEPTH, N_EXPERTS), 0.01)
    w_e_gate = nrm(ks[29], (DEPTH, N_EXPERTS, D_MODEL, EXPERT_FF), D_MODEL ** -0.5)
    w_e_up = nrm(ks[30], (DEPTH, N_EXPERTS, D_MODEL, EXPERT_FF), D_MODEL ** -0.5)
    w_e_down = nrm(ks[31], (DEPTH, N_EXPERTS, EXPERT_FF, D_MODEL), EXPERT_FF ** -0.5)
    norm_final_w = 1.0 + nrm(ks[32], (D_MODEL,), 0.02)
    return {'x': x, 'norm_mix_w': norm_mix_w, 'w_in': w_in, 'b_gate': b_gate,
            's5_lam_re': s5_lam_re, 's5_lam_im': s5_lam_im, 's5_log_step': s5_log_step,
            's5_b_re': s5_b_re, 's5_b_im': s5_b_im, 's5_c_re': s5_c_re, 's5_c_im': s5_c_im,
            's5_d': s5_d, 's5_w_glu': s5_w_glu, 's5_b_glu': s5_b_glu,
            'lru_conv_w': lru_conv_w, 'lru_conv_b': lru_conv_b, 'lru_w_a': lru_w_a, 'lru_b_a': lru_b_a,
            'lru_w_x': lru_w_x, 'lru_b_x': lru_b_x, 'lru_lambda': lru_lambda,
            'w_proj_s5': w_proj_s5, 'w_proj_lru': w_proj_lru, 'w_out': w_out,
            'norm_ffn_w': norm_ffn_w, 'w_router_group': w_router_group, 'b_router_group': b_router_group,
            'w_router_expert': w_router_expert, 'b_router_expert': b_router_expert,
            'w_e_gate': w_e_gate, 'w_e_up': w_e_up, 'w_e_down': w_e_down, 'norm_final_w': norm_final_w}


def reference(x, norm_mix_w, w_in, b_gate, s5_lam_re, s5_lam_im, s5_log_step, s5_b_re, s5_b_im,
              s5_c_re, s5_c_im, s5_d, s5_w_glu, s5_b_glu, lru_conv_w, lru_conv_b, lru_w_a, lru_b_a,
              lru_w_x, lru_b_x, lru_lambda, w_proj_s5, w_proj_lru, w_out, norm_ffn_w,
              w_router_group, b_router_group, w_router_expert, b_router_expert,
              w_e_gate, w_e_up, w_e_down, norm_final_w):
    for i in range(DEPTH):
        x = mixer_sublayer(x, norm_mix_w[i], w_in[i], b_gate[i], s5_lam_re[i], s5_lam_im[i],
                           s5_log_step[i], s5_b_re[i], s5_b_im[i], s5_c_re[i], s5_c_im[i], s5_d[i],
                           s5_w_glu[i], s5_b_glu[i], lru_conv_w[i], lru_conv_b[i], lru_w_a[i],
                           lru_b_a[i], lru_w_x[i], lru_b_x[i], lru_lambda[i], w_proj_s5[i],
                           w_proj_lru[i], w_out[i])
        x = moe_sublayer(x, norm_ffn_w[i], w_router_group[i], b_router_group[i],
                         w_router_expert[i], b_router_expert[i], w_e_gate[i], w_e_up[i], w_e_down[i])
    return rms_norm(x, norm_final_w)
```

```python
import math
from contextlib import ExitStack
import numpy as np
import ml_dtypes
import concourse.bass as bass
import concourse.mybir as mybir
from concourse.bass_utils import run_bass_kernel_spmd

F32 = mybir.dt.float32
BF16 = mybir.dt.bfloat16
AF = mybir.ActivationFunctionType
ALU = mybir.AluOpType
AX = mybir.AxisListType

D = 2048
NCORE = 8
NT = 2048
TB = 512
NB = NT // TB
L = 128
NPAY = 56
EPS = 1e-6
ARENA_WORDS = 53000


class Buf:
    __slots__ = ("name", "ap", "lw", "rd", "sem", "cnt", "root")

    def __init__(self, name, ap, root=None):
        self.name = name
        self.ap = ap
        self.root = root if root is not None else self
        self.lw = None
        self.rd = []
        self.sem = None
        self.cnt = 0


class Op:
    __slots__ = ("eng", "fn", "deps", "is_dma", "prim", "signal", "tok", "idx")


class Rec:
    EPOCH = 6000

    def __init__(self):
        self.ops = []
        self.since_barrier = []

    def _add(self, eng, fn, R, W, is_dma=False, prim=None):
        o = Op()
        o.eng, o.fn, o.is_dma, o.prim, o.signal, o.tok = eng, fn, is_dma, prim, False, None
        o.idx = len(self.ops)
        R = [b.root for b in R]
        W = [b.root for b in W]
        if prim is not None:
            o.prim = prim.root
        deps = set()
        for b in R:
            if b.lw is not None:
                deps.add(b.lw)
        for b in W:
            if b.lw is not None:
                deps.add(b.lw)
            deps.update(b.rd)
        o.deps = deps
        for b in R:
            b.rd.append(o.idx)
        for b in W:
            b.lw = o.idx
            b.rd = []
        self.ops.append(o)
        self.since_barrier.append(o.idx)
        return o

    def op(self, eng, fn, R=(), W=()):
        return self._add(eng, fn, list(R), list(W))

    def dma(self, queue, out_ap, in_ap, R, W, prim, slow=False):
        def fn(e, out_ap=out_ap, in_ap=in_ap, slow=slow):
            if slow:
                return e.dma_start(out=out_ap, in_=in_ap, allow_slow_non_contiguous=True)
            return e.dma_start(out=out_ap, in_=in_ap)
        return self._add(queue, fn, list(R), list(W), True, prim)

    def barrier(self):
        prev = list(self.since_barrier)
        self.since_barrier = []
        for eng in ("sp", "pool", "act", "dve", "pe"):
            o = Op()
            o.eng, o.fn, o.is_dma, o.prim, o.signal, o.tok = eng, None, False, None, False, None
            o.idx = len(self.ops)
            o.deps = set(prev)
            self.ops.append(o)

    def emit(self, nc, es):
        ops = self.ops
        for o in ops:
            for d in o.deps:
                ops[d].signal = True
        eng_cnt = {}
        sems = {}

        def get_sem(key):
            if key not in sems:
                sems[key] = es.enter_context(nc.semaphore("s%d" % len(sems)))
            return sems[key]

        for o in ops:
            if o.fn is None:
                continue
            if o.is_dma:
                b = o.prim
                b.cnt += 16
                o.tok = (("dma", id(b)), b.cnt)
            elif o.signal:
                c = eng_cnt.get(o.eng, 0) + 1
                eng_cnt[o.eng] = c
                o.tok = ((o.eng, (c - 1) // self.EPOCH), (c - 1) % self.EPOCH + 1)
        for o in ops:
            if o.tok is not None:
                get_sem(o.tok[0])
        by_eng = {}
        for o in ops:
            by_eng.setdefault(o.eng, []).append(o)

        def run(engobj, lst):
            waited = {}
            for o in lst:
                need = {}
                for d in o.deps:
                    p = ops[d]
                    if p.tok is None:
                        continue
                    if (not p.is_dma) and p.eng == o.eng and o.eng in ("pe",) and not o.is_dma:
                        continue
                    k, v = p.tok
                    if need.get(k, 0) < v:
                        need[k] = v
                for k, v in need.items():
                    if waited.get(k, 0) >= v:
                        continue
                    engobj.wait_ge(sems[k], v)
                    waited[k] = v
                if o.fn is None:
                    continue
                ins = o.fn(engobj)
                if o.tok is not None:
                    ins.then_inc(sems[o.tok[0]], 16 if o.is_dma else 1)

        with nc.Block() as block:
            @block.sync
            def _(e):
                run(e, by_eng.get("sp", []))

            @block.gpsimd
            def _(e):
                run(e, by_eng.get("pool", []))

            @block.scalar
            def _(e):
                run(e, by_eng.get("act", []))

            @block.vector
            def _(e):
                run(e, by_eng.get("dve", []))

            @block.tensor
            def _(e):
                run(e, by_eng.get("pe", []))


class Arena:
    def __init__(self, ap):
        self.ap = ap
        self.off = 0
        self.hi = 0

    def f32(self, n):
        a = self.ap[:, self.off:self.off + n]
        self.off += n
        self.hi = max(self.hi, self.off)
        assert self.off <= ARENA_WORDS, ("arena overflow", self.off)
        return a

    def bf16(self, n):
        w = (n + 1) // 2
        return self.f32(w).bitcast(BF16)


NPB = 28


def build(mode="fused"):
    full = True
    nc = bass.Bass("TRN2", target_bir_lowering=False)
    R = Rec()

    def din(name, shape, dt=F32):
        return nc.dram_tensor(name, list(shape), dt, kind="ExternalInput").ap()

    x = din("x", [NT, D])
    xp = din("xp", [NPB * TB, D])
    flagv = din("flagv", [128, NPB + 1])
    keepv = din("keepv", [128, NPB])
    revc = din("revc", [128, 1])
    nmw = din("nmw", [D])
    w_in = din("w_in", [D, 7680])
    lam_re = din("lam_re", [32, 64])
    lam_im = din("lam_im", [32, 64])
    lstep = din("lstep", [32])
    b_re = din("b_re", [32, 64, 16])
    b_im = din("b_im", [32, 64, 16])
    conv_w = din("conv_w", [4, 1536])
    conv_b = din("conv_b", [1536])
    w_a = din("w_a", [12, 128, 128])
    b_a = din("b_a", [1536])
    w_x = din("w_x", [12, 128, 128])
    b_x = din("b_x", [1536])
    lru_lam = din("lru_lam", [1536])
    ident = din("ident", [128, 128], BF16)
    ramp = din("ramp", [128, L])
    if full:
        b_gate = din("b_gate", [4096])
        c_re = din("c_re", [32, 16, 64])
        c_im = din("c_im", [32, 16, 64])
        s5_d = din("s5_d", [512])
        w_glu = din("w_glu", [512, 512])
        b_glu = din("b_glu", [512])
        wps = din("wps", [512, D])
        wpl = din("wpl", [1536, D])
        w_out = din("w_out", [D, D])
        nfw = din("nfw", [D])
        wrg = din("wrg", [D, 4])
        brg = din("brg", [4])
        wre = din("wre", [D, 32])
        bre = din("bre", [32])
        weg = din("weg", [32, D, 256])
        weu = din("weu", [32, D, 256])
        wed = din("wed", [32, 256, D])
        nfin = din("nfin", [D])
        out = nc.dram_tensor("out", [NT, D], F32, kind="ExternalOutput").ap()
        x1s = nc.dram_tensor("x1s", [NT, D], F32, kind="Internal").ap()
        x1s_b = Buf("x1s", x1s)
    else:
        pay = nc.dram_tensor("pay", [128, NPAY], F32, kind="ExternalOutput").ap()

    es = ExitStack()
    with es:
        arena_t = es.enter_context(nc.sbuf_tensor("arena", [128, ARENA_WORDS], F32))
        A = Arena(arena_t[:, :])
        ps = []
        for i in range(8):
            pt = es.enter_context(nc.psum_tensor("ps%d" % i, [128, 512], F32))
            ps.append(Buf("ps%d" % i, pt[:, :]))
        ps_rr = [0]

        ps_pool = [list(range(8))]

        def next_ps():
            pool = ps_pool[0]
            b = ps[pool[ps_rr[0] % len(pool)]]
            ps_rr[0] += 1
            return b

        def new(name, ap):
            return Buf(name, ap)

        def act(out, in_, func, R_, W_, bias=None, scale=None, accum=None):
            kw = {}
            if bias is not None:
                kw["bias"] = bias
            if scale is not None:
                kw["scale"] = scale
            if accum is not None:
                kw["accum_out"] = accum
            R.op("act", lambda e: e.activation(out=out, in_=in_, func=func, **kw), R_, W_)

        def tt(out, a, b, op, R_, W_, eng="dve"):
            R.op(eng, lambda e: e.tensor_tensor(out=out, in0=a, in1=b, op=op), R_, W_)

        def ts(out, a, s1, s2, op0, op1, R_, W_, eng="dve"):
            if s2 is None:
                R.op(eng, lambda e: e.tensor_scalar(out=out, in0=a, scalar1=s1, scalar2=None, op0=op0), R_, W_)
            else:
                R.op(eng, lambda e: e.tensor_scalar(out=out, in0=a, scalar1=s1, scalar2=s2, op0=op0, op1=op1), R_, W_)

        def stt(out, a, s, b, op0, op1, R_, W_, eng="dve"):
            R.op(eng, lambda e: e.scalar_tensor_tensor(out=out, in0=a, scalar=s, in1=b, op0=op0, op1=op1), R_, W_)

        def cp(out, in_, R_, W_, eng="dve"):
            R.op(eng, lambda e: e.tensor_copy(out=out, in_=in_), R_, W_)

        def scan(out, d0, d1, init, R_, W_):
            R.op("dve", lambda e: e.tensor_tensor_scan(out=out, data0=d0, data1=d1, initial=init,
                                                      op0=ALU.mult, op1=ALU.add), R_, W_)

        def mm(out, lhsT, rhs, start, stop, R_, W_):
            R.op("pe", lambda e: e.matmul(out, lhsT=lhsT, rhs=rhs, start=start, stop=stop), R_, W_)

        def tr(out, in_, R_, W_):
            R.op("pe", lambda e: e.transpose(out=out, in_=in_, identity=ident_b.ap), R_, W_)

        def load(dst_buf, dst_ap, src_ap, q="sp", slow=False, R_=()):
            R.dma(q, dst_ap, src_ap, list(R_), [dst_buf], dst_buf, slow=slow)

        def col(v, nt):
            return v.rearrange("(j p) -> p j", p=128)

        ident_b = new("ident", A.bf16(128))
        load(ident_b, ident_b.ap, ident)
        ramp_b = new("ramp", A.f32(L))
        load(ramp_b, ramp_b.ap, ramp)
        nwb = new("nwb", A.f32(D))
        load(nwb, nwb.ap, nmw.partition_broadcast(128))
        flag_b = new("flag", A.f32(NPB + 1))
        load(flag_b, flag_b.ap, flagv)
        keep_b = new("keep", A.f32(NPB))
        load(keep_b, keep_b.ap, keepv)

        prm = new("prm", A.f32(12 * 8))
        pv = prm.ap
        cw = pv[:, 0:48].rearrange("p (k j) -> p k j", k=4)
        for k in range(4):
            load(prm, cw[:, k, :], col(conv_w[k], 12), slow=True)
        cb = pv[:, 48:60]
        ba = pv[:, 60:72]
        bx = pv[:, 72:84]
        lamt = pv[:, 84:96]
        load(prm, cb, col(conv_b, 12), slow=True)
        load(prm, ba, col(b_a, 12), slow=True)
        load(prm, bx, col(b_x, 12), slow=True)
        load(prm, lamt, col(lru_lam, 12), slow=True)
        cexp = new("cexp", A.f32(12))
        tmp12 = new("tmp12", A.f32(12))
        act(tmp12.ap, lamt, AF.Exp, [prm], [tmp12], scale=-1.0)
        act(tmp12.ap, tmp12.ap, AF.Ln, [tmp12], [tmp12], bias=1.0)
        ts(cexp.ap, tmp12.ap, -8.0, None, ALU.mult, None, [tmp12], [cexp])
        hprm = new("hprm", A.f32(36))
        hba = hprm.ap[:, 0:12]
        hbx = hprm.ap[:, 12:24]
        hce = hprm.ap[:, 24:36]
        ts(hba, ba, 0.5, None, ALU.mult, None, [prm], [hprm])
        ts(hbx, bx, 0.5, None, ALU.mult, None, [prm], [hprm])
        ts(hce, cexp.ap, 0.5, None, ALU.mult, None, [cexp], [hprm])

        Wa = new("Wa", A.bf16(12 * 128))
        Wx = new("Wx", A.bf16(12 * 128))
        Wa3 = Wa.ap.rearrange("p (h j) -> p h j", h=12)
        Wx3 = Wx.ap.rearrange("p (h j) -> p h j", h=12)
        load(Wa, Wa3, w_a.rearrange("h i j -> i h j"), q="pool")
        load(Wx, Wx3, w_x.rearrange("h i j -> i h j"), q="pool")

        s5p = new("s5p", A.f32(16 * 3))
        lre = s5p.ap[:, 0:16]
        lim = s5p.ap[:, 16:32]
        stp = s5p.ap[:, 32:48]
        load(s5p, lre, lam_re.rearrange("(i g) p -> (g p) i", g=2), slow=True)
        load(s5p, lim, lam_im.rearrange("(i g) p -> (g p) i", g=2), slow=True)
        lst2 = lstep.rearrange("(i g) -> g i", g=2)
        for g2 in range(2):
            load(s5p, stp[g2 * 64:(g2 + 1) * 64, :], lst2[g2].partition_broadcast(64), slow=True)
        s5q = new("s5q", A.f32(16 * 12))
        q_ = s5q.ap

        def qv(k):
            return q_[:, 16 * k:16 * (k + 1)]
        stepv, rho, th, cos1, sin1, cr, ci, t0, t1_, t2_, ArT, AiT = [qv(k) for k in range(12)]
        act(stepv, stp, AF.Exp, [s5p], [s5q])
        tt(th, lim, stepv, ALU.mult, [s5p, s5q], [s5q])
        tt(t0, lre, stepv, ALU.mult, [s5p, s5q], [s5q])
        act(rho, t0, AF.Exp, [s5q], [s5q])
        TWO_PI = 2.0 * math.pi
        scs = new("scs", A.f32(48))
        tsm = scs.ap[:, 0:16]
        tsm2 = scs.ap[:, 16:32]
        tsi = scs.ap[:, 32:48].bitcast(mybir.dt.int32)
        SC = [scs]
        for (dst_, ph) in ((sin1, 0.0), (cos1, 0.25)):
            ts(tsm, th, 1.0 / TWO_PI, None, ALU.mult, None, [s5q], SC)
            if ph != 0.0:
                ts(tsm, tsm, ph, None, ALU.add, None, SC, SC)
            cp(tsi, tsm, SC, SC)
            cp(tsm2, tsi, SC, SC)
            tt(tsm, tsm, tsm2, ALU.subtract, SC, SC)
            ts(tsm2, tsm, 0.5, None, ALU.is_gt, None, SC, SC)
            tt(tsm, tsm, tsm2, ALU.subtract, SC, SC)
            ts(tsm2, tsm, -0.5, None, ALU.is_lt, None, SC, SC)
            tt(tsm, tsm, tsm2, ALU.add, SC, SC)
            act(dst_, tsm, AF.Sin, SC, [s5q], scale=TWO_PI)
        brm = new("brm", A.f32(16 * 4))
        brm1, bim, den, tq = [brm.ap[:, 16 * k:16 * (k + 1)] for k in range(4)]
        tt(brm1, rho, cos1, ALU.mult, [s5q], [brm])
        ts(brm1, brm1, -1.0, None, ALU.add, None, [brm], [brm])
        tt(bim, rho, sin1, ALU.mult, [s5q], [brm])
        tt(den, lre, lre, ALU.mult, [s5p], [brm])
        tt(tq, lim, lim, ALU.mult, [s5p], [brm])
        tt(den, den, tq, ALU.add, [brm], [brm])
        R.op("dve", lambda e: e.reciprocal(out=den, in_=den), [brm], [brm])
        tt(cr, brm1, lre, ALU.mult, [brm, s5p], [s5q])
        tt(tq, bim, lim, ALU.mult, [brm, s5p], [brm])
        tt(cr, cr, tq, ALU.add, [s5q, brm], [s5q])
        tt(cr, cr, den, ALU.mult, [s5q, brm], [s5q])
        tt(ci, bim, lre, ALU.mult, [brm, s5p], [s5q])
        tt(tq, brm1, lim, ALU.mult, [brm, s5p], [brm])
        tt(ci, ci, tq, ALU.subtract, [s5q, brm], [s5q])
        tt(ci, ci, den, ALU.mult, [s5q, brm], [s5q])
        Bq = new("Bq", A.f32(16 * 16 * 2))
        Bqr = Bq.ap[:, 0:256].rearrange("p (i c) -> p i c", i=16)
        Bqi = Bq.ap[:, 256:512].rearrange("p (i c) -> p i c", i=16)
        for g2 in range(2):
            load(Bq, Bqr[g2 * 64:(g2 + 1) * 64], b_re.rearrange("(i g) p c -> g p i c", g=2)[g2], slow=True)
            load(Bq, Bqi[g2 * 64:(g2 + 1) * 64], b_im.rearrange("(i g) p c -> g p i c", g=2)[g2], slow=True)
        bb = new("bb", A.f32(16 * 16 * 2))
        bbr = bb.ap[:, 0:256].rearrange("p (i c) -> p i c", i=16)
        bbi = bb.ap[:, 256:512].rearrange("p (i c) -> p i c", i=16)
        tq16 = new("tq16", A.f32(16))
        for i in range(16):
            ts(tq16.ap, Bqi[:, i, :], ci[:, i:i + 1], None, ALU.mult, None, [Bq, s5q], [tq16])
            stt(bbr[:, i, :], Bqr[:, i, :], cr[:, i:i + 1], tq16.ap, ALU.mult, ALU.subtract, [Bq, s5q, tq16], [bb])
            ts(tq16.ap, Bqr[:, i, :], ci[:, i:i + 1], None, ALU.mult, None, [Bq, s5q], [tq16])
            stt(bbi[:, i, :], Bqi[:, i, :], cr[:, i:i + 1], tq16.ap, ALU.mult, ALU.add, [Bq, s5q, tq16], [bb])

        CqB = [[None, None], [None, None]]
        CqV = [[None, None], [None, None]]
        for ri, csrc in enumerate((c_re, c_im)):
            for g2 in range(2):
                cb_ = new("Cq%d%d" % (ri, g2), A.f32(256))
                cv_ = cb_.ap.rearrange("p (i c) -> p i c", i=16)
                CqB[ri][g2] = cb_
                CqV[ri][g2] = cv_
                for i in range(16):
                    load(cb_, cv_[g2 * 64:(g2 + 1) * 64, i, :], csrc[2 * i + g2].rearrange("c p -> p c"), slow=True)

        sst = new("sst", A.f32(32))
        hst = new("hst", A.f32(12))
        pst = new("pst", A.f32(12))
        hal = new("hal", A.f32(36))
        hal3 = hal.ap.rearrange("p (j c) -> p j c", j=12)
        R.op("dve", lambda e: e.memset(sst.ap, 0.0), [], [sst])
        R.op("dve", lambda e: e.memset(hst.ap, 0.0), [], [hst])

        R.op("dve", lambda e: e.memset(hal.ap, 0.0), [], [hal])

        def norm_tile(src_ap, nwbuf, ns):
            norm_a(src_ap, ns)
            norm_b(nwbuf, ns)

        def norm_a(src_ap, ns):
            xt, hb, ssb = ns.xt, ns.hb, ns.ssb
            load(xt, xt.ap, src_ap)
            act(hb.ap, xt.ap, AF.Square, [xt], [hb, ssb], accum=ssb.ap[:, 0:1])

        def norm_b(nwbuf, ns):
            xt, hb, ssb = ns.xt, ns.hb, ns.ssb
            ts(ssb.ap[:, 1:2], ssb.ap[:, 0:1], 1.0 / D, EPS, ALU.mult, ALU.add, [ssb], [ssb])
            act(ssb.ap[:, 1:2], ssb.ap[:, 1:2], AF.Sqrt, [ssb], [ssb])
            R.op("dve", lambda e, o=ssb.ap[:, 1:2]: e.reciprocal(out=o, in_=o), [ssb], [ssb])
            stt(hb.ap, xt.ap, ssb.ap[:, 1:2], nwbuf.ap, ALU.mult, ALU.mult, [xt, ssb, nwbuf], [hb])

        def transpose_rows(srcb, dstb, dst3, col0, ncol=128):
            for half in range(2):
                pb = next_ps()
                pv_ = pb.ap.bitcast(BF16).rearrange("p (k t) -> p k t", k=8)
                for kk in range(8):
                    k = half * 8 + kk
                    tr(pv_[:, kk, :], srcb.ap[:, k * 128:(k + 1) * 128], [srcb, ident_b], [pb])
                eng = "act" if half == 0 else "dve"
                if eng == "act":
                    R.op("act", lambda e, o=dst3[:, half * 8:(half + 1) * 8, col0:col0 + ncol], i_=pv_[:, :, 0:ncol]:
                         e.activation(out=o, in_=i_, func=AF.Copy), [pb], [dstb])
                else:
                    cp(dst3[:, half * 8:(half + 1) * 8, col0:col0 + ncol], pv_[:, :, 0:ncol], [pb], [dstb])

        def lru_tile(fcol, j, px, py, ws):
            lru_s1(j, px, ws)
            lru_s1b(ws)
            lru_s2(fcol, j, py, ws)

        def lru_s1b(ws):
            cp(ws.xcb.ap, ws.xc.ap, [ws.xc], [ws.xcb])

        def lru_s1(j, px, ws, ceng="dve"):
            xl, xc, xcb = ws.xl, ws.xc, ws.xcb
            R.op("act", lambda e, o=xl.ap[:, 3:3 + TB], i_=px.ap: e.activation(out=o, in_=i_, func=AF.Copy), [px], [xl])
            cp(xl.ap[:, 0:3], hal3[:, j, :], [hal], [xl])
            cp(hal3[:, j, :], xl.ap[:, TB:TB + 3], [xl], [hal])
            if ceng == "pool":
                ct_ = ws.ctmp
                ts(xc.ap, xl.ap[:, 3:3 + TB], cw[:, 3, j:j + 1], cb[:, j:j + 1], ALU.mult, ALU.add, [xl, prm], [xc], eng="pool")
                ts(ct_.ap, xl.ap[:, 2:2 + TB], cw[:, 2, j:j + 1], None, ALU.mult, None, [xl, prm], [ct_], eng="pool")
                tt(xc.ap, xc.ap, ct_.ap, ALU.add, [xc, ct_], [xc], eng="pool")
                for k in range(2):
                    stt(xc.ap, xl.ap[:, k:k + TB], cw[:, k, j:j + 1], xc.ap, ALU.mult, ALU.add, [xl, prm, xc], [xc])
            else:
                ts(xc.ap, xl.ap[:, 3:3 + TB], cw[:, 3, j:j + 1], cb[:, j:j + 1], ALU.mult, ALU.add, [xl, prm], [xc])
                for k in range(3):
                    stt(xc.ap, xl.ap[:, k:k + TB], cw[:, k, j:j + 1], xc.ap, ALU.mult, ALU.add, [xl, prm, xc], [xc])

        def lru_s2(fcol, j, py, ws):
            xl, xc, xcb, rg, ig, at, mt, bt, ht, gy, tiny = (ws.xl, ws.xc, ws.xcb, ws.rg, ws.ig, ws.at, ws.mt,
                                                              ws.bt, ws.ht, ws.gy, ws.tiny)
            pr_ = next_ps()
            pi_ = next_ps()
            mm(pr_.ap, Wa3[:, j, :], xcb.ap, True, True, [Wa, xcb], [pr_])
            mm(pi_.ap, Wx3[:, j, :], xcb.ap, True, True, [Wx, xcb], [pi_])
            act(rg.ap, pr_.ap, AF.Tanh, [pr_, hprm], [rg], bias=hba[:, j:j + 1], scale=0.5)
            act(ig.ap, pi_.ap, AF.Tanh, [pi_, hprm], [ig], bias=hbx[:, j:j + 1], scale=0.5)
            act(at.ap, rg.ap, AF.Exp, [rg, hprm], [at], bias=hce[:, j:j + 1], scale=hce[:, j:j + 1])
            act(mt.ap, rg.ap, AF.Exp, [rg, cexp], [mt], bias=cexp.ap[:, j:j + 1], scale=cexp.ap[:, j:j + 1])
            act(mt.ap, mt.ap, AF.Sqrt, [mt], [mt], bias=0.25, scale=-0.25)
            if fcol is not None:
                ts(tiny.ap[:, 2:3], mt.ap[:, 0:1], -1.0, 0.5, ALU.mult, ALU.add, [mt], [tiny])
                stt(mt.ap[:, 0:1], tiny.ap[:, 2:3], flag_b.ap[:, fcol:fcol + 1], mt.ap[:, 0:1], ALU.mult, ALU.add, [tiny, flag_b, mt], [mt])
            tt(bt.ap, xc.ap, mt.ap, ALU.mult, [xc, mt], [bt])
            stt(bt.ap, ig.ap, 1.0, bt.ap, ALU.add, ALU.mult, [ig, bt], [bt])
            scan(ht.ap, at.ap, bt.ap, hst.ap[:, j:j + 1], [at, bt, hst], [ht])
            cp(hst.ap[:, j:j + 1], ht.ap[:, TB - 1:TB], [ht], [hst])
            if py is not None:
                act(gy.ap, py.ap, AF.Gelu_apprx_tanh, [py], [gy])
                tt(ol3[:, j, :], ht.ap, gy.ap, ALU.mult, [ht, gy], [out_lru])

        mark = A.off

        class _WS:
            pass
        revb = new("revb", A.f32(1))
        load(revb, revb.ap, revc)
        lam128 = new("lam128", A.f32(32))
        lr128 = lam128.ap[:, 0:16]
        li128 = lam128.ap[:, 16:32]
        tt(lr128, rho, cos1, ALU.mult, [s5q], [lam128])
        tt(li128, rho, sin1, ALU.mult, [s5q], [lam128])
        for _ in range(7):
            tt(t1_, lr128, lr128, ALU.mult, [lam128], [s5q])
            tt(t2_, li128, li128, ALU.mult, [lam128], [s5q])
            tt(li128, lr128, li128, ALU.mult, [lam128], [lam128])
            ts(li128, li128, 2.0, None, ALU.mult, None, [lam128], [lam128])
            tt(lr128, t1_, t2_, ALU.subtract, [s5q], [lam128])
        bbp = new("bbp", A.f32(2 * 512))
        bbpr = bbp.ap[:, 0:512]
        bbpi = bbp.ap[:, 512:1024]
        bbpr3 = bbpr.rearrange("p (i c) -> p i c", i=16)
        bbpi3 = bbpi.rearrange("p (i c) -> p i c", i=16)
        R.op("dve", lambda e: e.memset(bbp.ap, 0.0), [], [bbp])
        cp(bbpr3[0:64, :, 0:16], bbr[0:64], [bb], [bbp])
        cp(bbpr3[64:128, :, 16:32], bbr[64:128], [bb], [bbp])
        cp(bbpi3[0:64, :, 0:16], bbi[0:64], [bb], [bbp])
        cp(bbpi3[64:128, :, 16:32], bbi[64:128], [bb], [bbp])
        WresB = []
        Wres_ap = A.bf16(16 * 2048)
        Wres3 = Wres_ap.rearrange("p (k n) -> p k n", k=16)
        for c4 in range(4):
            wb = new("Wres%d" % c4, Wres3[:, :, c4 * 512:(c4 + 1) * 512])
            R.dma("pool", wb.ap, w_in[:, c4 * 512:(c4 + 1) * 512].rearrange("(k p) n -> p k n", p=128), [], [wb], wb)
            WresB.append(wb)
        GtR = new("GtR", A.bf16(2048))
        GtI = new("GtI", A.bf16(2048))
        hTps = [new("hTp%d" % i, A.bf16(16 * TB)) for i in range(2)]
        hTp3s = [h_.ap.rearrange("p (k t) -> p k t", k=16) for h_ in hTps]
        sq1 = new("sq1", A.f32(TB))
        sq2 = new("sq2", A.f32(TB))
        nsPs = []
        for si in range(2):
            n_ = _WS()
            n_.xt = new("xtP%d" % si, A.f32(D))
            n_.hb = new("hbP%d" % si, A.bf16(D))
            n_.ssb = new("ssbP%d" % si, A.f32(2))
            nsPs.append(n_)
        utok = [new("utok%d" % i, A.bf16(512)) for i in range(2)]
        zt = new("zt", A.f32(16 * 6))
        zre, zim, za, zb, zc, zd = [zt.ap[:, 16 * k:16 * (k + 1)] for k in range(6)]
        wsP = []
        for si in range(2):
            w = _WS()
            w.xl = new("xlP%d" % si, A.f32(3 + TB))
            for nm in ("xc", "rg", "ig", "at", "mt", "bt", "ht"):
                setattr(w, nm, new(nm + "P%d" % si, A.f32(TB)))
            w.gy = None
            w.xcb = new("xcbP%d" % si, A.bf16(TB))
            w.ctmp = new("ctmpP%d" % si, A.f32(TB))
            w.tiny = new("tinyP%d" % si, A.f32(8))
            wsP.append(w)
        wA = wsP[0]
        wB = wsP[1]
        lim_f = lam_im.rearrange("g p -> (g p)")
        lre_f = lam_re.rearrange("g p -> (g p)")
        INV2PI = 1.0 / (2.0 * math.pi)
        st8 = new("st8", A.f32(32))
        load(st8, st8.ap, lstep.partition_broadcast(128))
        act(st8.ap, st8.ap, AF.Exp, [st8], [st8])
        for ch in range(4):
            ssl = slice(ch * 512, (ch + 1) * 512)
            load(wA.xc, wA.xc.ap, lim_f[ssl].partition_broadcast(128))
            load(wA.rg, wA.rg.ap, lre_f[ssl].partition_broadcast(128))
            cp(wA.ig.ap.rearrange("p (g q) -> p g q", g=8),
               st8.ap[:, ch * 8:(ch + 1) * 8].unsqueeze(2).to_broadcast([128, 8, 64]), [st8], [wA.ig])
            tt(wA.xc.ap, wA.xc.ap, wA.ig.ap, ALU.mult, [wA.xc, wA.ig], [wA.xc])
            tt(wA.rg.ap, wA.rg.ap, wA.ig.ap, ALU.mult, [wA.rg, wA.ig], [wA.rg])
            act(wA.at.ap, wA.rg.ap, AF.Exp, [wA.rg, revb], [wA.at], scale=revb.ap[:, 0:1])
            for (ph, dstb) in ((0.0, GtI), (0.25, GtR)):
                tn, tn2, tni = wB.xc, wB.rg, wB.ig
                ts(tn.ap, wA.xc.ap, revb.ap[:, 0:1], INV2PI, ALU.mult, ALU.mult, [wA.xc, revb], [tn])
                if ph != 0.0:
                    ts(tn.ap, tn.ap, ph, None, ALU.add, None, [tn], [tn])
                tni_ap = tni.ap.bitcast(mybir.dt.int32)
                cp(tni_ap, tn.ap, [tn], [tni])
                cp(tn2.ap, tni_ap, [tni], [tn2])
                tt(tn.ap, tn.ap, tn2.ap, ALU.subtract, [tn, tn2], [tn])
                ts(tn2.ap, tn.ap, 0.5, None, ALU.is_gt, None, [tn], [tn2])
                tt(tn.ap, tn.ap, tn2.ap, ALU.subtract, [tn, tn2], [tn])
                ts(tn2.ap, tn.ap, -0.5, None, ALU.is_lt, None, [tn], [tn2])
                tt(tn.ap, tn.ap, tn2.ap, ALU.add, [tn, tn2], [tn])
                act(wB.at.ap, tn.ap, AF.Sin, [tn], [wB.at], scale=2.0 * math.pi)
                tt(dstb.ap[:, ssl], wB.at.ap, wA.at.ap, ALU.mult, [wB.at, wA.at], [dstb])

        sst2p = sst.ap.rearrange("p (i c) -> p i c", c=2)
        F_re = sst2p[:, :, 0]
        F_im = sst2p[:, :, 1]
        ZT = [zt]
        ps_pool[0] = [0, 1, 2, 3, 4]

        def pnorm(pb_, it_):
            pnormA(pb_, it_)
            pnormT(pb_, it_)

        def pnormA(pb_, it_):
            n_ = nsPs[it_ % 2]
            norm_tile(xp[pb_ * TB + it_ * 128:pb_ * TB + (it_ + 1) * 128, :], nwb, n_)

        def pnormT(pb_, it_):
            n_ = nsPs[it_ % 2]
            transpose_rows(n_.hb, hTps[pb_ % 2], hTp3s[pb_ % 2], it_ * 128)

        def s5p_a1(hTp, hTp3, it):
            pu = ps[5]
            for k in range(16):
                mm(pu.ap, hTp3[:, k, it * 128:(it + 1) * 128], Wres3[:, k, 0:512], k == 0, k == 15, [hTp, WresB[0]], [pu])
            ub = utok[it % 2]
            R.op("act", lambda e, o=ub.ap, i_=pu.ap: e.activation(out=o, in_=i_, func=AF.Copy), [pu], [ub])

        def s5p_a2(it):
            ub = utok[it % 2]
            pvr = ps[6]
            pvi = ps[7]
            for i in range(16):
                mm(pvr.ap[:, 32 * i:32 * i + 32], GtR.ap[:, 128 * i:128 * i + 128], ub.ap[:, 32 * i:32 * i + 32], True, True, [GtR, ub], [pvr])
            for i in range(16):
                mm(pvi.ap[:, 32 * i:32 * i + 32], GtI.ap[:, 128 * i:128 * i + 128], ub.ap[:, 32 * i:32 * i + 32], True, True, [GtI, ub], [pvi])

        def s5p_b():
            pvr = ps[6]
            pvi = ps[7]
            q1, q2 = sq1, sq2
            tt(q1.ap, pvr.ap, bbpr, ALU.mult, [pvr, bbp], [q1])
            tt(q2.ap, pvi.ap, bbpi, ALU.mult, [pvi, bbp], [q2])
            tt(q1.ap, q1.ap, q2.ap, ALU.subtract, [q1, q2], [q1])
            R.op("dve", lambda e, o=zre, i_=q1.ap.rearrange("p (i c) -> p i c", i=16): e.tensor_reduce(out=o, in_=i_, axis=AX.X, op=ALU.add), [q1], ZT)
            tt(q1.ap, pvi.ap, bbpr, ALU.mult, [pvi, bbp], [q1])
            tt(q2.ap, pvr.ap, bbpi, ALU.mult, [pvr, bbp], [q2])
            tt(q1.ap, q1.ap, q2.ap, ALU.add, [q1, q2], [q1])
            R.op("dve", lambda e, o=zim, i_=q1.ap.rearrange("p (i c) -> p i c", i=16): e.tensor_reduce(out=o, in_=i_, axis=AX.X, op=ALU.add), [q1], ZT)
            tt(za, lr128, F_re, ALU.mult, [lam128, sst], ZT)
            tt(zb, li128, F_im, ALU.mult, [lam128, sst], ZT)
            tt(za, za, zb, ALU.subtract, ZT, ZT)
            tt(za, za, zre, ALU.add, ZT, ZT)
            tt(zc, lr128, F_im, ALU.mult, [lam128, sst], ZT)
            tt(zd, li128, F_re, ALU.mult, [lam128, sst], ZT)
            tt(zc, zc, zd, ALU.add, ZT, ZT)
            tt(zc, zc, zim, ALU.add, ZT, ZT)
            cp(F_re, za, ZT, [sst])
            cp(F_im, zc, ZT, [sst])

        for it in range(4):
            pnorm(0, it)
        for pb in range(NPB):
            fcol = pb if pb % 4 == 0 else None
            hTp, hTp3 = hTps[pb % 2], hTp3s[pb % 2]
            side = {0: [("a1", 0)], 1: [("a2", 0), ("nA", 0)], 2: [("b", 0), ("a1", 1), ("nT", 0)],
                    3: [("a2", 1)], 4: [("b", 1), ("a1", 2), ("nA", 1)], 5: [("a2", 2), ("nT", 1)],
                    6: [("b", 2), ("a1", 3)], 7: [("a2", 3), ("nA", 2)], 8: [("b", 3), ("nT", 2)],
                    9: [], 10: [("nA", 3)], 11: [("nT", 3)]}
            for j in range(12):
                px = next_ps()
                cs_ = slice(512 + 128 * j, 512 + 128 * (j + 1))
                for k in range(16):
                    mm(px.ap, Wres3[:, k, cs_], hTp3[:, k, :], k == 0, k == 15, [WresB[1 + j // 4], hTp], [px])
                lru_s1(j, px, wsP[j % 2], ceng="pool")
                lru_s1b(wsP[j % 2])
                if j > 0:
                    lru_s2(fcol, j - 1, None, wsP[(j - 1) % 2])
                for kind, idx in side[j]:
                    if kind == "a1":
                        s5p_a1(hTp, hTp3, idx)
                    elif kind == "a2":
                        s5p_a2(idx)
                    elif kind == "b":
                        s5p_b()
                    elif pb + 1 < NPB:
                        if kind == "nA":
                            pnormA(pb + 1, idx)
                        else:
                            pnormT(pb + 1, idx)
            lru_s2(fcol, 11, None, wsP[1])
            kc = keep_b.ap[:, pb:pb + 1]
            ts(sst.ap, sst.ap, kc, None, ALU.mult, None, [sst, keep_b], [sst])
            ts(hst.ap, hst.ap, kc, None, ALU.mult, None, [hst, keep_b], [hst])
            ts(hal.ap, hal.ap, kc, None, ALU.mult, None, [hal, keep_b], [hal])
        ps_pool[0] = list(range(8))

        R.barrier()
        print("prefix arena hi", A.hi)
        global _DBG_A
        _DBG_A = A
        A.off = mark
        A.hi = mark
        Ecos = new("Ecos", A.f32(16 * L))
        Esin = new("Esin", A.f32(16 * L))
        Ec3 = Ecos.ap.rearrange("p (i l) -> p i l", i=16)
        Es3 = Esin.ap.rearrange("p (i l) -> p i l", i=16)
        targ = new("targ", A.f32(L))
        targ2 = new("targ2", A.f32(L))
        targi = new("targi", A.f32(L).bitcast(mybir.dt.int32))
        for i in range(16):
            for (tab, tabb, ph) in ((Es3, Esin, 0.0), (Ec3, Ecos, 0.25)):
                ts(targ.ap, ramp_b.ap, th[:, i:i + 1], 1.0 / TWO_PI, ALU.mult, ALU.mult, [ramp_b, s5q], [targ])
                if ph != 0.0:
                    ts(targ.ap, targ.ap, ph, None, ALU.add, None, [targ], [targ])
                cp(targi.ap, targ.ap, [targ], [targi])
                cp(targ2.ap, targi.ap, [targi], [targ2])
                tt(targ.ap, targ.ap, targ2.ap, ALU.subtract, [targ, targ2], [targ])
                ts(targ2.ap, targ.ap, 0.5, None, ALU.is_gt, None, [targ], [targ2])
                tt(targ.ap, targ.ap, targ2.ap, ALU.subtract, [targ, targ2], [targ])
                ts(targ2.ap, targ.ap, -0.5, None, ALU.is_lt, None, [targ], [targ2])
                tt(targ.ap, targ.ap, targ2.ap, ALU.add, [targ, targ2], [targ])
                act(tab[:, i, :], targ.ap, AF.Sin, [targ], [tabb], scale=TWO_PI)
        kk = new("kk", A.f32(64))
        K1 = kk.ap[:, 0:32].rearrange("p (i c) -> p i c", c=2)
        K2 = kk.ap[:, 32:64].rearrange("p (i c) -> p i c", c=2)
        cp(K1[:, :, 0], Ec3[:, :, L - 1], [Ecos], [kk])
        ts(K1[:, :, 1], Es3[:, :, L - 1], -1.0, None, ALU.mult, None, [Esin], [kk])
        cp(K2[:, :, 0], Es3[:, :, L - 1], [Esin], [kk])
        cp(K2[:, :, 1], Ec3[:, :, L - 1], [Ecos], [kk])
        fj = new("fj", A.f32(4))
        BtR = new("BtR", A.bf16(16 * 128))
        BtI = new("BtI", A.bf16(16 * 128))
        BtR3 = BtR.ap.rearrange("p (i q) -> p i q", i=16)
        BtI3 = BtI.ap.rearrange("p (i q) -> p i q", i=16)
        Sst = new("Sst", A.bf16(128))
        for i in range(16):
            base = (i % 4) * 32
            for (src, dstb, dst3) in ((bbr, BtR, BtR3), (bbi, BtI, BtI3)):
                R.op("dve", lambda e: e.memset(Sst.ap, 0.0), [], [Sst])
                cp(Sst.ap[0:64, base:base + 16], src[0:64, i, :], [bb], [Sst])
                cp(Sst.ap[64:128, base + 16:base + 32], src[64:128, i, :], [bb], [Sst])
                pb = next_ps()
                pbv = pb.ap.bitcast(BF16)[:, 0:128]
                tr(pbv, Sst.ap, [Sst, ident_b], [pb])
                cp(dst3[:, i, :], pbv, [pb], [dstb])
        if full:
            bgt = new("bgt", A.f32(32 + 4 + 4))
            load(bgt, bgt.ap[:, 0:32], col(b_gate, 32), slow=True)
            s5dt = bgt.ap[:, 32:36]
            bglt = bgt.ap[:, 36:40]
            load(bgt, s5dt, col(s5_d, 4), slow=True)
            load(bgt, bglt, col(b_glu, 4), slow=True)
            Wglu = new("Wglu", A.bf16(4 * 512))
            Wglu3 = Wglu.ap.rearrange("p (k n) -> p k n", k=4)
            load(Wglu, Wglu3, w_glu.rearrange("(k p) n -> p k n", p=128), q="pool")
            CtR = new("CtR", A.bf16(16 * 128))
            CtI = new("CtI", A.bf16(16 * 128))
            CtR3 = CtR.ap.rearrange("p (i q) -> p i q", i=16)
            CtI3 = CtI.ap.rearrange("p (i q) -> p i q", i=16)
            R.op("dve", lambda e: e.memset(CtR.ap, 0.0), [], [CtR])
            R.op("dve", lambda e: e.memset(CtI.ap, 0.0), [], [CtI])
            for i in range(16):
                base = (i % 4) * 32
                for g2 in range(2):
                    cs_ = slice(base + 16 * g2, base + 16 * g2 + 16)
                    ps_ = slice(g2 * 64, (g2 + 1) * 64)
                    cp(CtR3[ps_, i, cs_], CqV[0][g2][ps_, i, :], [CqB[0][g2]], [CtR])
                    cp(CtI3[ps_, i, cs_], CqV[1][g2][ps_, i, :], [CqB[1][g2]], [CtI])

        hT = new("hT", A.bf16(16 * TB))
        hT3 = hT.ap.rearrange("p (k t) -> p k t", k=16)
        uT = new("uT", A.bf16(4 * TB))
        uT3 = uT.ap.rearrange("p (k t) -> p k t", k=4)
        NSLOT = 3
        ring = [new("ring%d" % s, A.bf16(16 * 512)) for s in range(NSLOT)]
        ring_h = []
        for s in range(NSLOT):
            r3 = ring[s].ap.rearrange("p (k n) -> p k n", k=16)
            ring_h.append(r3)
        rr = [0]

        def next_slot():
            s = rr[0] % NSLOT
            rr[0] += 1
            return ring[s], ring_h[s]

        def w_in_cols(c0, n):
            return w_in[:, c0:c0 + n].rearrange("(k p) n -> p k n", p=128)

        xt = new("xt", A.f32(D))
        hb = new("hb", A.bf16(D))
        junk = hb
        ssb = new("ssb", A.f32(2))
        wk0 = A.off
        xl = new("xl", A.f32(3 + TB))
        xc = new("xc", A.f32(TB))
        xcb = new("xcb", A.bf16(TB))
        rg = new("rg", A.f32(TB))
        ig = new("ig", A.f32(TB))
        at = new("at", A.f32(TB))
        mtbt = new("mtbt", A.f32(2 * TB))
        mt = Buf("mt", mtbt.ap[:, 0:TB], root=mtbt)
        bt = Buf("bt", mtbt.ap[:, TB:2 * TB], root=mtbt)
        w2v = mtbt.ap.rearrange("p (c t) -> p c t", c=2)
        ht = new("ht", A.f32(TB))
        gy = new("gy", A.f32(TB))
        A.off = wk0
        f1, f2, f3, f4, wre_b, wim_b = xc, rg, ig, at, mt, bt
        A.off = A.hi
        tiny = new("tiny", A.f32(8))
        if full:
            sre = new("sre", A.bf16(TB))
            sim = new("sim", A.bf16(TB))
            yv = ht
            ygT = new("ygT", A.bf16(4 * TB))
            ygT3 = ygT.ap.rearrange("p (k t) -> p k t", k=4)
            glt = gy
            out_s5 = new("out_s5", A.bf16(4 * TB))
            os53 = out_s5.ap.rearrange("p (k t) -> p k t", k=4)
            out_lru = new("out_lru", A.bf16(12 * TB))
            ol3 = out_lru.ap.rearrange("p (k t) -> p k t", k=12)
            mergedT = new("mergedT", A.bf16(16 * TB))
            mg3 = mergedT.ap.rearrange("p (k t) -> p k t", k=16)
            g1 = xc
            g2b = rg
            xr = [ig, at]

        class _NS:
            pass
        ns0 = _NS()
        ns0.xt, ns0.hb, ns0.ssb = xt, hb, ssb
        ws0 = _NS()
        (ws0.xl, ws0.xc, ws0.xcb, ws0.rg, ws0.ig, ws0.at, ws0.mt, ws0.bt, ws0.ht, ws0.gy, ws0.tiny) = (
            xl, xc, xcb, rg, ig, at, mt, bt, ht, gy, tiny)
        ws1 = _NS()
        (ws1.rg, ws1.ig, ws1.at, ws1.mt, ws1.bt, ws1.ht, ws1.gy, ws1.tiny) = (rg, ig, at, mt, bt, ht, gy, tiny)
        ws1.xl = new("xl1", A.f32(3 + TB))
        ws1.xc = new("xc1", A.f32(TB))
        ws1.xcb = new("xcb1", A.bf16(TB))
        wsO = [ws0, ws1]

        def s5_block(do_out):
            for i in range(16):
                pr = next_ps()
                pi = next_ps()
                mm(pr.ap, BtR3[:, i, :], uT3[:, i // 4, :], True, True, [BtR, uT], [pr])
                mm(pi.ap, BtI3[:, i, :], uT3[:, i // 4, :], True, True, [BtI, uT], [pi])
                ec = Ec3[:, i:i + 1, :].to_broadcast([128, TB // L, L])
                es_ = Es3[:, i:i + 1, :].to_broadcast([128, TB // L, L])

                def v4(ap):
                    return ap.rearrange("p (b l) -> p b l", l=L)
                tt(v4(f1.ap), v4(pr.ap), ec, ALU.mult, [pr, Ecos], [f1])
                tt(v4(f2.ap), v4(pi.ap), es_, ALU.mult, [pi, Esin], [f2])
                tt(f1.ap, f1.ap, f2.ap, ALU.add, [f1, f2], [f1])
                tt(v4(f3.ap), v4(pi.ap), ec, ALU.mult, [pi, Ecos], [f3])
                tt(v4(f4.ap), v4(pr.ap), es_, ALU.mult, [pr, Esin], [f4])
                tt(f3.ap, f3.ap, f4.ap, ALU.subtract, [f3, f4], [f3])
                s_re = sst.ap[:, 2 * i:2 * i + 1]
                s_im = sst.ap[:, 2 * i + 1:2 * i + 2]
                rb = rho[:, i:i + 1].to_broadcast([128, L])
                for b in range(TB // L):
                    sl_ = slice(b * L, (b + 1) * L)
                    scan(wre_b.ap[:, sl_], rb, f1.ap[:, sl_], s_re, [s5q, f1, sst], [wre_b])
                    scan(wim_b.ap[:, sl_], rb, f3.ap[:, sl_], s_im, [s5q, f3, sst], [wim_b])
                    last2 = w2v[:, :, (b + 1) * L - 1]
                    stt(fj.ap[:, 0:2], last2, 1.0, K1[:, i, :], ALU.mult, ALU.mult, [mtbt, kk], [fj, sst])
                    R.ops[-1].fn = (lambda e, o=fj.ap[:, 0:2], a_=last2, k_=K1[:, i, :], acc_=s_re:
                                    e.scalar_tensor_tensor(out=o, in0=a_, scalar=1.0, in1=k_, op0=ALU.mult, op1=ALU.mult, accum_out=acc_))
                    stt(fj.ap[:, 2:4], last2, 1.0, K2[:, i, :], ALU.mult, ALU.mult, [mtbt, kk], [fj, sst])
                    R.ops[-1].fn = (lambda e, o=fj.ap[:, 2:4], a_=last2, k_=K2[:, i, :], acc_=s_im:
                                    e.scalar_tensor_tensor(out=o, in0=a_, scalar=1.0, in1=k_, op0=ALU.mult, op1=ALU.mult, accum_out=acc_))
                if do_out:
                    tt(v4(f2.ap), v4(wre_b.ap), ec, ALU.mult, [wre_b, Ecos], [f2])
                    tt(v4(f4.ap), v4(wim_b.ap), es_, ALU.mult, [wim_b, Esin], [f4])
                    tt(sre.ap, f2.ap, f4.ap, ALU.subtract, [f2, f4], [sre])
                    tt(v4(f2.ap), v4(wre_b.ap), es_, ALU.mult, [wre_b, Esin], [f2])
                    tt(v4(f4.ap), v4(wim_b.ap), ec, ALU.mult, [wim_b, Ecos], [f4])
                    stt(sim.ap, f2.ap, -1.0, f4.ap, ALU.mult, ALU.subtract, [f2, f4], [sim])
                    jt = i // 4
                    if i % 4 == 0:
                        s5_block.yc = next_ps()
                    yc = s5_block.yc
                    mm(yc.ap, CtR3[:, i, :], sre.ap, i % 4 == 0, False, [CtR, sre], [yc])
                    mm(yc.ap, CtI3[:, i, :], sim.ap, False, i % 4 == 3, [CtI, sim], [yc])
                    if i % 4 == 3:
                        stt(yv.ap, uT3[:, jt, :], s5dt[:, jt:jt + 1], yc.ap, ALU.mult, ALU.add, [uT, bgt, yc], [yv])
                        act(ygT3[:, jt, :], yv.ap, AF.Gelu_apprx_tanh, [yv], [ygT])
            if do_out:
                for nt_ in range(4):
                    pg = next_ps()
                    for k in range(4):
                        mm(pg.ap, Wglu3[:, k, nt_ * 128:(nt_ + 1) * 128], ygT3[:, k, :], k == 0, k == 3, [Wglu, ygT], [pg])
                    act(glt.ap, pg.ap, AF.Sigmoid, [pg, bgt], [glt], bias=bglt[:, nt_:nt_ + 1])
                    tt(os53[:, nt_, :], ygT3[:, nt_, :], glt.ap, ALU.mult, [ygT, glt], [out_s5])

        def front(src, row0, do_out, fcol):
            for it in range(4):
                norm_tile(src[row0 + it * 128:row0 + (it + 1) * 128, :], nwb, ns0)
                transpose_rows(hb, hT, hT3, it * 128)
            sb, s3 = next_slot()
            load(sb, s3, w_in_cols(0, 512), q="pool")
            for ct in range(4):
                pu = next_ps()
                for k in range(16):
                    mm(pu.ap, s3[:, k, ct * 128:(ct + 1) * 128], hT3[:, k, :], k == 0, k == 15, [sb, hT], [pu])
                R.op("act", lambda e, o=uT3[:, ct, :], i_=pu.ap: e.activation(out=o, in_=i_, func=AF.Copy), [pu], [uT])
            s5_block(do_out)
            pend_l = None
            for q in range(6):
                sb, s3 = next_slot()
                R.dma("pool", s3[:, :, 0:256], w_in_cols(512 + 256 * q, 256), [], [sb], sb)
                if do_out:
                    R.dma("pool", s3[:, :, 256:512], w_in_cols(2048 + 256 * q, 256), [], [sb], sb)
                for jj in range(2):
                    j = 2 * q + jj
                    px = next_ps()
                    for k in range(16):
                        mm(px.ap, s3[:, k, jj * 128:(jj + 1) * 128], hT3[:, k, :], k == 0, k == 15, [sb, hT], [px])
                    py = None
                    if do_out:
                        py = next_ps()
                        for k in range(16):
                            mm(py.ap, s3[:, k, 256 + jj * 128:256 + (jj + 1) * 128], hT3[:, k, :], k == 0, k == 15, [sb, hT], [py])
                    lru_s1(j, px, wsO[j % 2])
                    lru_s1b(wsO[j % 2])
                    if pend_l is not None:
                        lru_s2(fcol, pend_l[0], pend_l[1], wsO[pend_l[0] % 2])
                    pend_l = (j, py)
            lru_s2(fcol, pend_l[0], pend_l[1], wsO[pend_l[0] % 2])

        for tb in range(NB):
            t0_ = tb * TB
            front(x, t0_, True, NPB if tb == 0 else None)
            for mg in range(4):
                gsb, gs3 = next_slot()
                load(gsb, gs3, w_in_cols(3584 + 512 * mg, 512), q="pool")
                glb, gl3 = next_slot()
                load(glb, gl3, w_in_cols(5632 + 512 * mg, 512), q="pool")
                prb, pr3 = next_slot()
                R.dma("pool", pr3[:, 0:4, :], wps[:, 512 * mg:512 * (mg + 1)].rearrange("(k p) n -> p k n", p=128), [], [prb], prb)
                R.dma("pool", pr3[:, 4:16, :], wpl[:, 512 * mg:512 * (mg + 1)].rearrange("(k p) n -> p k n", p=128), [], [prb], prb)
                for ct in range(4):
                    m = mg * 4 + ct
                    cs = slice(ct * 128, (ct + 1) * 128)
                    pC = next_ps()
                    for k in range(16):
                        mm(pC.ap, gs3[:, k, cs], hT3[:, k, :], k == 0, k == 15, [gsb, hT], [pC])
                    pD = next_ps()
                    for k in range(16):
                        mm(pD.ap, gl3[:, k, cs], hT3[:, k, :], k == 0, k == 15, [glb, hT], [pD])
                    pA = next_ps()
                    for k in range(4):
                        mm(pA.ap, pr3[:, k, cs], os53[:, k, :], k == 0, k == 3, [prb, out_s5], [pA])
                    pB = next_ps()
                    for k in range(12):
                        mm(pB.ap, pr3[:, 4 + k, cs], ol3[:, k, :], k == 0, k == 11, [prb, out_lru], [pB])
                    act(g1.ap, pC.ap, AF.Sigmoid, [pC, bgt], [g1], bias=bgt.ap[:, m:m + 1])
                    act(g2b.ap, pD.ap, AF.Sigmoid, [pD, bgt], [g2b], bias=bgt.ap[:, 16 + m:17 + m])
                    tt(g1.ap, g1.ap, pA.ap, ALU.mult, [g1, pA], [g1])
                    tt(g2b.ap, g2b.ap, pB.ap, ALU.mult, [g2b, pB], [g2b])
                    tt(mg3[:, m, :], g1.ap, g2b.ap, ALU.add, [g1, g2b], [mergedT])
            for n in range(4):
                wob, wo3 = next_slot()
                load(wob, wo3, w_out[:, 512 * n:512 * (n + 1)].rearrange("(k p) n -> p k n", p=128), q="pool")
                for it in range(4):
                    po = next_ps()
                    for k in range(16):
                        mm(po.ap, mg3[:, k, it * 128:(it + 1) * 128], wo3[:, k, :], k == 0, k == 15, [mergedT, wob], [po])
                    xb = xr[(n * 4 + it) % 2]
                    rows = slice(t0_ + it * 128, t0_ + (it + 1) * 128)
                    load(xb, xb.ap, x[rows, 512 * n:512 * (n + 1)])
                    tt(xb.ap, xb.ap, po.ap, ALU.add, [xb, po], [xb])
                    R.dma("sp", x1s[rows, 512 * n:512 * (n + 1)], xb.ap, [xb], [], xb)

        R.barrier()
        print("own-pass arena hi", A.hi)
        A.off = 0
        ident_b2 = new("ident2", A.bf16(128))
        load(ident_b2, ident_b2.ap, ident)
        ident_b = ident_b2
        nfwb = new("nfwb", A.f32(D))
        load(nfwb, nfwb.ap, nfw.partition_broadcast(128))
        nfinb = new("nfinb", A.f32(D))
        load(nfinb, nfinb.ap, nfin.partition_broadcast(128))
        wr = new("wr", A.bf16(16 * 36))
        wr3 = wr.ap.rearrange("p (k n) -> p k n", k=16)
        R.dma("pool", wr3[:, :, 0:4], wrg.rearrange("(k p) n -> p k n", p=128), [], [wr], wr, slow=True)
        R.dma("pool", wr3[:, :, 4:36], wre.rearrange("(k p) n -> p k n", p=128), [], [wr], wr, slow=True)
        brb = new("brb", A.f32(36))
        load(brb, brb.ap[:, 0:4], brg.partition_broadcast(128))
        load(brb, brb.ap[:, 4:36], bre.partition_broadcast(128))
        HT = 1024
        acc = [new("acc%d" % i, A.f32(D)) for i in range(8)]
        h2T = new("h2T", A.bf16(16 * HT))
        h2T3 = h2T.ap.rearrange("p (k t) -> p k t", k=16)
        comb = new("comb", A.f32(8 * 32))
        comb3 = comb.ap.rearrange("p (i e) -> p i e", i=8)
        Wgu = [new("Wgu%d" % s, A.bf16(16 * 512)) for s in range(2)]
        Wgu3 = [w.ap.rearrange("p (k n) -> p k n", k=16) for w in Wgu]
        Wd = [new("Wd%d" % s, A.bf16(2 * D)) for s in range(2)]
        Wd3 = [w.ap.rearrange("p (f n) -> p f n", f=2) for w in Wd]
        junk2 = new("junk2", A.bf16(D))
        hb2 = new("hb2", A.bf16(D))
        ss2 = new("ss2", A.f32(2))
        sg = [new("sg%d" % s, A.f32(256)) for s in range(2)]
        actb = [new("actb%d" % s, A.bf16(256)) for s in range(2)]
        actT = [new("actT%d" % s, A.bf16(256)) for s in range(2)]
        rt = new("rt", A.f32(36 + 8 * 6 + 16))
        lg = rt.ap[:, 0:36]
        oh = rt.ap[:, 36:40]
        ge = rt.ap[:, 40:44]
        esel = rt.ap[:, 44:52]
        m1k = rt.ap[:, 52:60]
        m2k = rt.ap[:, 60:68]
        es2 = rt.ap[:, 68:76]
        cm8 = rt.ap[:, 76:84]
        sc = rt.ap[:, 84:100]
        ot = new("ot", A.f32(D))

        for hp in range(NT // HT):
            for it in range(8):
                rows = slice(hp * HT + it * 128, hp * HT + (it + 1) * 128)
                R.dma("sp", acc[it].ap, x1s[rows, :], [], [acc[it]], acc[it])
                a_ = acc[it]
                act(junk2.ap, a_.ap, AF.Square, [a_], [junk2, ss2], accum=ss2.ap[:, 0:1])
                ts(ss2.ap[:, 1:2], ss2.ap[:, 0:1], 1.0 / D, EPS, ALU.mult, ALU.add, [ss2], [ss2])
                act(ss2.ap[:, 1:2], ss2.ap[:, 1:2], AF.Sqrt, [ss2], [ss2])
                R.op("dve", lambda e: e.reciprocal(out=ss2.ap[:, 1:2], in_=ss2.ap[:, 1:2]), [ss2], [ss2])
                stt(hb2.ap, a_.ap, ss2.ap[:, 1:2], nfwb.ap, ALU.mult, ALU.mult, [a_, ss2, nfwb], [hb2])
                transpose_rows(hb2, h2T, h2T3, it * 128)
                pr_ = next_ps()
                for k in range(16):
                    mm(pr_.ap[:, 0:36], h2T3[:, k, it * 128:(it + 1) * 128], wr3[:, k, :], k == 0, k == 15, [h2T, wr], [pr_])
                RT = [rt]
                tt(lg, pr_.ap[:, 0:36], brb.ap, ALU.add, [pr_, brb], RT)
                gm, ngm, gs_, gtop, m1, m2, dl, ee, w1, w2, c1, c2 = [sc[:, k:k + 1] for k in range(12)]
                R.op("dve", lambda e, o=gm, i_=lg[:, 0:4]: e.tensor_reduce(out=o, in_=i_, axis=AX.X, op=ALU.max), RT, RT)
                ts(ngm, gm, -1.0, None, ALU.mult, None, RT, RT)
                act(ge, lg[:, 0:4], AF.Exp, RT, RT, bias=ngm, accum=gs_)
                R.op("dve", lambda e, o=gtop, i_=gs_: e.reciprocal(out=o, in_=i_), RT, RT)
                ts(oh, lg[:, 0:4], gm, None, ALU.is_equal, None, RT, RT)
                ts(esel, lg[:, 4:12], oh[:, 0:1], None, ALU.mult, None, RT, RT)
                for g in range(1, 4):
                    stt(esel, lg[:, 4 + 8 * g:12 + 8 * g], oh[:, g:g + 1], esel, ALU.mult, ALU.add, RT, RT)
                R.op("dve", lambda e, o=m1, i_=esel: e.tensor_reduce(out=o, in_=i_, axis=AX.X, op=ALU.max), RT, RT)
                ts(m1k, esel, m1, None, ALU.is_equal, None, RT, RT)
                stt(es2, m1k, -1e30, esel, ALU.mult, ALU.add, RT, RT)
                R.op("dve", lambda e, o=m2, i_=es2: e.tensor_reduce(out=o, in_=i_, axis=AX.X, op=ALU.max), RT, RT)
                ts(m2k, es2, m2, None, ALU.is_equal, None, RT, RT)
                tt(dl, m2, m1, ALU.subtract, RT, RT)
                act(ee, dl, AF.Exp, RT, RT)
                ts(w1, ee, 1.0, None, ALU.add, None, RT, RT)
                R.op("dve", lambda e, o=w1, i_=w1: e.reciprocal(out=o, in_=i_), RT, RT)
                tt(w2, ee, w1, ALU.mult, RT, RT)
                tt(c1, w1, gtop, ALU.mult, RT, RT)
                tt(c2, w2, gtop, ALU.mult, RT, RT)
                ts(cm8, m1k, c1, None, ALU.mult, None, RT, RT)
                stt(cm8, m2k, c2, cm8, ALU.mult, ALU.add, RT, RT)
                for g in range(4):
                    ts(comb3[:, it, 8 * g:8 * g + 8], cm8, oh[:, g:g + 1], None, ALU.mult, None, RT, [comb])
            seq = [(e, it) for e in range(32) for it in range(8)]
            gu_ps = {}

            def load_expert(e):
                s = e % 2
                R.dma("pool", Wgu3[s][:, :, 0:256], weg[e].rearrange("(k p) n -> p k n", p=128), [], [Wgu[s]], Wgu[s])
                R.dma("pool", Wgu3[s][:, :, 256:512], weu[e].rearrange("(k p) n -> p k n", p=128), [], [Wgu[s]], Wgu[s])
                R.dma("pool", Wd3[s], wed[e].rearrange("(f p) n -> p f n", p=128), [], [Wd[s]], Wd[s])

            def emit_gu(idx):
                e, it = seq[idx]
                s = e % 2
                pg = ps[idx % 2]
                for k in range(16):
                    mm(pg.ap, h2T3[:, k, it * 128:(it + 1) * 128], Wgu3[s][:, k, :], k == 0, k == 15, [h2T, Wgu[s]], [pg])
                gu_ps[idx] = pg

            load_expert(0)
            emit_gu(0)
            for idx in range(len(seq)):
                e, it = seq[idx]
                if it == 0 and e + 1 < 32:
                    load_expert(e + 1)
                if idx + 1 < len(seq):
                    emit_gu(idx + 1)
                s = e % 2
                b2 = idx % 2
                pg = gu_ps.pop(idx)
                act(sg[b2].ap, pg.ap[:, 0:256], AF.Silu, [pg], [sg[b2]])
                stt(actb[b2].ap, sg[b2].ap, comb3[:, it, e:e + 1], pg.ap[:, 256:512], ALU.mult, ALU.mult,
                    [sg[b2], comb, pg], [actb[b2]])
                pt_ = ps[2 + b2]
                ptv = pt_.ap.bitcast(BF16)[:, 0:256].rearrange("p (f t) -> p f t", f=2)
                for f in range(2):
                    tr(ptv[:, f, :], actb[b2].ap[:, f * 128:(f + 1) * 128], [actb[b2], ident_b], [pt_])
                aT3 = actT[b2].ap.rearrange("p (f t) -> p f t", f=2)
                R.op("act", lambda e_, o=aT3, i_=ptv: e_.activation(out=o, in_=i_, func=AF.Copy), [pt_], [actT[b2]])
                for n in range(4):
                    pd = ps[4 + n]
                    for f in range(2):
                        mm(pd.ap, aT3[:, f, :], Wd3[s][:, f, n * 512:(n + 1) * 512], f == 0, f == 1, [actT[b2], Wd[s]], [pd])
                    tt(acc[it].ap[:, n * 512:(n + 1) * 512], acc[it].ap[:, n * 512:(n + 1) * 512], pd.ap, ALU.add,
                       [acc[it], pd], [acc[it]])
            for it in range(8):
                rows = slice(hp * HT + it * 128, hp * HT + (it + 1) * 128)
                a_ = acc[it]
                act(junk2.ap, a_.ap, AF.Square, [a_], [junk2, ss2], accum=ss2.ap[:, 0:1])
                ts(ss2.ap[:, 1:2], ss2.ap[:, 0:1], 1.0 / D, EPS, ALU.mult, ALU.add, [ss2], [ss2])
                act(ss2.ap[:, 1:2], ss2.ap[:, 1:2], AF.Sqrt, [ss2], [ss2])
                R.op("dve", lambda e: e.reciprocal(out=ss2.ap[:, 1:2], in_=ss2.ap[:, 1:2]), [ss2], [ss2])
                stt(ot.ap, a_.ap, ss2.ap[:, 1:2], nfinb.ap, ALU.mult, ALU.mult, [a_, ss2, nfinb], [ot])
                R.dma("sp", out[rows, :], ot.ap, [ot], [], ot)
        R.op("sp", None, [ot], [ot])
        R.emit(nc, es)
    return nc


_CACHE = {}


def _get(mode="fused"):
    if mode not in _CACHE:
        _CACHE[mode] = build(mode)
    return _CACHE[mode]


def kernel(**inp):
    f32 = np.float32
    g = {k: np.ascontiguousarray(np.asarray(v), dtype=f32) for k, v in inp.items()}
    x = g["x"].reshape(NCORE * NT, D)
    ident = np.eye(128, dtype=f32).astype(ml_dtypes.bfloat16)
    ramp = np.broadcast_to(np.arange(1, L + 1, dtype=f32)[None, :], (128, L)).copy()
    common = {
        "nmw": g["norm_mix_w"][0], "w_in": g["w_in"][0],
        "lam_re": g["s5_lam_re"][0], "lam_im": g["s5_lam_im"][0], "lstep": g["s5_log_step"][0],
        "b_re": g["s5_b_re"][0], "b_im": g["s5_b_im"][0],
        "conv_w": g["lru_conv_w"][0], "conv_b": g["lru_conv_b"][0],
        "w_a": g["lru_w_a"][0], "b_a": g["lru_b_a"][0], "w_x": g["lru_w_x"][0], "b_x": g["lru_b_x"][0],
        "lru_lam": g["lru_lambda"][0], "ident": ident, "ramp": ramp,
        "revc": (127.0 - np.arange(128, dtype=f32)).reshape(128, 1),
        "b_gate": g["b_gate"][0], "c_re": g["s5_c_re"][0], "c_im": g["s5_c_im"][0],
        "s5_d": g["s5_d"][0], "w_glu": g["s5_w_glu"][0], "b_glu": g["s5_b_glu"][0],
        "wps": g["w_proj_s5"][0], "wpl": g["w_proj_lru"][0], "w_out": g["w_out"][0],
        "nfw": g["norm_ffn_w"][0], "wrg": g["w_router_group"][0], "brg": g["b_router_group"][0],
        "wre": g["w_router_expert"][0], "bre": g["b_router_expert"][0],
        "weg": g["w_e_gate"][0], "weu": g["w_e_up"][0], "wed": g["w_e_down"][0], "nfin": g["norm_final_w"],
    }
    in_maps = []
    for c in range(NCORE):
        xp = np.zeros((NPB * TB, D), f32)
        if c > 0:
            xp[NPB * TB - c * NT:] = x[:c * NT]
        first_real = NPB - 4 * c
        keep = np.zeros((128, NPB), f32)
        keep[:, first_real:] = 1.0
        flag = np.zeros((128, NPB + 1), f32)
        flag[:, first_real] = 1.0
        in_maps.append(dict(common, x=x[c * NT:(c + 1) * NT], xp=xp, keepv=keep, flagv=flag))
    res = run_bass_kernel_spmd(_get(), in_maps, core_ids=list(range(NCORE)))
    outs = [np.asarray(r["out"], dtype=f32) for r in res.results]
    return np.concatenate(outs, axis=0).reshape(1, NCORE * NT, D)
```

```python
import math
from contextlib import ExitStack
import numpy as np
import ml_dtypes
import concourse.bass as bass
import concourse.mybir as mybir
from concourse.bass_utils import run_bass_kernel_spmd

F32 = mybir.dt.float32
BF16 = mybir.dt.bfloat16
AF = mybir.ActivationFunctionType
ALU = mybir.AluOpType
AX = mybir.AxisListType

D = 2048
NCORE = 8
NT = 2048
TB = 512
NB = NT // TB
L = 128
NPAY = 56
EPS = 1e-6
ARENA_WORDS = 53000


class Buf:
    __slots__ = ("name", "ap", "lw", "rd", "sem", "cnt", "root")

    def __init__(self, name, ap, root=None):
        self.name = name
        self.ap = ap
        self.root = root if root is not None else self
        self.lw = None
        self.rd = []
        self.sem = None
        self.cnt = 0


class Op:
    __slots__ = ("eng", "fn", "deps", "is_dma", "prim", "signal", "tok", "idx")


class Rec:
    EPOCH = 6000

    def __init__(self):
        self.ops = []
        self.since_barrier = []

    def _add(self, eng, fn, R, W, is_dma=False, prim=None):
        o = Op()
        o.eng, o.fn, o.is_dma, o.prim, o.signal, o.tok = eng, fn, is_dma, prim, False, None
        o.idx = len(self.ops)
        R = [b.root for b in R]
        W = [b.root for b in W]
        if prim is not None:
            o.prim = prim.root
        deps = set()
        for b in R:
            if b.lw is not None:
                deps.add(b.lw)
        for b in W:
            if b.lw is not None:
                deps.add(b.lw)
            deps.update(b.rd)
        o.deps = deps
        for b in R:
            b.rd.append(o.idx)
        for b in W:
            b.lw = o.idx
            b.rd = []
        self.ops.append(o)
        self.since_barrier.append(o.idx)
        return o

    def op(self, eng, fn, R=(), W=()):
        return self._add(eng, fn, list(R), list(W))

    def dma(self, queue, out_ap, in_ap, R, W, prim, slow=False):
        def fn(e, out_ap=out_ap, in_ap=in_ap, slow=slow):
            if slow:
                return e.dma_start(out=out_ap, in_=in_ap, allow_slow_non_contiguous=True)
            return e.dma_start(out=out_ap, in_=in_ap)
        return self._add(queue, fn, list(R), list(W), True, prim)

    def barrier(self):
        prev = list(self.since_barrier)
        self.since_barrier = []
        for eng in ("sp", "pool", "act", "dve", "pe"):
            o = Op()
            o.eng, o.fn, o.is_dma, o.prim, o.signal, o.tok = eng, None, False, None, False, None
            o.idx = len(self.ops)
            o.deps = set(prev)
            self.ops.append(o)

    def emit(self, nc, es):
        ops = self.ops
        for o in ops:
            for d in o.deps:
                ops[d].signal = True
        eng_cnt = {}
        sems = {}

        def get_sem(key):
            if key not in sems:
                sems[key] = es.enter_context(nc.semaphore("s%d" % len(sems)))
            return sems[key]

        for o in ops:
            if o.fn is None:
                continue
            if o.is_dma:
                b = o.prim
                b.cnt += 16
                o.tok = (("dma", id(b)), b.cnt)
            elif o.signal:
                c = eng_cnt.get(o.eng, 0) + 1
                eng_cnt[o.eng] = c
                o.tok = ((o.eng, (c - 1) // self.EPOCH), (c - 1) % self.EPOCH + 1)
        for o in ops:
            if o.tok is not None:
                get_sem(o.tok[0])
        by_eng = {}
        for o in ops:
            by_eng.setdefault(o.eng, []).append(o)

        def run(engobj, lst):
            waited = {}
            for o in lst:
                need = {}
                for d in o.deps:
                    p = ops[d]
                    if p.tok is None:
                        continue
                    if (not p.is_dma) and p.eng == o.eng and o.eng in ("pe",) and not o.is_dma:
                        continue
                    k, v = p.tok
                    if need.get(k, 0) < v:
                        need[k] = v
                for k, v in need.items():
                    if waited.get(k, 0) >= v:
                        continue
                    engobj.wait_ge(sems[k], v)
                    waited[k] = v
                if o.fn is None:
                    continue
                ins = o.fn(engobj)
                if o.tok is not None:
                    ins.then_inc(sems[o.tok[0]], 16 if o.is_dma else 1)

        with nc.Block() as block:
            @block.sync
            def _(e):
                run(e, by_eng.get("sp", []))

            @block.gpsimd
            def _(e):
                run(e, by_eng.get("pool", []))

            @block.scalar
            def _(e):
                run(e, by_eng.get("act", []))

            @block.vector
            def _(e):
                run(e, by_eng.get("dve", []))

            @block.tensor
            def _(e):
                run(e, by_eng.get("pe", []))


class Arena:
    def __init__(self, ap):
        self.ap = ap
        self.off = 0
        self.hi = 0

    def f32(self, n):
        a = self.ap[:, self.off:self.off + n]
        self.off += n
        self.hi = max(self.hi, self.off)
        assert self.off <= ARENA_WORDS, ("arena overflow", self.off)
        return a

    def bf16(self, n):
        w = (n + 1) // 2
        return self.f32(w).bitcast(BF16)


NPB = 28


def build(mode="fused"):
    full = True
    nc = bass.Bass("TRN2", target_bir_lowering=False)
    R = Rec()

    def din(name, shape, dt=F32):
        return nc.dram_tensor(name, list(shape), dt, kind="ExternalInput").ap()

    x = din("x", [NT, D])
    xp = din("xp", [NPB * TB, D])
    flagv = din("flagv", [128, NPB + 1])
    keepv = din("keepv", [128, NPB])
    revc = din("revc", [128, 1])
    nmw = din("nmw", [D])
    w_in = din("w_in", [D, 7680])
    lam_re = din("lam_re", [32, 64])
    lam_im = din("lam_im", [32, 64])
    lstep = din("lstep", [32])
    b_re = din("b_re", [32, 64, 16])
    b_im = din("b_im", [32, 64, 16])
    conv_w = din("conv_w", [4, 1536])
    conv_b = din("conv_b", [1536])
    w_a = din("w_a", [12, 128, 128])
    b_a = din("b_a", [1536])
    w_x = din("w_x", [12, 128, 128])
    b_x = din("b_x", [1536])
    lru_lam = din("lru_lam", [1536])
    ident = din("ident", [128, 128], BF16)
    ramp = din("ramp", [128, L])
    if full:
        b_gate = din("b_gate", [4096])
        c_re = din("c_re", [32, 16, 64])
        c_im = din("c_im", [32, 16, 64])
        s5_d = din("s5_d", [512])
        w_glu = din("w_glu", [512, 512])
        b_glu = din("b_glu", [512])
        wps = din("wps", [512, D])
        wpl = din("wpl", [1536, D])
        w_out = din("w_out", [D, D])
        nfw = din("nfw", [D])
        wrg = din("wrg", [D, 4])
        brg = din("brg", [4])
        wre = din("wre", [D, 32])
        bre = din("bre", [32])
        weg = din("weg", [32, D, 256])
        weu = din("weu", [32, D, 256])
        wed = din("wed", [32, 256, D])
        nfin = din("nfin", [D])
        out = nc.dram_tensor("out", [NT, D], F32, kind="ExternalOutput").ap()
        x1s = nc.dram_tensor("x1s", [NT, D], F32, kind="Internal").ap()
        x1s_b = Buf("x1s", x1s)
    else:
        pay = nc.dram_tensor("pay", [128, NPAY], F32, kind="ExternalOutput").ap()

    es = ExitStack()
    with es:
        arena_t = es.enter_context(nc.sbuf_tensor("arena", [128, ARENA_WORDS], F32))
        A = Arena(arena_t[:, :])
        ps = []
        for i in range(8):
            pt = es.enter_context(nc.psum_tensor("ps%d" % i, [128, 512], F32))
            ps.append(Buf("ps%d" % i, pt[:, :]))
        ps_rr = [0]

        ps_pool = [list(range(8))]

        def next_ps():
            pool = ps_pool[0]
            b = ps[pool[ps_rr[0] % len(pool)]]
            ps_rr[0] += 1
            return b

        def new(name, ap):
            return Buf(name, ap)

        def act(out, in_, func, R_, W_, bias=None, scale=None, accum=None):
            kw = {}
            if bias is not None:
                kw["bias"] = bias
            if scale is not None:
                kw["scale"] = scale
            if accum is not None:
                kw["accum_out"] = accum
            R.op("act", lambda e: e.activation(out=out, in_=in_, func=func, **kw), R_, W_)

        def tt(out, a, b, op, R_, W_, eng="dve"):
            R.op(eng, lambda e: e.tensor_tensor(out=out, in0=a, in1=b, op=op), R_, W_)

        def ts(out, a, s1, s2, op0, op1, R_, W_, eng="dve"):
            if s2 is None:
                R.op(eng, lambda e: e.tensor_scalar(out=out, in0=a, scalar1=s1, scalar2=None, op0=op0), R_, W_)
            else:
                R.op(eng, lambda e: e.tensor_scalar(out=out, in0=a, scalar1=s1, scalar2=s2, op0=op0, op1=op1), R_, W_)

        def stt(out, a, s, b, op0, op1, R_, W_, eng="dve"):
            R.op(eng, lambda e: e.scalar_tensor_tensor(out=out, in0=a, scalar=s, in1=b, op0=op0, op1=op1), R_, W_)

        def cp(out, in_, R_, W_, eng="dve"):
            R.op(eng, lambda e: e.tensor_copy(out=out, in_=in_), R_, W_)

        def scan(out, d0, d1, init, R_, W_):
            R.op("dve", lambda e: e.tensor_tensor_scan(out=out, data0=d0, data1=d1, initial=init,
                                                      op0=ALU.mult, op1=ALU.add), R_, W_)

        def mm(out, lhsT, rhs, start, stop, R_, W_):
            R.op("pe", lambda e: e.matmul(out, lhsT=lhsT, rhs=rhs, start=start, stop=stop), R_, W_)

        def tr(out, in_, R_, W_):
            R.op("pe", lambda e: e.transpose(out=out, in_=in_, identity=ident_b.ap), R_, W_)

        def load(dst_buf, dst_ap, src_ap, q="sp", slow=False, R_=()):
            R.dma(q, dst_ap, src_ap, list(R_), [dst_buf], dst_buf, slow=slow)

        def col(v, nt):
            return v.rearrange("(j p) -> p j", p=128)

        ident_b = new("ident", A.bf16(128))
        load(ident_b, ident_b.ap, ident)
        ramp_b = new("ramp", A.f32(L))
        load(ramp_b, ramp_b.ap, ramp)
        nwb = new("nwb", A.f32(D))
        load(nwb, nwb.ap, nmw.partition_broadcast(128))
        flag_b = new("flag", A.f32(NPB + 1))
        load(flag_b, flag_b.ap, flagv)
        keep_b = new("keep", A.f32(NPB))
        load(keep_b, keep_b.ap, keepv)

        prm = new("prm", A.f32(12 * 8))
        pv = prm.ap
        cw = pv[:, 0:48].rearrange("p (k j) -> p k j", k=4)
        for k in range(4):
            load(prm, cw[:, k, :], col(conv_w[k], 12), slow=True)
        cb = pv[:, 48:60]
        ba = pv[:, 60:72]
        bx = pv[:, 72:84]
        lamt = pv[:, 84:96]
        load(prm, cb, col(conv_b, 12), slow=True)
        load(prm, ba, col(b_a, 12), slow=True)
        load(prm, bx, col(b_x, 12), slow=True)
        load(prm, lamt, col(lru_lam, 12), slow=True)
        cexp = new("cexp", A.f32(12))
        tmp12 = new("tmp12", A.f32(12))
        act(tmp12.ap, lamt, AF.Exp, [prm], [tmp12], scale=-1.0)
        act(tmp12.ap, tmp12.ap, AF.Ln, [tmp12], [tmp12], bias=1.0)
        ts(cexp.ap, tmp12.ap, -8.0, None, ALU.mult, None, [tmp12], [cexp])
        hprm = new("hprm", A.f32(36))
        hba = hprm.ap[:, 0:12]
        hbx = hprm.ap[:, 12:24]
        hce = hprm.ap[:, 24:36]
        ts(hba, ba, 0.5, None, ALU.mult, None, [prm], [hprm])
        ts(hbx, bx, 0.5, None, ALU.mult, None, [prm], [hprm])
        ts(hce, cexp.ap, 0.5, None, ALU.mult, None, [cexp], [hprm])

        Wa = new("Wa", A.bf16(12 * 128))
        Wx = new("Wx", A.bf16(12 * 128))
        Wa3 = Wa.ap.rearrange("p (h j) -> p h j", h=12)
        Wx3 = Wx.ap.rearrange("p (h j) -> p h j", h=12)
        load(Wa, Wa3, w_a.rearrange("h i j -> i h j"), q="pool")
        load(Wx, Wx3, w_x.rearrange("h i j -> i h j"), q="pool")

        s5p = new("s5p", A.f32(16 * 3))
        lre = s5p.ap[:, 0:16]
        lim = s5p.ap[:, 16:32]
        stp = s5p.ap[:, 32:48]
        load(s5p, lre, lam_re.rearrange("(i g) p -> (g p) i", g=2), slow=True)
        load(s5p, lim, lam_im.rearrange("(i g) p -> (g p) i", g=2), slow=True)
        lst2 = lstep.rearrange("(i g) -> g i", g=2)
        for g2 in range(2):
            load(s5p, stp[g2 * 64:(g2 + 1) * 64, :], lst2[g2].partition_broadcast(64), slow=True)
        s5q = new("s5q", A.f32(16 * 12))
        q_ = s5q.ap

        def qv(k):
            return q_[:, 16 * k:16 * (k + 1)]
        stepv, rho, th, cos1, sin1, cr, ci, t0, t1_, t2_, ArT, AiT = [qv(k) for k in range(12)]
        act(stepv, stp, AF.Exp, [s5p], [s5q])
        tt(th, lim, stepv, ALU.mult, [s5p, s5q], [s5q])
        tt(t0, lre, stepv, ALU.mult, [s5p, s5q], [s5q])
        act(rho, t0, AF.Exp, [s5q], [s5q])
        TWO_PI = 2.0 * math.pi
        scs = new("scs", A.f32(48))
        tsm = scs.ap[:, 0:16]
        tsm2 = scs.ap[:, 16:32]
        tsi = scs.ap[:, 32:48].bitcast(mybir.dt.int32)
        SC = [scs]
        for (dst_, ph) in ((sin1, 0.0), (cos1, 0.25)):
            ts(tsm, th, 1.0 / TWO_PI, None, ALU.mult, None, [s5q], SC)
            if ph != 0.0:
                ts(tsm, tsm, ph, None, ALU.add, None, SC, SC)
            cp(tsi, tsm, SC, SC)
            cp(tsm2, tsi, SC, SC)
            tt(tsm, tsm, tsm2, ALU.subtract, SC, SC)
            ts(tsm2, tsm, 0.5, None, ALU.is_gt, None, SC, SC)
            tt(tsm, tsm, tsm2, ALU.subtract, SC, SC)
            ts(tsm2, tsm, -0.5, None, ALU.is_lt, None, SC, SC)
            tt(tsm, tsm, tsm2, ALU.add, SC, SC)
            act(dst_, tsm, AF.Sin, SC, [s5q], scale=TWO_PI)
        brm = new("brm", A.f32(16 * 4))
        brm1, bim, den, tq = [brm.ap[:, 16 * k:16 * (k + 1)] for k in range(4)]
        tt(brm1, rho, cos1, ALU.mult, [s5q], [brm])
        ts(brm1, brm1, -1.0, None, ALU.add, None, [brm], [brm])
        tt(bim, rho, sin1, ALU.mult, [s5q], [brm])
        tt(den, lre, lre, ALU.mult, [s5p], [brm])
        tt(tq, lim, lim, ALU.mult, [s5p], [brm])
        tt(den, den, tq, ALU.add, [brm], [brm])
        R.op("dve", lambda e: e.reciprocal(out=den, in_=den), [brm], [brm])
        tt(cr, brm1, lre, ALU.mult, [brm, s5p], [s5q])
        tt(tq, bim, lim, ALU.mult, [brm, s5p], [brm])
        tt(cr, cr, tq, ALU.add, [s5q, brm], [s5q])
        tt(cr, cr, den, ALU.mult, [s5q, brm], [s5q])
        tt(ci, bim, lre, ALU.mult, [brm, s5p], [s5q])
        tt(tq, brm1, lim, ALU.mult, [brm, s5p], [brm])
        tt(ci, ci, tq, ALU.subtract, [s5q, brm], [s5q])
        tt(ci, ci, den, ALU.mult, [s5q, brm], [s5q])
        Bq = new("Bq", A.f32(16 * 16 * 2))
        Bqr = Bq.ap[:, 0:256].rearrange("p (i c) -> p i c", i=16)
        Bqi = Bq.ap[:, 256:512].rearrange("p (i c) -> p i c", i=16)
        for g2 in range(2):
            load(Bq, Bqr[g2 * 64:(g2 + 1) * 64], b_re.rearrange("(i g) p c -> g p i c", g=2)[g2], slow=True)
            load(Bq, Bqi[g2 * 64:(g2 + 1) * 64], b_im.rearrange("(i g) p c -> g p i c", g=2)[g2], slow=True)
        bb = new("bb", A.f32(16 * 16 * 2))
        bbr = bb.ap[:, 0:256].rearrange("p (i c) -> p i c", i=16)
        bbi = bb.ap[:, 256:512].rearrange("p (i c) -> p i c", i=16)
        tq16 = new("tq16", A.f32(16))
        for i in range(16):
            ts(tq16.ap, Bqi[:, i, :], ci[:, i:i + 1], None, ALU.mult, None, [Bq, s5q], [tq16])
            stt(bbr[:, i, :], Bqr[:, i, :], cr[:, i:i + 1], tq16.ap, ALU.mult, ALU.subtract, [Bq, s5q, tq16], [bb])
            ts(tq16.ap, Bqr[:, i, :], ci[:, i:i + 1], None, ALU.mult, None, [Bq, s5q], [tq16])
            stt(bbi[:, i, :], Bqi[:, i, :], cr[:, i:i + 1], tq16.ap, ALU.mult, ALU.add, [Bq, s5q, tq16], [bb])

        CqB = [[None, None], [None, None]]
        CqV = [[None, None], [None, None]]
        for ri, csrc in enumerate((c_re, c_im)):
            for g2 in range(2):
                cb_ = new("Cq%d%d" % (ri, g2), A.f32(256))
                cv_ = cb_.ap.rearrange("p (i c) -> p i c", i=16)
                CqB[ri][g2] = cb_
                CqV[ri][g2] = cv_
                for i in range(16):
                    load(cb_, cv_[g2 * 64:(g2 + 1) * 64, i, :], csrc[2 * i + g2].rearrange("c p -> p c"), slow=True)

        sst = new("sst", A.f32(32))
        hst = new("hst", A.f32(12))
        pst = new("pst", A.f32(12))
        hal = new("hal", A.f32(36))
        hal3 = hal.ap.rearrange("p (j c) -> p j c", j=12)
        R.op("dve", lambda e: e.memset(sst.ap, 0.0), [], [sst])
        R.op("dve", lambda e: e.memset(hst.ap, 0.0), [], [hst])

        R.op("dve", lambda e: e.memset(hal.ap, 0.0), [], [hal])

        def norm_tile(src_ap, nwbuf, ns):
            norm_a(src_ap, ns)
            norm_b(nwbuf, ns)

        def norm_a(src_ap, ns):
            xt, hb, ssb = ns.xt, ns.hb, ns.ssb
            load(xt, xt.ap, src_ap)
            act(hb.ap, xt.ap, AF.Square, [xt], [hb, ssb], accum=ssb.ap[:, 0:1])

        def norm_b(nwbuf, ns):
            xt, hb, ssb = ns.xt, ns.hb, ns.ssb
            ts(ssb.ap[:, 1:2], ssb.ap[:, 0:1], 1.0 / D, EPS, ALU.mult, ALU.add, [ssb], [ssb])
            act(ssb.ap[:, 1:2], ssb.ap[:, 1:2], AF.Sqrt, [ssb], [ssb])
            R.op("dve", lambda e, o=ssb.ap[:, 1:2]: e.reciprocal(out=o, in_=o), [ssb], [ssb])
            stt(hb.ap, xt.ap, ssb.ap[:, 1:2], nwbuf.ap, ALU.mult, ALU.mult, [xt, ssb, nwbuf], [hb])

        def transpose_rows(srcb, dstb, dst3, col0, ncol=128):
            for half in range(2):
                pb = next_ps()
                pv_ = pb.ap.bitcast(BF16).rearrange("p (k t) -> p k t", k=8)
                for kk in range(8):
                    k = half * 8 + kk
                    tr(pv_[:, kk, :], srcb.ap[:, k * 128:(k + 1) * 128], [srcb, ident_b], [pb])
                eng = "act" if half == 0 else "dve"
                if eng == "act":
                    R.op("act", lambda e, o=dst3[:, half * 8:(half + 1) * 8, col0:col0 + ncol], i_=pv_[:, :, 0:ncol]:
                         e.activation(out=o, in_=i_, func=AF.Copy), [pb], [dstb])
                else:
                    cp(dst3[:, half * 8:(half + 1) * 8, col0:col0 + ncol], pv_[:, :, 0:ncol], [pb], [dstb])

        def lru_tile(fcol, j, px, py, ws):
            lru_s1(j, px, ws)
            lru_s1b(ws)
            lru_s2(fcol, j, py, ws)

        def lru_s1b(ws):
            cp(ws.xcb.ap, ws.xc.ap, [ws.xc], [ws.xcb])

        def lru_s1(j, px, ws, ceng="dve"):
            xl, xc, xcb = ws.xl, ws.xc, ws.xcb
            R.op("act", lambda e, o=xl.ap[:, 3:3 + TB], i_=px.ap: e.activation(out=o, in_=i_, func=AF.Copy), [px], [xl])
            cp(xl.ap[:, 0:3], hal3[:, j, :], [hal], [xl])
            cp(hal3[:, j, :], xl.ap[:, TB:TB + 3], [xl], [hal])
            if ceng == "pool":
                ct_ = ws.ctmp
                ts(xc.ap, xl.ap[:, 3:3 + TB], cw[:, 3, j:j + 1], cb[:, j:j + 1], ALU.mult, ALU.add, [xl, prm], [xc], eng="pool")
                ts(ct_.ap, xl.ap[:, 2:2 + TB], cw[:, 2, j:j + 1], None, ALU.mult, None, [xl, prm], [ct_], eng="pool")
                tt(xc.ap, xc.ap, ct_.ap, ALU.add, [xc, ct_], [xc], eng="pool")
                for k in range(2):
                    stt(xc.ap, xl.ap[:, k:k + TB], cw[:, k, j:j + 1], xc.ap, ALU.mult, ALU.add, [xl, prm, xc], [xc])
            else:
                ts(xc.ap, xl.ap[:, 3:3 + TB], cw[:, 3, j:j + 1], cb[:, j:j + 1], ALU.mult, ALU.add, [xl, prm], [xc])
                for k in range(3):
                    stt(xc.ap, xl.ap[:, k:k + TB], cw[:, k, j:j + 1], xc.ap, ALU.mult, ALU.add, [xl, prm, xc], [xc])

        def lru_s2(fcol, j, py, ws):
            xl, xc, xcb, rg, ig, at, mt, bt, ht, gy, tiny = (ws.xl, ws.xc, ws.xcb, ws.rg, ws.ig, ws.at, ws.mt,
                                                              ws.bt, ws.ht, ws.gy, ws.tiny)
            pr_ = next_ps()
            pi_ = next_ps()
            mm(pr_.ap, Wa3[:, j, :], xcb.ap, True, True, [Wa, xcb], [pr_])
            mm(pi_.ap, Wx3[:, j, :], xcb.ap, True, True, [Wx, xcb], [pi_])
            act(rg.ap, pr_.ap, AF.Tanh, [pr_, hprm], [rg], bias=hba[:, j:j + 1], scale=0.5)
            act(ig.ap, pi_.ap, AF.Tanh, [pi_, hprm], [ig], bias=hbx[:, j:j + 1], scale=0.5)
            act(at.ap, rg.ap, AF.Exp, [rg, hprm], [at], bias=hce[:, j:j + 1], scale=hce[:, j:j + 1])
            act(mt.ap, rg.ap, AF.Exp, [rg, cexp], [mt], bias=cexp.ap[:, j:j + 1], scale=cexp.ap[:, j:j + 1])
            act(mt.ap, mt.ap, AF.Sqrt, [mt], [mt], bias=0.25, scale=-0.25)
            if fcol is not None:
                ts(tiny.ap[:, 2:3], mt.ap[:, 0:1], -1.0, 0.5, ALU.mult, ALU.add, [mt], [tiny])
                stt(mt.ap[:, 0:1], tiny.ap[:, 2:3], flag_b.ap[:, fcol:fcol + 1], mt.ap[:, 0:1], ALU.mult, ALU.add, [tiny, flag_b, mt], [mt])
            tt(bt.ap, xc.ap, mt.ap, ALU.mult, [xc, mt], [bt])
            stt(bt.ap, ig.ap, 1.0, bt.ap, ALU.add, ALU.mult, [ig, bt], [bt])
            scan(ht.ap, at.ap, bt.ap, hst.ap[:, j:j + 1], [at, bt, hst], [ht])
            cp(hst.ap[:, j:j + 1], ht.ap[:, TB - 1:TB], [ht], [hst])
            if py is not None:
                act(gy.ap, py.ap, AF.Gelu_apprx_tanh, [py], [gy])
                tt(ol3[:, j, :], ht.ap, gy.ap, ALU.mult, [ht, gy], [out_lru])

        mark = A.off

        class _WS:
            pass
        revb = new("revb", A.f32(1))
        load(revb, revb.ap, revc)
        lam128 = new("lam128", A.f32(32))
        lr128 = lam128.ap[:, 0:16]
        li128 = lam128.ap[:, 16:32]
        tt(lr128, rho, cos1, ALU.mult, [s5q], [lam128])
        tt(li128, rho, sin1, ALU.mult, [s5q], [lam128])
        for _ in range(7):
            tt(t1_, lr128, lr128, ALU.mult, [lam128], [s5q])
            tt(t2_, li128, li128, ALU.mult, [lam128], [s5q])
            tt(li128, lr128, li128, ALU.mult, [lam128], [lam128])
            ts(li128, li128, 2.0, None, ALU.mult, None, [lam128], [lam128])
            tt(lr128, t1_, t2_, ALU.subtract, [s5q], [lam128])
        bbp = new("bbp", A.f32(2 * 512))
        bbpr = bbp.ap[:, 0:512]
        bbpi = bbp.ap[:, 512:1024]
        bbpr3 = bbpr.rearrange("p (i c) -> p i c", i=16)
        bbpi3 = bbpi.rearrange("p (i c) -> p i c", i=16)
        R.op("dve", lambda e: e.memset(bbp.ap, 0.0), [], [bbp])
        cp(bbpr3[0:64, :, 0:16], bbr[0:64], [bb], [bbp])
        cp(bbpr3[64:128, :, 16:32], bbr[64:128], [bb], [bbp])
        cp(bbpi3[0:64, :, 0:16], bbi[0:64], [bb], [bbp])
        cp(bbpi3[64:128, :, 16:32], bbi[64:128], [bb], [bbp])
        WresB = []
        Wres_ap = A.bf16(16 * 2048)
        Wres3 = Wres_ap.rearrange("p (k n) -> p k n", k=16)
        for c4 in range(4):
            wb = new("Wres%d" % c4, Wres3[:, :, c4 * 512:(c4 + 1) * 512])
            R.dma("pool", wb.ap, w_in[:, c4 * 512:(c4 + 1) * 512].rearrange("(k p) n -> p k n", p=128), [], [wb], wb)
            WresB.append(wb)
        GtR = new("GtR", A.bf16(2048))
        GtI = new("GtI", A.bf16(2048))
        hTps = [new("hTp%d" % i, A.bf16(16 * TB)) for i in range(2)]
        hTp3s = [h_.ap.rearrange("p (k t) -> p k t", k=16) for h_ in hTps]
        sq1 = new("sq1", A.f32(TB))
        sq2 = new("sq2", A.f32(TB))
        nsPs = []
        for si in range(2):
            n_ = _WS()
            n_.xt = new("xtP%d" % si, A.f32(D))
            n_.hb = new("hbP%d" % si, A.bf16(D))
            n_.ssb = new("ssbP%d" % si, A.f32(2))
            nsPs.append(n_)
        utok = [new("utok%d" % i, A.bf16(512)) for i in range(2)]
        zt = new("zt", A.f32(16 * 6))
        zre, zim, za, zb, zc, zd = [zt.ap[:, 16 * k:16 * (k + 1)] for k in range(6)]
        wsP = []
        for si in range(2):
            w = _WS()
            w.xl = new("xlP%d" % si, A.f32(3 + TB))
            for nm in ("xc", "rg", "ig", "at", "mt", "bt", "ht"):
                setattr(w, nm, new(nm + "P%d" % si, A.f32(TB)))
            w.gy = None
            w.xcb = new("xcbP%d" % si, A.bf16(TB))
            w.ctmp = new("ctmpP%d" % si, A.f32(TB))
            w.tiny = new("tinyP%d" % si, A.f32(8))
            wsP.append(w)
        wA = wsP[0]
        wB = wsP[1]
        lim_f = lam_im.rearrange("g p -> (g p)")
        lre_f = lam_re.rearrange("g p -> (g p)")
        INV2PI = 1.0 / (2.0 * math.pi)
        st8 = new("st8", A.f32(32))
        load(st8, st8.ap, lstep.partition_broadcast(128))
        act(st8.ap, st8.ap, AF.Exp, [st8], [st8])
        for ch in range(4):
            ssl = slice(ch * 512, (ch + 1) * 512)
            load(wA.xc, wA.xc.ap, lim_f[ssl].partition_broadcast(128))
            load(wA.rg, wA.rg.ap, lre_f[ssl].partition_broadcast(128))
            cp(wA.ig.ap.rearrange("p (g q) -> p g q", g=8),
               st8.ap[:, ch * 8:(ch + 1) * 8].unsqueeze(2).to_broadcast([128, 8, 64]), [st8], [wA.ig])
            tt(wA.xc.ap, wA.xc.ap, wA.ig.ap, ALU.mult, [wA.xc, wA.ig], [wA.xc])
            tt(wA.rg.ap, wA.rg.ap, wA.ig.ap, ALU.mult, [wA.rg, wA.ig], [wA.rg])
            act(wA.at.ap, wA.rg.ap, AF.Exp, [wA.rg, revb], [wA.at], scale=revb.ap[:, 0:1])
            for (ph, dstb) in ((0.0, GtI), (0.25, GtR)):
                tn, tn2, tni = wB.xc, wB.rg, wB.ig
                ts(tn.ap, wA.xc.ap, revb.ap[:, 0:1], INV2PI, ALU.mult, ALU.mult, [wA.xc, revb], [tn])
                if ph != 0.0:
                    ts(tn.ap, tn.ap, ph, None, ALU.add, None, [tn], [tn])
                tni_ap = tni.ap.bitcast(mybir.dt.int32)
                cp(tni_ap, tn.ap, [tn], [tni])
                cp(tn2.ap, tni_ap, [tni], [tn2])
                tt(tn.ap, tn.ap, tn2.ap, ALU.subtract, [tn, tn2], [tn])
                ts(tn2.ap, tn.ap, 0.5, None, ALU.is_gt, None, [tn], [tn2])
                tt(tn.ap, tn.ap, tn2.ap, ALU.subtract, [tn, tn2], [tn])
                ts(tn2.ap, tn.ap, -0.5, None, ALU.is_lt, None, [tn], [tn2])
                tt(tn.ap, tn.ap, tn2.ap, ALU.add, [tn, tn2], [tn])
                act(wB.at.ap, tn.ap, AF.Sin, [tn], [wB.at], scale=2.0 * math.pi)
                tt(dstb.ap[:, ssl], wB.at.ap, wA.at.ap, ALU.mult, [wB.at, wA.at], [dstb])

        sst2p = sst.ap.rearrange("p (i c) -> p i c", c=2)
        F_re = sst2p[:, :, 0]
        F_im = sst2p[:, :, 1]
        ZT = [zt]
        ps_pool[0] = [0, 1, 2, 3, 4]

        def pnorm(pb_, it_):
            pnormA(pb_, it_)
            pnormT(pb_, it_)

        def pnormA(pb_, it_):
            n_ = nsPs[it_ % 2]
            norm_tile(xp[pb_ * TB + it_ * 128:pb_ * TB + (it_ + 1) * 128, :], nwb, n_)

        def pnormT(pb_, it_):
            n_ = nsPs[it_ % 2]
            transpose_rows(n_.hb, hTps[pb_ % 2], hTp3s[pb_ % 2], it_ * 128)

        def s5p_a1(hTp, hTp3, it):
            pu = ps[5]
            for k in range(16):
                mm(pu.ap, hTp3[:, k, it * 128:(it + 1) * 128], Wres3[:, k, 0:512], k == 0, k == 15, [hTp, WresB[0]], [pu])
            ub = utok[it % 2]
            R.op("act", lambda e, o=ub.ap, i_=pu.ap: e.activation(out=o, in_=i_, func=AF.Copy), [pu], [ub])

        def s5p_a2(it):
            ub = utok[it % 2]
            pvr = ps[6]
            pvi = ps[7]
            for i in range(16):
                mm(pvr.ap[:, 32 * i:32 * i + 32], GtR.ap[:, 128 * i:128 * i + 128], ub.ap[:, 32 * i:32 * i + 32], True, True, [GtR, ub], [pvr])
            for i in range(16):
                mm(pvi.ap[:, 32 * i:32 * i + 32], GtI.ap[:, 128 * i:128 * i + 128], ub.ap[:, 32 * i:32 * i + 32], True, True, [GtI, ub], [pvi])

        def s5p_b():
            pvr = ps[6]
            pvi = ps[7]
            q1, q2 = sq1, sq2
            tt(q1.ap, pvr.ap, bbpr, ALU.mult, [pvr, bbp], [q1])
            tt(q2.ap, pvi.ap, bbpi, ALU.mult, [pvi, bbp], [q2])
            tt(q1.ap, q1.ap, q2.ap, ALU.subtract, [q1, q2], [q1])
            R.op("dve", lambda e, o=zre, i_=q1.ap.rearrange("p (i c) -> p i c", i=16): e.tensor_reduce(out=o, in_=i_, axis=AX.X, op=ALU.add), [q1], ZT)
            tt(q1.ap, pvi.ap, bbpr, ALU.mult, [pvi, bbp], [q1])
            tt(q2.ap, pvr.ap, bbpi, ALU.mult, [pvr, bbp], [q2])
            tt(q1.ap, q1.ap, q2.ap, ALU.add, [q1, q2], [q1])
            R.op("dve", lambda e, o=zim, i_=q1.ap.rearrange("p (i c) -> p i c", i=16): e.tensor_reduce(out=o, in_=i_, axis=AX.X, op=ALU.add), [q1], ZT)
            tt(za, lr128, F_re, ALU.mult, [lam128, sst], ZT)
            tt(zb, li128, F_im, ALU.mult, [lam128, sst], ZT)
            tt(za, za, zb, ALU.subtract, ZT, ZT)
            tt(za, za, zre, ALU.add, ZT, ZT)
            tt(zc, lr128, F_im, ALU.mult, [lam128, sst], ZT)
            tt(zd, li128, F_re, ALU.mult, [lam128, sst], ZT)
            tt(zc, zc, zd, ALU.add, ZT, ZT)
            tt(zc, zc, zim, ALU.add, ZT, ZT)
            cp(F_re, za, ZT, [sst])
            cp(F_im, zc, ZT, [sst])

        for it in range(4):
            pnorm(0, it)
        for pb in range(NPB):
            fcol = pb if pb % 4 == 0 else None
            hTp, hTp3 = hTps[pb % 2], hTp3s[pb % 2]
            side = {0: [("a1", 0)], 1: [("a2", 0), ("nA", 0)], 2: [("b", 0), ("a1", 1), ("nT", 0)],
                    3: [("a2", 1)], 4: [("b", 1), ("a1", 2), ("nA", 1)], 5: [("a2", 2), ("nT", 1)],
                    6: [("b", 2), ("a1", 3)], 7: [("a2", 3), ("nA", 2)], 8: [("b", 3), ("nT", 2)],
                    9: [], 10: [("nA", 3)], 11: [("nT", 3)]}
            for j in range(12):
                px = next_ps()
                cs_ = slice(512 + 128 * j, 512 + 128 * (j + 1))
                for k in range(16):
                    mm(px.ap, Wres3[:, k, cs_], hTp3[:, k, :], k == 0, k == 15, [WresB[1 + j // 4], hTp], [px])
                lru_s1(j, px, wsP[j % 2])
                lru_s1b(wsP[j % 2])
                if j > 0:
                    lru_s2(fcol, j - 1, None, wsP[(j - 1) % 2])
                for kind, idx in side[j]:
                    if kind == "a1":
                        s5p_a1(hTp, hTp3, idx)
                    elif kind == "a2":
                        s5p_a2(idx)
                    elif kind == "b":
                        s5p_b()
                    elif pb + 1 < NPB:
                        if kind == "nA":
                            pnormA(pb + 1, idx)
                        else:
                            pnormT(pb + 1, idx)
            lru_s2(fcol, 11, None, wsP[1])
            kc = keep_b.ap[:, pb:pb + 1]
            ts(sst.ap, sst.ap, kc, None, ALU.mult, None, [sst, keep_b], [sst])
            ts(hst.ap, hst.ap, kc, None, ALU.mult, None, [hst, keep_b], [hst])
            ts(hal.ap, hal.ap, kc, None, ALU.mult, None, [hal, keep_b], [hal])
        ps_pool[0] = list(range(8))

        R.barrier()
        print("prefix arena hi", A.hi)
        global _DBG_A
        _DBG_A = A
        A.off = mark
        A.hi = mark
        Ecos = new("Ecos", A.f32(16 * L))
        Esin = new("Esin", A.f32(16 * L))
        Ec3 = Ecos.ap.rearrange("p (i l) -> p i l", i=16)
        Es3 = Esin.ap.rearrange("p (i l) -> p i l", i=16)
        targ = new("targ", A.f32(L))
        targ2 = new("targ2", A.f32(L))
        targi = new("targi", A.f32(L).bitcast(mybir.dt.int32))
        for i in range(16):
            for (tab, tabb, ph) in ((Es3, Esin, 0.0), (Ec3, Ecos, 0.25)):
                ts(targ.ap, ramp_b.ap, th[:, i:i + 1], 1.0 / TWO_PI, ALU.mult, ALU.mult, [ramp_b, s5q], [targ])
                if ph != 0.0:
                    ts(targ.ap, targ.ap, ph, None, ALU.add, None, [targ], [targ])
                cp(targi.ap, targ.ap, [targ], [targi])
                cp(targ2.ap, targi.ap, [targi], [targ2])
                tt(targ.ap, targ.ap, targ2.ap, ALU.subtract, [targ, targ2], [targ])
                ts(targ2.ap, targ.ap, 0.5, None, ALU.is_gt, None, [targ], [targ2])
                tt(targ.ap, targ.ap, targ2.ap, ALU.subtract, [targ, targ2], [targ])
                ts(targ2.ap, targ.ap, -0.5, None, ALU.is_lt, None, [targ], [targ2])
                tt(targ.ap, targ.ap, targ2.ap, ALU.add, [targ, targ2], [targ])
                act(tab[:, i, :], targ.ap, AF.Sin, [targ], [tabb], scale=TWO_PI)
        kk = new("kk", A.f32(64))
        K1 = kk.ap[:, 0:32].rearrange("p (i c) -> p i c", c=2)
        K2 = kk.ap[:, 32:64].rearrange("p (i c) -> p i c", c=2)
        cp(K1[:, :, 0], Ec3[:, :, L - 1], [Ecos], [kk])
        ts(K1[:, :, 1], Es3[:, :, L - 1], -1.0, None, ALU.mult, None, [Esin], [kk])
        cp(K2[:, :, 0], Es3[:, :, L - 1], [Esin], [kk])
        cp(K2[:, :, 1], Ec3[:, :, L - 1], [Ecos], [kk])
        fj = new("fj", A.f32(4))
        BtR = new("BtR", A.bf16(16 * 128))
        BtI = new("BtI", A.bf16(16 * 128))
        BtR3 = BtR.ap.rearrange("p (i q) -> p i q", i=16)
        BtI3 = BtI.ap.rearrange("p (i q) -> p i q", i=16)
        Sst = new("Sst", A.bf16(128))
        for i in range(16):
            base = (i % 4) * 32
            for (src, dstb, dst3) in ((bbr, BtR, BtR3), (bbi, BtI, BtI3)):
                R.op("dve", lambda e: e.memset(Sst.ap, 0.0), [], [Sst])
                cp(Sst.ap[0:64, base:base + 16], src[0:64, i, :], [bb], [Sst])
                cp(Sst.ap[64:128, base + 16:base + 32], src[64:128, i, :], [bb], [Sst])
                pb = next_ps()
                pbv = pb.ap.bitcast(BF16)[:, 0:128]
                tr(pbv, Sst.ap, [Sst, ident_b], [pb])
                cp(dst3[:, i, :], pbv, [pb], [dstb])
        if full:
            bgt = new("bgt", A.f32(32 + 4 + 4))
            load(bgt, bgt.ap[:, 0:32], col(b_gate, 32), slow=True)
            s5dt = bgt.ap[:, 32:36]
            bglt = bgt.ap[:, 36:40]
            load(bgt, s5dt, col(s5_d, 4), slow=True)
            load(bgt, bglt, col(b_glu, 4), slow=True)
            Wglu = new("Wglu", A.bf16(4 * 512))
            Wglu3 = Wglu.ap.rearrange("p (k n) -> p k n", k=4)
            load(Wglu, Wglu3, w_glu.rearrange("(k p) n -> p k n", p=128), q="pool")
            CtR = new("CtR", A.bf16(16 * 128))
            CtI = new("CtI", A.bf16(16 * 128))
            CtR3 = CtR.ap.rearrange("p (i q) -> p i q", i=16)
            CtI3 = CtI.ap.rearrange("p (i q) -> p i q", i=16)
            R.op("dve", lambda e: e.memset(CtR.ap, 0.0), [], [CtR])
            R.op("dve", lambda e: e.memset(CtI.ap, 0.0), [], [CtI])
            for i in range(16):
                base = (i % 4) * 32
                for g2 in range(2):
                    cs_ = slice(base + 16 * g2, base + 16 * g2 + 16)
                    ps_ = slice(g2 * 64, (g2 + 1) * 64)
                    cp(CtR3[ps_, i, cs_], CqV[0][g2][ps_, i, :], [CqB[0][g2]], [CtR])
                    cp(CtI3[ps_, i, cs_], CqV[1][g2][ps_, i, :], [CqB[1][g2]], [CtI])

        hT = new("hT", A.bf16(16 * TB))
        hT3 = hT.ap.rearrange("p (k t) -> p k t", k=16)
        uT = new("uT", A.bf16(4 * TB))
        uT3 = uT.ap.rearrange("p (k t) -> p k t", k=4)
        NSLOT = 3
        ring = [new("ring%d" % s, A.bf16(16 * 512)) for s in range(NSLOT)]
        ring_h = []
        for s in range(NSLOT):
            r3 = ring[s].ap.rearrange("p (k n) -> p k n", k=16)
            ring_h.append(r3)
        rr = [0]

        def next_slot():
            s = rr[0] % NSLOT
            rr[0] += 1
            return ring[s], ring_h[s]

        def w_in_cols(c0, n):
            return w_in[:, c0:c0 + n].rearrange("(k p) n -> p k n", p=128)

        xt = new("xt", A.f32(D))
        hb = new("hb", A.bf16(D))
        junk = hb
        ssb = new("ssb", A.f32(2))
        wk0 = A.off
        xl = new("xl", A.f32(3 + TB))
        xc = new("xc", A.f32(TB))
        xcb = new("xcb", A.bf16(TB))
        rg = new("rg", A.f32(TB))
        ig = new("ig", A.f32(TB))
        at = new("at", A.f32(TB))
        mtbt = new("mtbt", A.f32(2 * TB))
        mt = Buf("mt", mtbt.ap[:, 0:TB], root=mtbt)
        bt = Buf("bt", mtbt.ap[:, TB:2 * TB], root=mtbt)
        w2v = mtbt.ap.rearrange("p (c t) -> p c t", c=2)
        ht = new("ht", A.f32(TB))
        gy = new("gy", A.f32(TB))
        A.off = wk0
        f1, f2, f3, f4, wre_b, wim_b = xc, rg, ig, at, mt, bt
        A.off = A.hi
        tiny = new("tiny", A.f32(8))
        if full:
            sre = new("sre", A.bf16(TB))
            sim = new("sim", A.bf16(TB))
            yv = ht
            ygT = new("ygT", A.bf16(4 * TB))
            ygT3 = ygT.ap.rearrange("p (k t) -> p k t", k=4)
            glt = gy
            out_s5 = new("out_s5", A.bf16(4 * TB))
            os53 = out_s5.ap.rearrange("p (k t) -> p k t", k=4)
            out_lru = new("out_lru", A.bf16(12 * TB))
            ol3 = out_lru.ap.rearrange("p (k t) -> p k t", k=12)
            mergedT = new("mergedT", A.bf16(16 * TB))
            mg3 = mergedT.ap.rearrange("p (k t) -> p k t", k=16)
            g1 = xc
            g2b = rg
            xr = [ig, at]

        class _NS:
            pass
        ns0 = _NS()
        ns0.xt, ns0.hb, ns0.ssb = xt, hb, ssb
        ws0 = _NS()
        (ws0.xl, ws0.xc, ws0.xcb, ws0.rg, ws0.ig, ws0.at, ws0.mt, ws0.bt, ws0.ht, ws0.gy, ws0.tiny) = (
            xl, xc, xcb, rg, ig, at, mt, bt, ht, gy, tiny)
        ws1 = _NS()
        (ws1.rg, ws1.ig, ws1.at, ws1.mt, ws1.bt, ws1.ht, ws1.gy, ws1.tiny) = (rg, ig, at, mt, bt, ht, gy, tiny)
        ws1.xl = new("xl1", A.f32(3 + TB))
        ws1.xc = new("xc1", A.f32(TB))
        ws1.xcb = new("xcb1", A.bf16(TB))
        wsO = [ws0, ws1]

        def s5_block(do_out):
            for i in range(16):
                pr = next_ps()
                pi = next_ps()
                mm(pr.ap, BtR3[:, i, :], uT3[:, i // 4, :], True, True, [BtR, uT], [pr])
                mm(pi.ap, BtI3[:, i, :], uT3[:, i // 4, :], True, True, [BtI, uT], [pi])
                ec = Ec3[:, i:i + 1, :].to_broadcast([128, TB // L, L])
                es_ = Es3[:, i:i + 1, :].to_broadcast([128, TB // L, L])

                def v4(ap):
                    return ap.rearrange("p (b l) -> p b l", l=L)
                tt(v4(f1.ap), v4(pr.ap), ec, ALU.mult, [pr, Ecos], [f1])
                tt(v4(f2.ap), v4(pi.ap), es_, ALU.mult, [pi, Esin], [f2])
                tt(f1.ap, f1.ap, f2.ap, ALU.add, [f1, f2], [f1])
                tt(v4(f3.ap), v4(pi.ap), ec, ALU.mult, [pi, Ecos], [f3])
                tt(v4(f4.ap), v4(pr.ap), es_, ALU.mult, [pr, Esin], [f4])
                tt(f3.ap, f3.ap, f4.ap, ALU.subtract, [f3, f4], [f3])
                s_re = sst.ap[:, 2 * i:2 * i + 1]
                s_im = sst.ap[:, 2 * i + 1:2 * i + 2]
                rb = rho[:, i:i + 1].to_broadcast([128, L])
                for b in range(TB // L):
                    sl_ = slice(b * L, (b + 1) * L)
                    scan(wre_b.ap[:, sl_], rb, f1.ap[:, sl_], s_re, [s5q, f1, sst], [wre_b])
                    scan(wim_b.ap[:, sl_], rb, f3.ap[:, sl_], s_im, [s5q, f3, sst], [wim_b])
                    last2 = w2v[:, :, (b + 1) * L - 1]
                    stt(fj.ap[:, 0:2], last2, 1.0, K1[:, i, :], ALU.mult, ALU.mult, [mtbt, kk], [fj, sst])
                    R.ops[-1].fn = (lambda e, o=fj.ap[:, 0:2], a_=last2, k_=K1[:, i, :], acc_=s_re:
                                    e.scalar_tensor_tensor(out=o, in0=a_, scalar=1.0, in1=k_, op0=ALU.mult, op1=ALU.mult, accum_out=acc_))
                    stt(fj.ap[:, 2:4], last2, 1.0, K2[:, i, :], ALU.mult, ALU.mult, [mtbt, kk], [fj, sst])
                    R.ops[-1].fn = (lambda e, o=fj.ap[:, 2:4], a_=last2, k_=K2[:, i, :], acc_=s_im:
                                    e.scalar_tensor_tensor(out=o, in0=a_, scalar=1.0, in1=k_, op0=ALU.mult, op1=ALU.mult, accum_out=acc_))
                if do_out:
                    tt(v4(f2.ap), v4(wre_b.ap), ec, ALU.mult, [wre_b, Ecos], [f2])
                    tt(v4(f4.ap), v4(wim_b.ap), es_, ALU.mult, [wim_b, Esin], [f4])
                    tt(sre.ap, f2.ap, f4.ap, ALU.subtract, [f2, f4], [sre])
                    tt(v4(f2.ap), v4(wre_b.ap), es_, ALU.mult, [wre_b, Esin], [f2])
                    tt(v4(f4.ap), v4(wim_b.ap), ec, ALU.mult, [wim_b, Ecos], [f4])
                    stt(sim.ap, f2.ap, -1.0, f4.ap, ALU.mult, ALU.subtract, [f2, f4], [sim])
                    jt = i // 4
                    if i % 4 == 0:
                        s5_block.yc = next_ps()
                    yc = s5_block.yc
                    mm(yc.ap, CtR3[:, i, :], sre.ap, i % 4 == 0, False, [CtR, sre], [yc])
                    mm(yc.ap, CtI3[:, i, :], sim.ap, False, i % 4 == 3, [CtI, sim], [yc])
                    if i % 4 == 3:
                        stt(yv.ap, uT3[:, jt, :], s5dt[:, jt:jt + 1], yc.ap, ALU.mult, ALU.add, [uT, bgt, yc], [yv])
                        act(ygT3[:, jt, :], yv.ap, AF.Gelu_apprx_tanh, [yv], [ygT])
            if do_out:
                for nt_ in range(4):
                    pg = next_ps()
                    for k in range(4):
                        mm(pg.ap, Wglu3[:, k, nt_ * 128:(nt_ + 1) * 128], ygT3[:, k, :], k == 0, k == 3, [Wglu, ygT], [pg])
                    act(glt.ap, pg.ap, AF.Sigmoid, [pg, bgt], [glt], bias=bglt[:, nt_:nt_ + 1])
                    tt(os53[:, nt_, :], ygT3[:, nt_, :], glt.ap, ALU.mult, [ygT, glt], [out_s5])

        def front(src, row0, do_out, fcol):
            for it in range(4):
                norm_tile(src[row0 + it * 128:row0 + (it + 1) * 128, :], nwb, ns0)
                transpose_rows(hb, hT, hT3, it * 128)
            sb, s3 = next_slot()
            load(sb, s3, w_in_cols(0, 512), q="pool")
            for ct in range(4):
                pu = next_ps()
                for k in range(16):
                    mm(pu.ap, s3[:, k, ct * 128:(ct + 1) * 128], hT3[:, k, :], k == 0, k == 15, [sb, hT], [pu])
                R.op("act", lambda e, o=uT3[:, ct, :], i_=pu.ap: e.activation(out=o, in_=i_, func=AF.Copy), [pu], [uT])
            s5_block(do_out)
            pend_l = None
            for q in range(6):
                sb, s3 = next_slot()
                R.dma("pool", s3[:, :, 0:256], w_in_cols(512 + 256 * q, 256), [], [sb], sb)
                if do_out:
                    R.dma("pool", s3[:, :, 256:512], w_in_cols(2048 + 256 * q, 256), [], [sb], sb)
                for jj in range(2):
                    j = 2 * q + jj
                    px = next_ps()
                    for k in range(16):
                        mm(px.ap, s3[:, k, jj * 128:(jj + 1) * 128], hT3[:, k, :], k == 0, k == 15, [sb, hT], [px])
                    py = None
                    if do_out:
                        py = next_ps()
                        for k in range(16):
                            mm(py.ap, s3[:, k, 256 + jj * 128:256 + (jj + 1) * 128], hT3[:, k, :], k == 0, k == 15, [sb, hT], [py])
                    lru_s1(j, px, wsO[j % 2])
                    lru_s1b(wsO[j % 2])
                    if pend_l is not None:
                        lru_s2(fcol, pend_l[0], pend_l[1], wsO[pend_l[0] % 2])
                    pend_l = (j, py)
            lru_s2(fcol, pend_l[0], pend_l[1], wsO[pend_l[0] % 2])

        for tb in range(NB):
            t0_ = tb * TB
            front(x, t0_, True, NPB if tb == 0 else None)
            for mg in range(4):
                gsb, gs3 = next_slot()
                load(gsb, gs3, w_in_cols(3584 + 512 * mg, 512), q="pool")
                glb, gl3 = next_slot()
                load(glb, gl3, w_in_cols(5632 + 512 * mg, 512), q="pool")
                prb, pr3 = next_slot()
                R.dma("pool", pr3[:, 0:4, :], wps[:, 512 * mg:512 * (mg + 1)].rearrange("(k p) n -> p k n", p=128), [], [prb], prb)
                R.dma("pool", pr3[:, 4:16, :], wpl[:, 512 * mg:512 * (mg + 1)].rearrange("(k p) n -> p k n", p=128), [], [prb], prb)
                for ct in range(4):
                    m = mg * 4 + ct
                    cs = slice(ct * 128, (ct + 1) * 128)
                    pC = next_ps()
                    for k in range(16):
                        mm(pC.ap, gs3[:, k, cs], hT3[:, k, :], k == 0, k == 15, [gsb, hT], [pC])
                    pD = next_ps()
                    for k in range(16):
                        mm(pD.ap, gl3[:, k, cs], hT3[:, k, :], k == 0, k == 15, [glb, hT], [pD])
                    pA = next_ps()
                    for k in range(4):
                        mm(pA.ap, pr3[:, k, cs], os53[:, k, :], k == 0, k == 3, [prb, out_s5], [pA])
                    pB = next_ps()
                    for k in range(12):
                        mm(pB.ap, pr3[:, 4 + k, cs], ol3[:, k, :], k == 0, k == 11, [prb, out_lru], [pB])
                    act(g1.ap, pC.ap, AF.Sigmoid, [pC, bgt], [g1], bias=bgt.ap[:, m:m + 1])
                    act(g2b.ap, pD.ap, AF.Sigmoid, [pD, bgt], [g2b], bias=bgt.ap[:, 16 + m:17 + m])
                    tt(g1.ap, g1.ap, pA.ap, ALU.mult, [g1, pA], [g1])
                    tt(g2b.ap, g2b.ap, pB.ap, ALU.mult, [g2b, pB], [g2b])
                    tt(mg3[:, m, :], g1.ap, g2b.ap, ALU.add, [g1, g2b], [mergedT])
            for n in range(4):
                wob, wo3 = next_slot()
                load(wob, wo3, w_out[:, 512 * n:512 * (n + 1)].rearrange("(k p) n -> p k n", p=128), q="pool")
                for it in range(4):
                    po = next_ps()
                    for k in range(16):
                        mm(po.ap, mg3[:, k, it * 128:(it + 1) * 128], wo3[:, k, :], k == 0, k == 15, [mergedT, wob], [po])
                    xb = xr[(n * 4 + it) % 2]
                    rows = slice(t0_ + it * 128, t0_ + (it + 1) * 128)
                    load(xb, xb.ap, x[rows, 512 * n:512 * (n + 1)])
                    tt(xb.ap, xb.ap, po.ap, ALU.add, [xb, po], [xb])
                    R.dma("sp", x1s[rows, 512 * n:512 * (n + 1)], xb.ap, [xb], [], xb)

        R.barrier()
        print("own-pass arena hi", A.hi)
        A.off = 0
        ident_b2 = new("ident2", A.bf16(128))
        load(ident_b2, ident_b2.ap, ident)
        ident_b = ident_b2
        nfwb = new("nfwb", A.f32(D))
        load(nfwb, nfwb.ap, nfw.partition_broadcast(128))
        nfinb = new("nfinb", A.f32(D))
        load(nfinb, nfinb.ap, nfin.partition_broadcast(128))
        wr = new("wr", A.bf16(16 * 36))
        wr3 = wr.ap.rearrange("p (k n) -> p k n", k=16)
        R.dma("pool", wr3[:, :, 0:4], wrg.rearrange("(k p) n -> p k n", p=128), [], [wr], wr, slow=True)
        R.dma("pool", wr3[:, :, 4:36], wre.rearrange("(k p) n -> p k n", p=128), [], [wr], wr, slow=True)
        brb = new("brb", A.f32(36))
        load(brb, brb.ap[:, 0:4], brg.partition_broadcast(128))
        load(brb, brb.ap[:, 4:36], bre.partition_broadcast(128))
        HT = 1024
        acc = [new("acc%d" % i, A.f32(D)) for i in range(8)]
        h2T = new("h2T", A.bf16(16 * HT))
        h2T3 = h2T.ap.rearrange("p (k t) -> p k t", k=16)
        comb = new("comb", A.f32(8 * 32))
        comb3 = comb.ap.rearrange("p (i e) -> p i e", i=8)
        Wgu = [new("Wgu%d" % s, A.bf16(16 * 512)) for s in range(2)]
        Wgu3 = [w.ap.rearrange("p (k n) -> p k n", k=16) for w in Wgu]
        Wd = [new("Wd%d" % s, A.bf16(2 * D)) for s in range(2)]
        Wd3 = [w.ap.rearrange("p (f n) -> p f n", f=2) for w in Wd]
        junk2 = new("junk2", A.bf16(D))
        hb2 = new("hb2", A.bf16(D))
        ss2 = new("ss2", A.f32(2))
        sg = [new("sg%d" % s, A.f32(256)) for s in range(2)]
        actb = [new("actb%d" % s, A.bf16(256)) for s in range(2)]
        actT = [new("actT%d" % s, A.bf16(256)) for s in range(2)]
        rt = new("rt", A.f32(36 + 8 * 6 + 16))
        lg = rt.ap[:, 0:36]
        oh = rt.ap[:, 36:40]
        ge = rt.ap[:, 40:44]
        esel = rt.ap[:, 44:52]
        m1k = rt.ap[:, 52:60]
        m2k = rt.ap[:, 60:68]
        es2 = rt.ap[:, 68:76]
        cm8 = rt.ap[:, 76:84]
        sc = rt.ap[:, 84:100]
        ot = new("ot", A.f32(D))

        for hp in range(NT // HT):
            for it in range(8):
                rows = slice(hp * HT + it * 128, hp * HT + (it + 1) * 128)
                R.dma("sp", acc[it].ap, x1s[rows, :], [], [acc[it]], acc[it])
                a_ = acc[it]
                act(junk2.ap, a_.ap, AF.Square, [a_], [junk2, ss2], accum=ss2.ap[:, 0:1])
                ts(ss2.ap[:, 1:2], ss2.ap[:, 0:1], 1.0 / D, EPS, ALU.mult, ALU.add, [ss2], [ss2])
                act(ss2.ap[:, 1:2], ss2.ap[:, 1:2], AF.Sqrt, [ss2], [ss2])
                R.op("dve", lambda e: e.reciprocal(out=ss2.ap[:, 1:2], in_=ss2.ap[:, 1:2]), [ss2], [ss2])
                stt(hb2.ap, a_.ap, ss2.ap[:, 1:2], nfwb.ap, ALU.mult, ALU.mult, [a_, ss2, nfwb], [hb2])
                transpose_rows(hb2, h2T, h2T3, it * 128)
                pr_ = next_ps()
                for k in range(16):
                    mm(pr_.ap[:, 0:36], h2T3[:, k, it * 128:(it + 1) * 128], wr3[:, k, :], k == 0, k == 15, [h2T, wr], [pr_])
                RT = [rt]
                tt(lg, pr_.ap[:, 0:36], brb.ap, ALU.add, [pr_, brb], RT)
                gm, ngm, gs_, gtop, m1, m2, dl, ee, w1, w2, c1, c2 = [sc[:, k:k + 1] for k in range(12)]
                R.op("dve", lambda e, o=gm, i_=lg[:, 0:4]: e.tensor_reduce(out=o, in_=i_, axis=AX.X, op=ALU.max), RT, RT)
                ts(ngm, gm, -1.0, None, ALU.mult, None, RT, RT)
                act(ge, lg[:, 0:4], AF.Exp, RT, RT, bias=ngm, accum=gs_)
                R.op("dve", lambda e, o=gtop, i_=gs_: e.reciprocal(out=o, in_=i_), RT, RT)
                ts(oh, lg[:, 0:4], gm, None, ALU.is_equal, None, RT, RT)
                ts(esel, lg[:, 4:12], oh[:, 0:1], None, ALU.mult, None, RT, RT)
                for g in range(1, 4):
                    stt(esel, lg[:, 4 + 8 * g:12 + 8 * g], oh[:, g:g + 1], esel, ALU.mult, ALU.add, RT, RT)
                R.op("dve", lambda e, o=m1, i_=esel: e.tensor_reduce(out=o, in_=i_, axis=AX.X, op=ALU.max), RT, RT)
                ts(m1k, esel, m1, None, ALU.is_equal, None, RT, RT)
                stt(es2, m1k, -1e30, esel, ALU.mult, ALU.add, RT, RT)
                R.op("dve", lambda e, o=m2, i_=es2: e.tensor_reduce(out=o, in_=i_, axis=AX.X, op=ALU.max), RT, RT)
                ts(m2k, es2, m2, None, ALU.is_equal, None, RT, RT)
                tt(dl, m2, m1, ALU.subtract, RT, RT)
                act(ee, dl, AF.Exp, RT, RT)
                ts(w1, ee, 1.0, None, ALU.add, None, RT, RT)
                R.op("dve", lambda e, o=w1, i_=w1: e.reciprocal(out=o, in_=i_), RT, RT)
                tt(w2, ee, w1, ALU.mult, RT, RT)
                tt(c1, w1, gtop, ALU.mult, RT, RT)
                tt(c2, w2, gtop, ALU.mult, RT, RT)
                ts(cm8, m1k, c1, None, ALU.mult, None, RT, RT)
                stt(cm8, m2k, c2, cm8, ALU.mult, ALU.add, RT, RT)
                for g in range(4):
                    ts(comb3[:, it, 8 * g:8 * g + 8], cm8, oh[:, g:g + 1], None, ALU.mult, None, RT, [comb])
            seq = [(e, it) for e in range(32) for it in range(8)]
            gu_ps = {}

            def load_expert(e):
                s = e % 2
                R.dma("pool", Wgu3[s][:, :, 0:256], weg[e].rearrange("(k p) n -> p k n", p=128), [], [Wgu[s]], Wgu[s])
                R.dma("pool", Wgu3[s][:, :, 256:512], weu[e].rearrange("(k p) n -> p k n", p=128), [], [Wgu[s]], Wgu[s])
                R.dma("pool", Wd3[s], wed[e].rearrange("(f p) n -> p f n", p=128), [], [Wd[s]], Wd[s])

            def emit_gu(idx):
                e, it = seq[idx]
                s = e % 2
                pg = ps[idx % 2]
                for k in range(16):
                    mm(pg.ap, h2T3[:, k, it * 128:(it + 1) * 128], Wgu3[s][:, k, :], k == 0, k == 15, [h2T, Wgu[s]], [pg])
                gu_ps[idx] = pg

            load_expert(0)
            emit_gu(0)
            for idx in range(len(seq)):
                e, it = seq[idx]
                if it == 0 and e + 1 < 32:
                    load_expert(e + 1)
                if idx + 1 < len(seq):
                    emit_gu(idx + 1)
                s = e % 2
                b2 = idx % 2
                pg = gu_ps.pop(idx)
                act(sg[b2].ap, pg.ap[:, 0:256], AF.Silu, [pg], [sg[b2]])
                stt(actb[b2].ap, sg[b2].ap, comb3[:, it, e:e + 1], pg.ap[:, 256:512], ALU.mult, ALU.mult,
                    [sg[b2], comb, pg], [actb[b2]])
                pt_ = ps[2 + b2]
                ptv = pt_.ap.bitcast(BF16)[:, 0:256].rearrange("p (f t) -> p f t", f=2)
                for f in range(2):
                    tr(ptv[:, f, :], actb[b2].ap[:, f * 128:(f + 1) * 128], [actb[b2], ident_b], [pt_])
                aT3 = actT[b2].ap.rearrange("p (f t) -> p f t", f=2)
                R.op("act", lambda e_, o=aT3, i_=ptv: e_.activation(out=o, in_=i_, func=AF.Copy), [pt_], [actT[b2]])
                for n in range(4):
                    pd = ps[4 + n]
                    for f in range(2):
                        mm(pd.ap, aT3[:, f, :], Wd3[s][:, f, n * 512:(n + 1) * 512], f == 0, f == 1, [actT[b2], Wd[s]], [pd])
                    tt(acc[it].ap[:, n * 512:(n + 1) * 512], acc[it].ap[:, n * 512:(n + 1) * 512], pd.ap, ALU.add,
                       [acc[it], pd], [acc[it]])
            for it in range(8):
                rows = slice(hp * HT + it * 128, hp * HT + (it + 1) * 128)
                a_ = acc[it]
                act(junk2.ap, a_.ap, AF.Square, [a_], [junk2, ss2], accum=ss2.ap[:, 0:1])
                ts(ss2.ap[:, 1:2], ss2.ap[:, 0:1], 1.0 / D, EPS, ALU.mult, ALU.add, [ss2], [ss2])
                act(ss2.ap[:, 1:2], ss2.ap[:, 1:2], AF.Sqrt, [ss2], [ss2])
                R.op("dve", lambda e: e.reciprocal(out=ss2.ap[:, 1:2], in_=ss2.ap[:, 1:2]), [ss2], [ss2])
                stt(ot.ap, a_.ap, ss2.ap[:, 1:2], nfinb.ap, ALU.mult, ALU.mult, [a_, ss2, nfinb], [ot])
                R.dma("sp", out[rows, :], ot.ap, [ot], [], ot)
        R.op("sp", None, [ot], [ot])
        R.emit(nc, es)
    return nc


_CACHE = {}


def _get(mode="fused"):
    if mode not in _CACHE:
        _CACHE[mode] = build(mode)
    return _CACHE[mode]


def kernel(**inp):
    f32 = np.float32
    g = {k: np.ascontiguousarray(np.asarray(v), dtype=f32) for k, v in inp.items()}
    x = g["x"].reshape(NCORE * NT, D)
    ident = np.eye(128, dtype=f32).astype(ml_dtypes.bfloat16)
    ramp = np.broadcast_to(np.arange(1, L + 1, dtype=f32)[None, :], (128, L)).copy()
    common = {
        "nmw": g["norm_mix_w"][0], "w_in": g["w_in"][0],
        "lam_re": g["s5_lam_re"][0], "lam_im": g["s5_lam_im"][0], "lstep": g["s5_log_step"][0],
        "b_re": g["s5_b_re"][0], "b_im": g["s5_b_im"][0],
        "conv_w": g["lru_conv_w"][0], "conv_b": g["lru_conv_b"][0],
        "w_a": g["lru_w_a"][0], "b_a": g["lru_b_a"][0], "w_x": g["lru_w_x"][0], "b_x": g["lru_b_x"][0],
        "lru_lam": g["lru_lambda"][0], "ident": ident, "ramp": ramp,
        "revc": (127.0 - np.arange(128, dtype=f32)).reshape(128, 1),
        "b_gate": g["b_gate"][0], "c_re": g["s5_c_re"][0], "c_im": g["s5_c_im"][0],
        "s5_d": g["s5_d"][0], "w_glu": g["s5_w_glu"][0], "b_glu": g["s5_b_glu"][0],
        "wps": g["w_proj_s5"][0], "wpl": g["w_proj_lru"][0], "w_out": g["w_out"][0],
        "nfw": g["norm_ffn_w"][0], "wrg": g["w_router_group"][0], "brg": g["b_router_group"][0],
        "wre": g["w_router_expert"][0], "bre": g["b_router_expert"][0],
        "weg": g["w_e_gate"][0], "weu": g["w_e_up"][0], "wed": g["w_e_down"][0], "nfin": g["norm_final_w"],
    }
    in_maps = []
    for c in range(NCORE):
        xp = np.zeros((NPB * TB, D), f32)
        if c > 0:
            xp[NPB * TB - c * NT:] = x[:c * NT]
        first_real = NPB - 4 * c
        keep = np.zeros((128, NPB), f32)
        keep[:, first_real:] = 1.0
        flag = np.zeros((128, NPB + 1), f32)
        flag[:, first_real] = 1.0
        in_maps.append(dict(common, x=x[c * NT:(c + 1) * NT], xp=xp, keepv=keep, flagv=flag))
    res = run_bass_kernel_spmd(_get(), in_maps, core_ids=list(range(NCORE)))
    outs = [np.asarray(r["out"], dtype=f32) for r in res.results]
    return np.concatenate(outs, axis=0).reshape(1, NCORE * NT, D)
```

```python
import math
from contextlib import ExitStack
import numpy as np
import ml_dtypes
import concourse.bass as bass
import concourse.mybir as mybir
from concourse.bass_utils import run_bass_kernel_spmd

F32 = mybir.dt.float32
BF16 = mybir.dt.bfloat16
AF = mybir.ActivationFunctionType
ALU = mybir.AluOpType
AX = mybir.AxisListType

D = 2048
NCORE = 8
NT = 2048
TB = 512
NB = NT // TB
L = 128
NPAY = 56
EPS = 1e-6
ARENA_WORDS = 53000


class Buf:
    __slots__ = ("name", "ap", "lw", "rd", "sem", "cnt", "root")

    def __init__(self, name, ap, root=None):
        self.name = name
        self.ap = ap
        self.root = root if root is not None else self
        self.lw = None
        self.rd = []
        self.sem = None
        self.cnt = 0


class Op:
    __slots__ = ("eng", "fn", "deps", "is_dma", "prim", "signal", "tok", "idx")


class Rec:
    EPOCH = 6000

    def __init__(self):
        self.ops = []
        self.since_barrier = []

    def _add(self, eng, fn, R, W, is_dma=False, prim=None):
        o = Op()
        o.eng, o.fn, o.is_dma, o.prim, o.signal, o.tok = eng, fn, is_dma, prim, False, None
        o.idx = len(self.ops)
        R = [b.root for b in R]
        W = [b.root for b in W]
        if prim is not None:
            o.prim = prim.root
        deps = set()
        for b in R:
            if b.lw is not None:
                deps.add(b.lw)
        for b in W:
            if b.lw is not None:
                deps.add(b.lw)
            deps.update(b.rd)
        o.deps = deps
        for b in R:
            b.rd.append(o.idx)
        for b in W:
            b.lw = o.idx
            b.rd = []
        self.ops.append(o)
        self.since_barrier.append(o.idx)
        return o

    def op(self, eng, fn, R=(), W=()):
        return self._add(eng, fn, list(R), list(W))

    def dma(self, queue, out_ap, in_ap, R, W, prim, slow=False):
        def fn(e, out_ap=out_ap, in_ap=in_ap, slow=slow):
            if slow:
                return e.dma_start(out=out_ap, in_=in_ap, allow_slow_non_contiguous=True)
            return e.dma_start(out=out_ap, in_=in_ap)
        return self._add(queue, fn, list(R), list(W), True, prim)

    def barrier(self):
        prev = list(self.since_barrier)
        self.since_barrier = []
        for eng in ("sp", "pool", "act", "dve", "pe"):
            o = Op()
            o.eng, o.fn, o.is_dma, o.prim, o.signal, o.tok = eng, None, False, None, False, None
            o.idx = len(self.ops)
            o.deps = set(prev)
            self.ops.append(o)

    def emit(self, nc, es):
        ops = self.ops
        for o in ops:
            for d in o.deps:
                ops[d].signal = True
        eng_cnt = {}
        sems = {}

        def get_sem(key):
            if key not in sems:
                sems[key] = es.enter_context(nc.semaphore("s%d" % len(sems)))
            return sems[key]

        for o in ops:
            if o.fn is None:
                continue
            if o.is_dma:
                b = o.prim
                b.cnt += 16
                o.tok = (("dma", id(b)), b.cnt)
            elif o.signal:
                c = eng_cnt.get(o.eng, 0) + 1
                eng_cnt[o.eng] = c
                o.tok = ((o.eng, (c - 1) // self.EPOCH), (c - 1) % self.EPOCH + 1)
        for o in ops:
            if o.tok is not None:
                get_sem(o.tok[0])
        by_eng = {}
        for o in ops:
            by_eng.setdefault(o.eng, []).append(o)

        def run(engobj, lst):
            waited = {}
            for o in lst:
                need = {}
                for d in o.deps:
                    p = ops[d]
                    if p.tok is None:
                        continue
                    if (not p.is_dma) and p.eng == o.eng and o.eng in ("pe",) and not o.is_dma:
                        continue
                    k, v = p.tok
                    if need.get(k, 0) < v:
                        need[k] = v
                for k, v in need.items():
                    if waited.get(k, 0) >= v:
                        continue
                    engobj.wait_ge(sems[k], v)
                    waited[k] = v
                if o.fn is None:
                    continue
                ins = o.fn(engobj)
                if o.tok is not None:
                    ins.then_inc(sems[o.tok[0]], 16 if o.is_dma else 1)

        with nc.Block() as block:
            @block.sync
            def _(e):
                run(e, by_eng.get("sp", []))

            @block.gpsimd
            def _(e):
                run(e, by_eng.get("pool", []))

            @block.scalar
            def _(e):
                run(e, by_eng.get("act", []))

            @block.vector
            def _(e):
                run(e, by_eng.get("dve", []))

            @block.tensor
            def _(e):
                run(e, by_eng.get("pe", []))


class Arena:
    def __init__(self, ap):
        self.ap = ap
        self.off = 0
        self.hi = 0

    def f32(self, n):
        a = self.ap[:, self.off:self.off + n]
        self.off += n
        self.hi = max(self.hi, self.off)
        assert self.off <= ARENA_WORDS, ("arena overflow", self.off)
        return a

    def bf16(self, n):
        w = (n + 1) // 2
        return self.f32(w).bitcast(BF16)


NPB = 28


def build(mode="fused"):
    full = True
    nc = bass.Bass("TRN2", target_bir_lowering=False)
    R = Rec()

    def din(name, shape, dt=F32):
        return nc.dram_tensor(name, list(shape), dt, kind="ExternalInput").ap()

    x = din("x", [NT, D])
    xp = din("xp", [NPB * TB, D])
    flagv = din("flagv", [128, NPB + 1])
    keepv = din("keepv", [128, NPB])
    revc = din("revc", [128, 1])
    nmw = din("nmw", [D])
    w_in = din("w_in", [D, 7680])
    lam_re = din("lam_re", [32, 64])
    lam_im = din("lam_im", [32, 64])
    lstep = din("lstep", [32])
    b_re = din("b_re", [32, 64, 16])
    b_im = din("b_im", [32, 64, 16])
    conv_w = din("conv_w", [4, 1536])
    conv_b = din("conv_b", [1536])
    w_a = din("w_a", [12, 128, 128])
    b_a = din("b_a", [1536])
    w_x = din("w_x", [12, 128, 128])
    b_x = din("b_x", [1536])
    lru_lam = din("lru_lam", [1536])
    ident = din("ident", [128, 128], BF16)
    ramp = din("ramp", [128, L])
    if full:
        b_gate = din("b_gate", [4096])
        c_re = din("c_re", [32, 16, 64])
        c_im = din("c_im", [32, 16, 64])
        s5_d = din("s5_d", [512])
        w_glu = din("w_glu", [512, 512])
        b_glu = din("b_glu", [512])
        wps = din("wps", [512, D])
        wpl = din("wpl", [1536, D])
        w_out = din("w_out", [D, D])
        nfw = din("nfw", [D])
        wrg = din("wrg", [D, 4])
        brg = din("brg", [4])
        wre = din("wre", [D, 32])
        bre = din("bre", [32])
        weg = din("weg", [32, D, 256])
        weu = din("weu", [32, D, 256])
        wed = din("wed", [32, 256, D])
        nfin = din("nfin", [D])
        out = nc.dram_tensor("out", [NT, D], F32, kind="ExternalOutput").ap()
        x1s = nc.dram_tensor("x1s", [NT, D], F32, kind="Internal").ap()
        wsc = nc.dram_tensor("wsc", [16, 128, 16 * 512], BF16, kind="Internal").ap()
        x1s_b = Buf("x1s", x1s)
        wscB = [Buf("wsc%d" % u, wsc[u]) for u in range(16)]
    else:
        pay = nc.dram_tensor("pay", [128, NPAY], F32, kind="ExternalOutput").ap()

    es = ExitStack()
    with es:
        arena_t = es.enter_context(nc.sbuf_tensor("arena", [128, ARENA_WORDS], F32))
        A = Arena(arena_t[:, :])
        ps = []
        for i in range(8):
            pt = es.enter_context(nc.psum_tensor("ps%d" % i, [128, 512], F32))
            ps.append(Buf("ps%d" % i, pt[:, :]))
        ps_rr = [0]

        ps_pool = [list(range(8))]

        def next_ps():
            pool = ps_pool[0]
            b = ps[pool[ps_rr[0] % len(pool)]]
            ps_rr[0] += 1
            return b

        def new(name, ap):
            return Buf(name, ap)

        def act(out, in_, func, R_, W_, bias=None, scale=None, accum=None):
            kw = {}
            if bias is not None:
                kw["bias"] = bias
            if scale is not None:
                kw["scale"] = scale
            if accum is not None:
                kw["accum_out"] = accum
            R.op("act", lambda e: e.activation(out=out, in_=in_, func=func, **kw), R_, W_)

        def tt(out, a, b, op, R_, W_, eng="dve"):
            R.op(eng, lambda e: e.tensor_tensor(out=out, in0=a, in1=b, op=op), R_, W_)

        def ts(out, a, s1, s2, op0, op1, R_, W_, eng="dve"):
            if s2 is None:
                R.op(eng, lambda e: e.tensor_scalar(out=out, in0=a, scalar1=s1, scalar2=None, op0=op0), R_, W_)
            else:
                R.op(eng, lambda e: e.tensor_scalar(out=out, in0=a, scalar1=s1, scalar2=s2, op0=op0, op1=op1), R_, W_)

        def stt(out, a, s, b, op0, op1, R_, W_, eng="dve"):
            R.op(eng, lambda e: e.scalar_tensor_tensor(out=out, in0=a, scalar=s, in1=b, op0=op0, op1=op1), R_, W_)

        def cp(out, in_, R_, W_, eng="dve"):
            R.op(eng, lambda e: e.tensor_copy(out=out, in_=in_), R_, W_)

        def scan(out, d0, d1, init, R_, W_):
            R.op("dve", lambda e: e.tensor_tensor_scan(out=out, data0=d0, data1=d1, initial=init,
                                                      op0=ALU.mult, op1=ALU.add), R_, W_)

        def mm(out, lhsT, rhs, start, stop, R_, W_):
            R.op("pe", lambda e: e.matmul(out, lhsT=lhsT, rhs=rhs, start=start, stop=stop), R_, W_)

        def tr(out, in_, R_, W_):
            R.op("pe", lambda e: e.transpose(out=out, in_=in_, identity=ident_b.ap), R_, W_)

        def load(dst_buf, dst_ap, src_ap, q="sp", slow=False, R_=()):
            R.dma(q, dst_ap, src_ap, list(R_), [dst_buf], dst_buf, slow=slow)

        def col(v, nt):
            return v.rearrange("(j p) -> p j", p=128)

        ident_b = new("ident", A.bf16(128))
        load(ident_b, ident_b.ap, ident)
        ramp_b = new("ramp", A.f32(L))
        load(ramp_b, ramp_b.ap, ramp)
        nwb = new("nwb", A.f32(D))
        load(nwb, nwb.ap, nmw.partition_broadcast(128))
        flag_b = new("flag", A.f32(NPB + 1))
        load(flag_b, flag_b.ap, flagv)
        keep_b = new("keep", A.f32(NPB))
        load(keep_b, keep_b.ap, keepv)

        prm = new("prm", A.f32(12 * 8))
        pv = prm.ap
        cw = pv[:, 0:48].rearrange("p (k j) -> p k j", k=4)
        for k in range(4):
            load(prm, cw[:, k, :], col(conv_w[k], 12), slow=True)
        cb = pv[:, 48:60]
        ba = pv[:, 60:72]
        bx = pv[:, 72:84]
        lamt = pv[:, 84:96]
        load(prm, cb, col(conv_b, 12), slow=True)
        load(prm, ba, col(b_a, 12), slow=True)
        load(prm, bx, col(b_x, 12), slow=True)
        load(prm, lamt, col(lru_lam, 12), slow=True)
        cexp = new("cexp", A.f32(12))
        tmp12 = new("tmp12", A.f32(12))
        act(tmp12.ap, lamt, AF.Exp, [prm], [tmp12], scale=-1.0)
        act(tmp12.ap, tmp12.ap, AF.Ln, [tmp12], [tmp12], bias=1.0)
        ts(cexp.ap, tmp12.ap, -8.0, None, ALU.mult, None, [tmp12], [cexp])
        hprm = new("hprm", A.f32(36))
        hba = hprm.ap[:, 0:12]
        hbx = hprm.ap[:, 12:24]
        hce = hprm.ap[:, 24:36]
        ts(hba, ba, 0.5, None, ALU.mult, None, [prm], [hprm])
        ts(hbx, bx, 0.5, None, ALU.mult, None, [prm], [hprm])
        ts(hce, cexp.ap, 0.5, None, ALU.mult, None, [cexp], [hprm])

        Wa = new("Wa", A.bf16(12 * 128))
        Wx = new("Wx", A.bf16(12 * 128))
        Wa3 = Wa.ap.rearrange("p (h j) -> p h j", h=12)
        Wx3 = Wx.ap.rearrange("p (h j) -> p h j", h=12)
        load(Wa, Wa3, w_a.rearrange("h i j -> i h j"), q="pool")
        load(Wx, Wx3, w_x.rearrange("h i j -> i h j"), q="pool")

        s5p = new("s5p", A.f32(16 * 3))
        lre = s5p.ap[:, 0:16]
        lim = s5p.ap[:, 16:32]
        stp = s5p.ap[:, 32:48]
        load(s5p, lre, lam_re.rearrange("(i g) p -> (g p) i", g=2), slow=True)
        load(s5p, lim, lam_im.rearrange("(i g) p -> (g p) i", g=2), slow=True)
        lst2 = lstep.rearrange("(i g) -> g i", g=2)
        for g2 in range(2):
            load(s5p, stp[g2 * 64:(g2 + 1) * 64, :], lst2[g2].partition_broadcast(64), slow=True)
        s5q = new("s5q", A.f32(16 * 12))
        q_ = s5q.ap

        def qv(k):
            return q_[:, 16 * k:16 * (k + 1)]
        stepv, rho, th, cos1, sin1, cr, ci, t0, t1_, t2_, ArT, AiT = [qv(k) for k in range(12)]
        act(stepv, stp, AF.Exp, [s5p], [s5q])
        tt(th, lim, stepv, ALU.mult, [s5p, s5q], [s5q])
        tt(t0, lre, stepv, ALU.mult, [s5p, s5q], [s5q])
        act(rho, t0, AF.Exp, [s5q], [s5q])
        TWO_PI = 2.0 * math.pi
        scs = new("scs", A.f32(48))
        tsm = scs.ap[:, 0:16]
        tsm2 = scs.ap[:, 16:32]
        tsi = scs.ap[:, 32:48].bitcast(mybir.dt.int32)
        SC = [scs]
        for (dst_, ph) in ((sin1, 0.0), (cos1, 0.25)):
            ts(tsm, th, 1.0 / TWO_PI, None, ALU.mult, None, [s5q], SC)
            if ph != 0.0:
                ts(tsm, tsm, ph, None, ALU.add, None, SC, SC)
            cp(tsi, tsm, SC, SC)
            cp(tsm2, tsi, SC, SC)
            tt(tsm, tsm, tsm2, ALU.subtract, SC, SC)
            ts(tsm2, tsm, 0.5, None, ALU.is_gt, None, SC, SC)
            tt(tsm, tsm, tsm2, ALU.subtract, SC, SC)
            ts(tsm2, tsm, -0.5, None, ALU.is_lt, None, SC, SC)
            tt(tsm, tsm, tsm2, ALU.add, SC, SC)
            act(dst_, tsm, AF.Sin, SC, [s5q], scale=TWO_PI)
        brm = new("brm", A.f32(16 * 4))
        brm1, bim, den, tq = [brm.ap[:, 16 * k:16 * (k + 1)] for k in range(4)]
        tt(brm1, rho, cos1, ALU.mult, [s5q], [brm])
        ts(brm1, brm1, -1.0, None, ALU.add, None, [brm], [brm])
        tt(bim, rho, sin1, ALU.mult, [s5q], [brm])
        tt(den, lre, lre, ALU.mult, [s5p], [brm])
        tt(tq, lim, lim, ALU.mult, [s5p], [brm])
        tt(den, den, tq, ALU.add, [brm], [brm])
        R.op("dve", lambda e: e.reciprocal(out=den, in_=den), [brm], [brm])
        tt(cr, brm1, lre, ALU.mult, [brm, s5p], [s5q])
        tt(tq, bim, lim, ALU.mult, [brm, s5p], [brm])
        tt(cr, cr, tq, ALU.add, [s5q, brm], [s5q])
        tt(cr, cr, den, ALU.mult, [s5q, brm], [s5q])
        tt(ci, bim, lre, ALU.mult, [brm, s5p], [s5q])
        tt(tq, brm1, lim, ALU.mult, [brm, s5p], [brm])
        tt(ci, ci, tq, ALU.subtract, [s5q, brm], [s5q])
        tt(ci, ci, den, ALU.mult, [s5q, brm], [s5q])
        Bq = new("Bq", A.f32(16 * 16 * 2))
        Bqr = Bq.ap[:, 0:256].rearrange("p (i c) -> p i c", i=16)
        Bqi = Bq.ap[:, 256:512].rearrange("p (i c) -> p i c", i=16)
        for g2 in range(2):
            load(Bq, Bqr[g2 * 64:(g2 + 1) * 64], b_re.rearrange("(i g) p c -> g p i c", g=2)[g2], slow=True)
            load(Bq, Bqi[g2 * 64:(g2 + 1) * 64], b_im.rearrange("(i g) p c -> g p i c", g=2)[g2], slow=True)
        bb = new("bb", A.f32(16 * 16 * 2))
        bbr = bb.ap[:, 0:256].rearrange("p (i c) -> p i c", i=16)
        bbi = bb.ap[:, 256:512].rearrange("p (i c) -> p i c", i=16)
        tq16 = new("tq16", A.f32(16))
        for i in range(16):
            ts(tq16.ap, Bqi[:, i, :], ci[:, i:i + 1], None, ALU.mult, None, [Bq, s5q], [tq16])
            stt(bbr[:, i, :], Bqr[:, i, :], cr[:, i:i + 1], tq16.ap, ALU.mult, ALU.subtract, [Bq, s5q, tq16], [bb])
            ts(tq16.ap, Bqr[:, i, :], ci[:, i:i + 1], None, ALU.mult, None, [Bq, s5q], [tq16])
            stt(bbi[:, i, :], Bqi[:, i, :], cr[:, i:i + 1], tq16.ap, ALU.mult, ALU.add, [Bq, s5q, tq16], [bb])

        CqB = [[None, None], [None, None]]
        CqV = [[None, None], [None, None]]
        for ri, csrc in enumerate((c_re, c_im)):
            for g2 in range(2):
                cb_ = new("Cq%d%d" % (ri, g2), A.f32(256))
                cv_ = cb_.ap.rearrange("p (i c) -> p i c", i=16)
                CqB[ri][g2] = cb_
                CqV[ri][g2] = cv_
                for i in range(16):
                    load(cb_, cv_[g2 * 64:(g2 + 1) * 64, i, :], csrc[2 * i + g2].rearrange("c p -> p c"), slow=True)

        sst = new("sst", A.f32(32))
        hst = new("hst", A.f32(12))
        pst = new("pst", A.f32(12))
        hal = new("hal", A.f32(36))
        hal3 = hal.ap.rearrange("p (j c) -> p j c", j=12)
        R.op("dve", lambda e: e.memset(sst.ap, 0.0), [], [sst])
        R.op("dve", lambda e: e.memset(hst.ap, 0.0), [], [hst])

        R.op("dve", lambda e: e.memset(hal.ap, 0.0), [], [hal])

        def norm_tile(src_ap, nwbuf, ns):
            norm_a(src_ap, ns)
            norm_b(nwbuf, ns)

        def norm_a(src_ap, ns):
            xt, hb, ssb = ns.xt, ns.hb, ns.ssb
            load(xt, xt.ap, src_ap)
            act(hb.ap, xt.ap, AF.Square, [xt], [hb, ssb], accum=ssb.ap[:, 0:1])

        def norm_b(nwbuf, ns):
            xt, hb, ssb = ns.xt, ns.hb, ns.ssb
            ts(ssb.ap[:, 1:2], ssb.ap[:, 0:1], 1.0 / D, EPS, ALU.mult, ALU.add, [ssb], [ssb])
            act(ssb.ap[:, 1:2], ssb.ap[:, 1:2], AF.Sqrt, [ssb], [ssb])
            R.op("dve", lambda e, o=ssb.ap[:, 1:2]: e.reciprocal(out=o, in_=o), [ssb], [ssb])
            stt(hb.ap, xt.ap, ssb.ap[:, 1:2], nwbuf.ap, ALU.mult, ALU.mult, [xt, ssb, nwbuf], [hb])

        def transpose_rows(srcb, dstb, dst3, col0, ncol=128):
            for half in range(2):
                pb = next_ps()
                pv_ = pb.ap.bitcast(BF16).rearrange("p (k t) -> p k t", k=8)
                for kk in range(8):
                    k = half * 8 + kk
                    tr(pv_[:, kk, :], srcb.ap[:, k * 128:(k + 1) * 128], [srcb, ident_b], [pb])
                eng = "act" if half == 0 else "dve"
                if eng == "act":
                    R.op("act", lambda e, o=dst3[:, half * 8:(half + 1) * 8, col0:col0 + ncol], i_=pv_[:, :, 0:ncol]:
                         e.activation(out=o, in_=i_, func=AF.Copy), [pb], [dstb])
                else:
                    cp(dst3[:, half * 8:(half + 1) * 8, col0:col0 + ncol], pv_[:, :, 0:ncol], [pb], [dstb])

        def lru_tile(fcol, j, px, py, ws):
            lru_s1(j, px, ws)
            lru_s1b(ws)
            lru_s2(fcol, j, py, ws)

        def lru_s1b(ws):
            cp(ws.xcb.ap, ws.xc.ap, [ws.xc], [ws.xcb])

        def lru_s1(j, px, ws, ceng="dve"):
            xl, xc, xcb = ws.xl, ws.xc, ws.xcb
            R.op("act", lambda e, o=xl.ap[:, 3:3 + TB], i_=px.ap: e.activation(out=o, in_=i_, func=AF.Copy), [px], [xl])
            cp(xl.ap[:, 0:3], hal3[:, j, :], [hal], [xl])
            cp(hal3[:, j, :], xl.ap[:, TB:TB + 3], [xl], [hal])
            if ceng == "pool":
                ct_ = ws.ctmp
                ts(xc.ap, xl.ap[:, 3:3 + TB], cw[:, 3, j:j + 1], cb[:, j:j + 1], ALU.mult, ALU.add, [xl, prm], [xc], eng="pool")
                ts(ct_.ap, xl.ap[:, 2:2 + TB], cw[:, 2, j:j + 1], None, ALU.mult, None, [xl, prm], [ct_], eng="pool")
                tt(xc.ap, xc.ap, ct_.ap, ALU.add, [xc, ct_], [xc], eng="pool")
                for k in range(2):
                    stt(xc.ap, xl.ap[:, k:k + TB], cw[:, k, j:j + 1], xc.ap, ALU.mult, ALU.add, [xl, prm, xc], [xc])
            else:
                ts(xc.ap, xl.ap[:, 3:3 + TB], cw[:, 3, j:j + 1], cb[:, j:j + 1], ALU.mult, ALU.add, [xl, prm], [xc])
                for k in range(3):
                    stt(xc.ap, xl.ap[:, k:k + TB], cw[:, k, j:j + 1], xc.ap, ALU.mult, ALU.add, [xl, prm, xc], [xc])

        def lru_s2(fcol, j, py, ws):
            xl, xc, xcb, rg, ig, at, mt, bt, ht, gy, tiny = (ws.xl, ws.xc, ws.xcb, ws.rg, ws.ig, ws.at, ws.mt,
                                                              ws.bt, ws.ht, ws.gy, ws.tiny)
            pr_ = next_ps()
            pi_ = next_ps()
            mm(pr_.ap, Wa3[:, j, :], xcb.ap, True, True, [Wa, xcb], [pr_])
            mm(pi_.ap, Wx3[:, j, :], xcb.ap, True, True, [Wx, xcb], [pi_])
            act(rg.ap, pr_.ap, AF.Tanh, [pr_, hprm], [rg], bias=hba[:, j:j + 1], scale=0.5)
            act(ig.ap, pi_.ap, AF.Tanh, [pi_, hprm], [ig], bias=hbx[:, j:j + 1], scale=0.5)
            act(at.ap, rg.ap, AF.Exp, [rg, hprm], [at], bias=hce[:, j:j + 1], scale=hce[:, j:j + 1])
            act(mt.ap, rg.ap, AF.Exp, [rg, cexp], [mt], bias=cexp.ap[:, j:j + 1], scale=cexp.ap[:, j:j + 1])
            act(mt.ap, mt.ap, AF.Sqrt, [mt], [mt], bias=0.25, scale=-0.25)
            if fcol is not None:
                ts(tiny.ap[:, 2:3], mt.ap[:, 0:1], -1.0, 0.5, ALU.mult, ALU.add, [mt], [tiny])
                stt(mt.ap[:, 0:1], tiny.ap[:, 2:3], flag_b.ap[:, fcol:fcol + 1], mt.ap[:, 0:1], ALU.mult, ALU.add, [tiny, flag_b, mt], [mt])
            tt(bt.ap, xc.ap, mt.ap, ALU.mult, [xc, mt], [bt])
            stt(bt.ap, ig.ap, 1.0, bt.ap, ALU.add, ALU.mult, [ig, bt], [bt])
            scan(ht.ap, at.ap, bt.ap, hst.ap[:, j:j + 1], [at, bt, hst], [ht])
            cp(hst.ap[:, j:j + 1], ht.ap[:, TB - 1:TB], [ht], [hst])
            if py is not None:
                act(gy.ap, py.ap, AF.Gelu_apprx_tanh, [py], [gy])
                tt(ol3[:, j, :], ht.ap, gy.ap, ALU.mult, [ht, gy], [out_lru])

        mark = A.off

        class _WS:
            pass
        revb = new("revb", A.f32(1))
        load(revb, revb.ap, revc)
        lam128 = new("lam128", A.f32(32))
        lr128 = lam128.ap[:, 0:16]
        li128 = lam128.ap[:, 16:32]
        tt(lr128, rho, cos1, ALU.mult, [s5q], [lam128])
        tt(li128, rho, sin1, ALU.mult, [s5q], [lam128])
        for _ in range(7):
            tt(t1_, lr128, lr128, ALU.mult, [lam128], [s5q])
            tt(t2_, li128, li128, ALU.mult, [lam128], [s5q])
            tt(li128, lr128, li128, ALU.mult, [lam128], [lam128])
            ts(li128, li128, 2.0, None, ALU.mult, None, [lam128], [lam128])
            tt(lr128, t1_, t2_, ALU.subtract, [s5q], [lam128])
        bbp = new("bbp", A.f32(2 * 512))
        bbpr = bbp.ap[:, 0:512]
        bbpi = bbp.ap[:, 512:1024]
        bbpr3 = bbpr.rearrange("p (i c) -> p i c", i=16)
        bbpi3 = bbpi.rearrange("p (i c) -> p i c", i=16)
        R.op("dve", lambda e: e.memset(bbp.ap, 0.0), [], [bbp])
        cp(bbpr3[0:64, :, 0:16], bbr[0:64], [bb], [bbp])
        cp(bbpr3[64:128, :, 16:32], bbr[64:128], [bb], [bbp])
        cp(bbpi3[0:64, :, 0:16], bbi[0:64], [bb], [bbp])
        cp(bbpi3[64:128, :, 16:32], bbi[64:128], [bb], [bbp])
        WresB = []
        Wres_ap = A.bf16(16 * 2048)
        Wres3 = Wres_ap.rearrange("p (k n) -> p k n", k=16)
        for c4 in range(4):
            wb = new("Wres%d" % c4, Wres3[:, :, c4 * 512:(c4 + 1) * 512])
            R.dma("pool", wb.ap, w_in[:, c4 * 512:(c4 + 1) * 512].rearrange("(k p) n -> p k n", p=128), [], [wb], wb)
            WresB.append(wb)
        GtR = new("GtR", A.bf16(2048))
        GtI = new("GtI", A.bf16(2048))
        hTps = [new("hTp%d" % i, A.bf16(16 * TB)) for i in range(2)]
        hTp3s = [h_.ap.rearrange("p (k t) -> p k t", k=16) for h_ in hTps]
        sq1 = new("sq1", A.f32(TB))
        sq2 = new("sq2", A.f32(TB))
        nsPs = []
        for si in range(2):
            n_ = _WS()
            n_.xt = new("xtP%d" % si, A.f32(D))
            n_.hb = new("hbP%d" % si, A.bf16(D))
            n_.ssb = new("ssbP%d" % si, A.f32(2))
            nsPs.append(n_)
        utok = [new("utok%d" % i, A.bf16(512)) for i in range(2)]
        zt = new("zt", A.f32(16 * 6))
        zre, zim, za, zb, zc, zd = [zt.ap[:, 16 * k:16 * (k + 1)] for k in range(6)]
        wsP = []
        for si in range(2):
            w = _WS()
            w.xl = new("xlP%d" % si, A.f32(3 + TB))
            for nm in ("xc", "rg", "ig", "at", "mt", "bt", "ht"):
                setattr(w, nm, new(nm + "P%d" % si, A.f32(TB)))
            w.gy = None
            w.xcb = new("xcbP%d" % si, A.bf16(TB))
            w.ctmp = new("ctmpP%d" % si, A.f32(TB))
            w.tiny = new("tinyP%d" % si, A.f32(8))
            wsP.append(w)
        wA = wsP[0]
        wB = wsP[1]
        lim_f = lam_im.rearrange("g p -> (g p)")
        lre_f = lam_re.rearrange("g p -> (g p)")
        INV2PI = 1.0 / (2.0 * math.pi)
        st8 = new("st8", A.f32(32))
        load(st8, st8.ap, lstep.partition_broadcast(128))
        act(st8.ap, st8.ap, AF.Exp, [st8], [st8])
        for ch in range(4):
            ssl = slice(ch * 512, (ch + 1) * 512)
            load(wA.xc, wA.xc.ap, lim_f[ssl].partition_broadcast(128))
            load(wA.rg, wA.rg.ap, lre_f[ssl].partition_broadcast(128))
            cp(wA.ig.ap.rearrange("p (g q) -> p g q", g=8),
               st8.ap[:, ch * 8:(ch + 1) * 8].unsqueeze(2).to_broadcast([128, 8, 64]), [st8], [wA.ig])
            tt(wA.xc.ap, wA.xc.ap, wA.ig.ap, ALU.mult, [wA.xc, wA.ig], [wA.xc])
            tt(wA.rg.ap, wA.rg.ap, wA.ig.ap, ALU.mult, [wA.rg, wA.ig], [wA.rg])
            act(wA.at.ap, wA.rg.ap, AF.Exp, [wA.rg, revb], [wA.at], scale=revb.ap[:, 0:1])
            for (ph, dstb) in ((0.0, GtI), (0.25, GtR)):
                tn, tn2, tni = wB.xc, wB.rg, wB.ig
                ts(tn.ap, wA.xc.ap, revb.ap[:, 0:1], INV2PI, ALU.mult, ALU.mult, [wA.xc, revb], [tn])
                if ph != 0.0:
                    ts(tn.ap, tn.ap, ph, None, ALU.add, None, [tn], [tn])
                tni_ap = tni.ap.bitcast(mybir.dt.int32)
                cp(tni_ap, tn.ap, [tn], [tni])
                cp(tn2.ap, tni_ap, [tni], [tn2])
                tt(tn.ap, tn.ap, tn2.ap, ALU.subtract, [tn, tn2], [tn])
                ts(tn2.ap, tn.ap, 0.5, None, ALU.is_gt, None, [tn], [tn2])
                tt(tn.ap, tn.ap, tn2.ap, ALU.subtract, [tn, tn2], [tn])
                ts(tn2.ap, tn.ap, -0.5, None, ALU.is_lt, None, [tn], [tn2])
                tt(tn.ap, tn.ap, tn2.ap, ALU.add, [tn, tn2], [tn])
                act(wB.at.ap, tn.ap, AF.Sin, [tn], [wB.at], scale=2.0 * math.pi)
                tt(dstb.ap[:, ssl], wB.at.ap, wA.at.ap, ALU.mult, [wB.at, wA.at], [dstb])

        sst2p = sst.ap.rearrange("p (i c) -> p i c", c=2)
        F_re = sst2p[:, :, 0]
        F_im = sst2p[:, :, 1]
        ZT = [zt]
        ps_pool[0] = [0, 1, 2, 3, 4]

        def pnorm(pb_, it_):
            pnormA(pb_, it_)
            pnormT(pb_, it_)

        def pnormA(pb_, it_):
            n_ = nsPs[it_ % 2]
            norm_tile(xp[pb_ * TB + it_ * 128:pb_ * TB + (it_ + 1) * 128, :], nwb, n_)

        def pnormT(pb_, it_):
            n_ = nsPs[it_ % 2]
            transpose_rows(n_.hb, hTps[pb_ % 2], hTp3s[pb_ % 2], it_ * 128)

        def s5p_a1(hTp, hTp3, it):
            pu = ps[5]
            for k in range(16):
                mm(pu.ap, hTp3[:, k, it * 128:(it + 1) * 128], Wres3[:, k, 0:512], k == 0, k == 15, [hTp, WresB[0]], [pu])
            ub = utok[it % 2]
            R.op("act", lambda e, o=ub.ap, i_=pu.ap: e.activation(out=o, in_=i_, func=AF.Copy), [pu], [ub])

        def s5p_a2(it):
            ub = utok[it % 2]
            pvr = ps[6]
            pvi = ps[7]
            for i in range(16):
                mm(pvr.ap[:, 32 * i:32 * i + 32], GtR.ap[:, 128 * i:128 * i + 128], ub.ap[:, 32 * i:32 * i + 32], True, True, [GtR, ub], [pvr])
            for i in range(16):
                mm(pvi.ap[:, 32 * i:32 * i + 32], GtI.ap[:, 128 * i:128 * i + 128], ub.ap[:, 32 * i:32 * i + 32], True, True, [GtI, ub], [pvi])

        def s5p_b():
            pvr = ps[6]
            pvi = ps[7]
            q1, q2 = sq1, sq2
            tt(q1.ap, pvr.ap, bbpr, ALU.mult, [pvr, bbp], [q1])
            tt(q2.ap, pvi.ap, bbpi, ALU.mult, [pvi, bbp], [q2])
            tt(q1.ap, q1.ap, q2.ap, ALU.subtract, [q1, q2], [q1])
            R.op("dve", lambda e, o=zre, i_=q1.ap.rearrange("p (i c) -> p i c", i=16): e.tensor_reduce(out=o, in_=i_, axis=AX.X, op=ALU.add), [q1], ZT)
            tt(q1.ap, pvi.ap, bbpr, ALU.mult, [pvi, bbp], [q1])
            tt(q2.ap, pvr.ap, bbpi, ALU.mult, [pvr, bbp], [q2])
            tt(q1.ap, q1.ap, q2.ap, ALU.add, [q1, q2], [q1])
            R.op("dve", lambda e, o=zim, i_=q1.ap.rearrange("p (i c) -> p i c", i=16): e.tensor_reduce(out=o, in_=i_, axis=AX.X, op=ALU.add), [q1], ZT)
            tt(za, lr128, F_re, ALU.mult, [lam128, sst], ZT)
            tt(zb, li128, F_im, ALU.mult, [lam128, sst], ZT)
            tt(za, za, zb, ALU.subtract, ZT, ZT)
            tt(za, za, zre, ALU.add, ZT, ZT)
            tt(zc, lr128, F_im, ALU.mult, [lam128, sst], ZT)
            tt(zd, li128, F_re, ALU.mult, [lam128, sst], ZT)
            tt(zc, zc, zd, ALU.add, ZT, ZT)
            tt(zc, zc, zim, ALU.add, ZT, ZT)
            cp(F_re, za, ZT, [sst])
            cp(F_im, zc, ZT, [sst])

        for it in range(4):
            pnorm(0, it)
        for pb in range(NPB):
            fcol = pb if pb % 4 == 0 else None
            hTp, hTp3 = hTps[pb % 2], hTp3s[pb % 2]
            side = {0: [("a1", 0)], 1: [("a2", 0), ("nA", 0)], 2: [("b", 0), ("a1", 1), ("nT", 0)],
                    3: [("a2", 1)], 4: [("b", 1), ("a1", 2), ("nA", 1)], 5: [("a2", 2), ("nT", 1)],
                    6: [("b", 2), ("a1", 3)], 7: [("a2", 3), ("nA", 2)], 8: [("b", 3), ("nT", 2)],
                    9: [], 10: [("nA", 3)], 11: [("nT", 3)]}
            for j in range(12):
                px = next_ps()
                cs_ = slice(512 + 128 * j, 512 + 128 * (j + 1))
                for k in range(16):
                    mm(px.ap, Wres3[:, k, cs_], hTp3[:, k, :], k == 0, k == 15, [WresB[1 + j // 4], hTp], [px])
                lru_s1(j, px, wsP[j % 2])
                lru_s1b(wsP[j % 2])
                if j > 0:
                    lru_s2(fcol, j - 1, None, wsP[(j - 1) % 2])
                for kind, idx in side[j]:
                    if kind == "a1":
                        s5p_a1(hTp, hTp3, idx)
                    elif kind == "a2":
                        s5p_a2(idx)
                    elif kind == "b":
                        s5p_b()
                    elif pb + 1 < NPB:
                        if kind == "nA":
                            pnormA(pb + 1, idx)
                        else:
                            pnormT(pb + 1, idx)
            lru_s2(fcol, 11, None, wsP[1])
            kc = keep_b.ap[:, pb:pb + 1]
            ts(sst.ap, sst.ap, kc, None, ALU.mult, None, [sst, keep_b], [sst])
            ts(hst.ap, hst.ap, kc, None, ALU.mult, None, [hst, keep_b], [hst])
            ts(hal.ap, hal.ap, kc, None, ALU.mult, None, [hal, keep_b], [hal])
        ps_pool[0] = list(range(8))

        R.barrier()
        print("prefix arena hi", A.hi)
        global _DBG_A
        _DBG_A = A
        A.off = mark
        A.hi = mark
        Ecos = new("Ecos", A.f32(16 * L))
        Esin = new("Esin", A.f32(16 * L))
        Ec3 = Ecos.ap.rearrange("p (i l) -> p i l", i=16)
        Es3 = Esin.ap.rearrange("p (i l) -> p i l", i=16)
        targ = new("targ", A.f32(L))
        targ2 = new("targ2", A.f32(L))
        targi = new("targi", A.f32(L).bitcast(mybir.dt.int32))
        for i in range(16):
            for (tab, tabb, ph) in ((Es3, Esin, 0.0), (Ec3, Ecos, 0.25)):
                ts(targ.ap, ramp_b.ap, th[:, i:i + 1], 1.0 / TWO_PI, ALU.mult, ALU.mult, [ramp_b, s5q], [targ])
                if ph != 0.0:
                    ts(targ.ap, targ.ap, ph, None, ALU.add, None, [targ], [targ])
                cp(targi.ap, targ.ap, [targ], [targi])
                cp(targ2.ap, targi.ap, [targi], [targ2])
                tt(targ.ap, targ.ap, targ2.ap, ALU.subtract, [targ, targ2], [targ])
                ts(targ2.ap, targ.ap, 0.5, None, ALU.is_gt, None, [targ], [targ2])
                tt(targ.ap, targ.ap, targ2.ap, ALU.subtract, [targ, targ2], [targ])
                ts(targ2.ap, targ.ap, -0.5, None, ALU.is_lt, None, [targ], [targ2])
                tt(targ.ap, targ.ap, targ2.ap, ALU.add, [targ, targ2], [targ])
                act(tab[:, i, :], targ.ap, AF.Sin, [targ], [tabb], scale=TWO_PI)
        kk = new("kk", A.f32(64))
        K1 = kk.ap[:, 0:32].rearrange("p (i c) -> p i c", c=2)
        K2 = kk.ap[:, 32:64].rearrange("p (i c) -> p i c", c=2)
        cp(K1[:, :, 0], Ec3[:, :, L - 1], [Ecos], [kk])
        ts(K1[:, :, 1], Es3[:, :, L - 1], -1.0, None, ALU.mult, None, [Esin], [kk])
        cp(K2[:, :, 0], Es3[:, :, L - 1], [Esin], [kk])
        cp(K2[:, :, 1], Ec3[:, :, L - 1], [Ecos], [kk])
        fj = new("fj", A.f32(4))
        BtR = new("BtR", A.bf16(16 * 128))
        BtI = new("BtI", A.bf16(16 * 128))
        BtR3 = BtR.ap.rearrange("p (i q) -> p i q", i=16)
        BtI3 = BtI.ap.rearrange("p (i q) -> p i q", i=16)
        Sst = new("Sst", A.bf16(128))
        for i in range(16):
            base = (i % 4) * 32
            for (src, dstb, dst3) in ((bbr, BtR, BtR3), (bbi, BtI, BtI3)):
                R.op("dve", lambda e: e.memset(Sst.ap, 0.0), [], [Sst])
                cp(Sst.ap[0:64, base:base + 16], src[0:64, i, :], [bb], [Sst])
                cp(Sst.ap[64:128, base + 16:base + 32], src[64:128, i, :], [bb], [Sst])
                pb = next_ps()
                pbv = pb.ap.bitcast(BF16)[:, 0:128]
                tr(pbv, Sst.ap, [Sst, ident_b], [pb])
                cp(dst3[:, i, :], pbv, [pb], [dstb])
        if full:
            bgt = new("bgt", A.f32(32 + 4 + 4))
            load(bgt, bgt.ap[:, 0:32], col(b_gate, 32), slow=True)
            s5dt = bgt.ap[:, 32:36]
            bglt = bgt.ap[:, 36:40]
            load(bgt, s5dt, col(s5_d, 4), slow=True)
            load(bgt, bglt, col(b_glu, 4), slow=True)
            Wglu = new("Wglu", A.bf16(4 * 512))
            Wglu3 = Wglu.ap.rearrange("p (k n) -> p k n", k=4)
            load(Wglu, Wglu3, w_glu.rearrange("(k p) n -> p k n", p=128), q="pool")
            CtR = new("CtR", A.bf16(16 * 128))
            CtI = new("CtI", A.bf16(16 * 128))
            CtR3 = CtR.ap.rearrange("p (i q) -> p i q", i=16)
            CtI3 = CtI.ap.rearrange("p (i q) -> p i q", i=16)
            R.op("dve", lambda e: e.memset(CtR.ap, 0.0), [], [CtR])
            R.op("dve", lambda e: e.memset(CtI.ap, 0.0), [], [CtI])
            for i in range(16):
                base = (i % 4) * 32
                for g2 in range(2):
                    cs_ = slice(base + 16 * g2, base + 16 * g2 + 16)
                    ps_ = slice(g2 * 64, (g2 + 1) * 64)
                    cp(CtR3[ps_, i, cs_], CqV[0][g2][ps_, i, :], [CqB[0][g2]], [CtR])
                    cp(CtI3[ps_, i, cs_], CqV[1][g2][ps_, i, :], [CqB[1][g2]], [CtI])

        hT = new("hT", A.bf16(16 * TB))
        hT3 = hT.ap.rearrange("p (k t) -> p k t", k=16)
        uT = new("uT", A.bf16(4 * TB))
        uT3 = uT.ap.rearrange("p (k t) -> p k t", k=4)
        NSLOT = 3
        ring = [new("ring%d" % s, A.bf16(16 * 512)) for s in range(NSLOT)]
        ring_h = []
        for s in range(NSLOT):
            r3 = ring[s].ap.rearrange("p (k n) -> p k n", k=16)
            ring_h.append(r3)
        rr = [0]

        def next_slot():
            s = rr[0] % NSLOT
            rr[0] += 1
            return ring[s], ring_h[s]

        def w_in_cols(c0, n):
            return w_in[:, c0:c0 + n].rearrange("(k p) n -> p k n", p=128)

        xt = new("xt", A.f32(D))
        hb = new("hb", A.bf16(D))
        junk = hb
        ssb = new("ssb", A.f32(2))
        wk0 = A.off
        xl = new("xl", A.f32(3 + TB))
        xc = new("xc", A.f32(TB))
        xcb = new("xcb", A.bf16(TB))
        rg = new("rg", A.f32(TB))
        ig = new("ig", A.f32(TB))
        at = new("at", A.f32(TB))
        mtbt = new("mtbt", A.f32(2 * TB))
        mt = Buf("mt", mtbt.ap[:, 0:TB], root=mtbt)
        bt = Buf("bt", mtbt.ap[:, TB:2 * TB], root=mtbt)
        w2v = mtbt.ap.rearrange("p (c t) -> p c t", c=2)
        ht = new("ht", A.f32(TB))
        gy = new("gy", A.f32(TB))
        A.off = wk0
        f1, f2, f3, f4, wre_b, wim_b = xc, rg, ig, at, mt, bt
        A.off = A.hi
        tiny = new("tiny", A.f32(8))
        if full:
            sre = new("sre", A.bf16(TB))
            sim = new("sim", A.bf16(TB))
            yv = ht
            ygT = new("ygT", A.bf16(4 * TB))
            ygT3 = ygT.ap.rearrange("p (k t) -> p k t", k=4)
            glt = gy
            out_s5 = new("out_s5", A.bf16(4 * TB))
            os53 = out_s5.ap.rearrange("p (k t) -> p k t", k=4)
            out_lru = new("out_lru", A.bf16(12 * TB))
            ol3 = out_lru.ap.rearrange("p (k t) -> p k t", k=12)
            mergedT = new("mergedT", A.bf16(16 * TB))
            mg3 = mergedT.ap.rearrange("p (k t) -> p k t", k=16)
            g1 = xc
            g2b = rg
            xr = [ig, at]

        class _NS:
            pass
        ns0 = _NS()
        ns0.xt, ns0.hb, ns0.ssb = xt, hb, ssb
        ws0 = _NS()
        (ws0.xl, ws0.xc, ws0.xcb, ws0.rg, ws0.ig, ws0.at, ws0.mt, ws0.bt, ws0.ht, ws0.gy, ws0.tiny) = (
            xl, xc, xcb, rg, ig, at, mt, bt, ht, gy, tiny)
        ws1 = _NS()
        (ws1.rg, ws1.ig, ws1.at, ws1.mt, ws1.bt, ws1.ht, ws1.gy, ws1.tiny) = (rg, ig, at, mt, bt, ht, gy, tiny)
        ws1.xl = new("xl1", A.f32(3 + TB))
        ws1.xc = new("xc1", A.f32(TB))
        ws1.xcb = new("xcb1", A.bf16(TB))
        wsO = [ws0, ws1]

        def s5_block(do_out):
            for i in range(16):
                pr = next_ps()
                pi = next_ps()
                mm(pr.ap, BtR3[:, i, :], uT3[:, i // 4, :], True, True, [BtR, uT], [pr])
                mm(pi.ap, BtI3[:, i, :], uT3[:, i // 4, :], True, True, [BtI, uT], [pi])
                ec = Ec3[:, i:i + 1, :].to_broadcast([128, TB // L, L])
                es_ = Es3[:, i:i + 1, :].to_broadcast([128, TB // L, L])

                def v4(ap):
                    return ap.rearrange("p (b l) -> p b l", l=L)
                tt(v4(f1.ap), v4(pr.ap), ec, ALU.mult, [pr, Ecos], [f1])
                tt(v4(f2.ap), v4(pi.ap), es_, ALU.mult, [pi, Esin], [f2])
                tt(f1.ap, f1.ap, f2.ap, ALU.add, [f1, f2], [f1])
                tt(v4(f3.ap), v4(pi.ap), ec, ALU.mult, [pi, Ecos], [f3])
                tt(v4(f4.ap), v4(pr.ap), es_, ALU.mult, [pr, Esin], [f4])
                tt(f3.ap, f3.ap, f4.ap, ALU.subtract, [f3, f4], [f3])
                s_re = sst.ap[:, 2 * i:2 * i + 1]
                s_im = sst.ap[:, 2 * i + 1:2 * i + 2]
                rb = rho[:, i:i + 1].to_broadcast([128, L])
                for b in range(TB // L):
                    sl_ = slice(b * L, (b + 1) * L)
                    scan(wre_b.ap[:, sl_], rb, f1.ap[:, sl_], s_re, [s5q, f1, sst], [wre_b])
                    scan(wim_b.ap[:, sl_], rb, f3.ap[:, sl_], s_im, [s5q, f3, sst], [wim_b])
                    last2 = w2v[:, :, (b + 1) * L - 1]
                    stt(fj.ap[:, 0:2], last2, 1.0, K1[:, i, :], ALU.mult, ALU.mult, [mtbt, kk], [fj, sst])
                    R.ops[-1].fn = (lambda e, o=fj.ap[:, 0:2], a_=last2, k_=K1[:, i, :], acc_=s_re:
                                    e.scalar_tensor_tensor(out=o, in0=a_, scalar=1.0, in1=k_, op0=ALU.mult, op1=ALU.mult, accum_out=acc_))
                    stt(fj.ap[:, 2:4], last2, 1.0, K2[:, i, :], ALU.mult, ALU.mult, [mtbt, kk], [fj, sst])
                    R.ops[-1].fn = (lambda e, o=fj.ap[:, 2:4], a_=last2, k_=K2[:, i, :], acc_=s_im:
                                    e.scalar_tensor_tensor(out=o, in0=a_, scalar=1.0, in1=k_, op0=ALU.mult, op1=ALU.mult, accum_out=acc_))
                if do_out:
                    tt(v4(f2.ap), v4(wre_b.ap), ec, ALU.mult, [wre_b, Ecos], [f2])
                    tt(v4(f4.ap), v4(wim_b.ap), es_, ALU.mult, [wim_b, Esin], [f4])
                    tt(sre.ap, f2.ap, f4.ap, ALU.subtract, [f2, f4], [sre])
                    tt(v4(f2.ap), v4(wre_b.ap), es_, ALU.mult, [wre_b, Esin], [f2])
                    tt(v4(f4.ap), v4(wim_b.ap), ec, ALU.mult, [wim_b, Ecos], [f4])
                    stt(sim.ap, f2.ap, -1.0, f4.ap, ALU.mult, ALU.subtract, [f2, f4], [sim])
                    jt = i // 4
                    if i % 4 == 0:
                        s5_block.yc = next_ps()
                    yc = s5_block.yc
                    mm(yc.ap, CtR3[:, i, :], sre.ap, i % 4 == 0, False, [CtR, sre], [yc])
                    mm(yc.ap, CtI3[:, i, :], sim.ap, False, i % 4 == 3, [CtI, sim], [yc])
                    if i % 4 == 3:
                        stt(yv.ap, uT3[:, jt, :], s5dt[:, jt:jt + 1], yc.ap, ALU.mult, ALU.add, [uT, bgt, yc], [yv])
                        act(ygT3[:, jt, :], yv.ap, AF.Gelu_apprx_tanh, [yv], [ygT])
            if do_out:
                for nt_ in range(4):
                    pg = next_ps()
                    for k in range(4):
                        mm(pg.ap, Wglu3[:, k, nt_ * 128:(nt_ + 1) * 128], ygT3[:, k, :], k == 0, k == 3, [Wglu, ygT], [pg])
                    act(glt.ap, pg.ap, AF.Sigmoid, [pg, bgt], [glt], bias=bglt[:, nt_:nt_ + 1])
                    tt(os53[:, nt_, :], ygT3[:, nt_, :], glt.ap, ALU.mult, [ygT, glt], [out_s5])

        def front(src, row0, do_out, fcol):
            for it in range(4):
                norm_tile(src[row0 + it * 128:row0 + (it + 1) * 128, :], nwb, ns0)
                transpose_rows(hb, hT, hT3, it * 128)
            sb, s3 = next_slot()
            load(sb, s3, w_in_cols(0, 512), q="pool")
            for ct in range(4):
                pu = next_ps()
                for k in range(16):
                    mm(pu.ap, s3[:, k, ct * 128:(ct + 1) * 128], hT3[:, k, :], k == 0, k == 15, [sb, hT], [pu])
                R.op("act", lambda e, o=uT3[:, ct, :], i_=pu.ap: e.activation(out=o, in_=i_, func=AF.Copy), [pu], [uT])
            s5_block(do_out)
            pend_l = None
            for q in range(6):
                sb, s3 = next_slot()
                R.dma("pool", s3[:, :, 0:256], w_in_cols(512 + 256 * q, 256), [], [sb], sb)
                if do_out:
                    R.dma("pool", s3[:, :, 256:512], w_in_cols(2048 + 256 * q, 256), [], [sb], sb)
                for jj in range(2):
                    j = 2 * q + jj
                    px = next_ps()
                    for k in range(16):
                        mm(px.ap, s3[:, k, jj * 128:(jj + 1) * 128], hT3[:, k, :], k == 0, k == 15, [sb, hT], [px])
                    py = None
                    if do_out:
                        py = next_ps()
                        for k in range(16):
                            mm(py.ap, s3[:, k, 256 + jj * 128:256 + (jj + 1) * 128], hT3[:, k, :], k == 0, k == 15, [sb, hT], [py])
                    lru_s1(j, px, wsO[j % 2])
                    lru_s1b(wsO[j % 2])
                    if pend_l is not None:
                        lru_s2(fcol, pend_l[0], pend_l[1], wsO[pend_l[0] % 2])
                    pend_l = (j, py)
            lru_s2(fcol, pend_l[0], pend_l[1], wsO[pend_l[0] % 2])

        for tb in range(NB):
            t0_ = tb * TB
            front(x, t0_, True, NPB if tb == 0 else None)
            def unit(uidx, parts):
                sb_, s3_ = next_slot()
                if tb == 0:
                    for (dsl, src) in parts:
                        R.dma("pool", s3_[:, dsl, :], src, [], [sb_], sb_)
                    R.dma("sp", wsc[uidx], sb_.ap, [sb_], [wscB[uidx]], wscB[uidx])
                else:
                    R.dma("sp", sb_.ap, wsc[uidx], [wscB[uidx]], [sb_], sb_)
                return sb_, s3_

            for mg in range(4):
                gsb, gs3 = unit(3 * mg + 0, [(slice(0, 16), w_in_cols(3584 + 512 * mg, 512))])
                glb, gl3 = unit(3 * mg + 1, [(slice(0, 16), w_in_cols(5632 + 512 * mg, 512))])
                prb, pr3 = unit(3 * mg + 2, [
                    (slice(0, 4), wps[:, 512 * mg:512 * (mg + 1)].rearrange("(k p) n -> p k n", p=128)),
                    (slice(4, 16), wpl[:, 512 * mg:512 * (mg + 1)].rearrange("(k p) n -> p k n", p=128))])
                for ct in range(4):
                    m = mg * 4 + ct
                    cs = slice(ct * 128, (ct + 1) * 128)
                    pC = next_ps()
                    for k in range(16):
                        mm(pC.ap, gs3[:, k, cs], hT3[:, k, :], k == 0, k == 15, [gsb, hT], [pC])
                    pD = next_ps()
                    for k in range(16):
                        mm(pD.ap, gl3[:, k, cs], hT3[:, k, :], k == 0, k == 15, [glb, hT], [pD])
                    pA = next_ps()
                    for k in range(4):
                        mm(pA.ap, pr3[:, k, cs], os53[:, k, :], k == 0, k == 3, [prb, out_s5], [pA])
                    pB = next_ps()
                    for k in range(12):
                        mm(pB.ap, pr3[:, 4 + k, cs], ol3[:, k, :], k == 0, k == 11, [prb, out_lru], [pB])
                    act(g1.ap, pC.ap, AF.Sigmoid, [pC, bgt], [g1], bias=bgt.ap[:, m:m + 1])
                    act(g2b.ap, pD.ap, AF.Sigmoid, [pD, bgt], [g2b], bias=bgt.ap[:, 16 + m:17 + m])
                    tt(g1.ap, g1.ap, pA.ap, ALU.mult, [g1, pA], [g1])
                    tt(g2b.ap, g2b.ap, pB.ap, ALU.mult, [g2b, pB], [g2b])
                    tt(mg3[:, m, :], g1.ap, g2b.ap, ALU.add, [g1, g2b], [mergedT])
            for n in range(4):
                wob, wo3 = unit(12 + n, [(slice(0, 16), w_out[:, 512 * n:512 * (n + 1)].rearrange("(k p) n -> p k n", p=128))])
                for it in range(4):
                    po = next_ps()
                    for k in range(16):
                        mm(po.ap, mg3[:, k, it * 128:(it + 1) * 128], wo3[:, k, :], k == 0, k == 15, [mergedT, wob], [po])
                    xb = xr[(n * 4 + it) % 2]
                    rows = slice(t0_ + it * 128, t0_ + (it + 1) * 128)
                    load(xb, xb.ap, x[rows, 512 * n:512 * (n + 1)])
                    tt(xb.ap, xb.ap, po.ap, ALU.add, [xb, po], [xb])
                    R.dma("sp", x1s[rows, 512 * n:512 * (n + 1)], xb.ap, [xb], [], xb)

        R.barrier()
        print("own-pass arena hi", A.hi)
        A.off = 0
        ident_b2 = new("ident2", A.bf16(128))
        load(ident_b2, ident_b2.ap, ident)
        ident_b = ident_b2
        nfwb = new("nfwb", A.f32(D))
        load(nfwb, nfwb.ap, nfw.partition_broadcast(128))
        nfinb = new("nfinb", A.f32(D))
        load(nfinb, nfinb.ap, nfin.partition_broadcast(128))
        wr = new("wr", A.bf16(16 * 36))
        wr3 = wr.ap.rearrange("p (k n) -> p k n", k=16)
        R.dma("pool", wr3[:, :, 0:4], wrg.rearrange("(k p) n -> p k n", p=128), [], [wr], wr, slow=True)
        R.dma("pool", wr3[:, :, 4:36], wre.rearrange("(k p) n -> p k n", p=128), [], [wr], wr, slow=True)
        brb = new("brb", A.f32(36))
        load(brb, brb.ap[:, 0:4], brg.partition_broadcast(128))
        load(brb, brb.ap[:, 4:36], bre.partition_broadcast(128))
        HT = 1024
        acc = [new("acc%d" % i, A.f32(D)) for i in range(8)]
        h2T = new("h2T", A.bf16(16 * HT))
        h2T3 = h2T.ap.rearrange("p (k t) -> p k t", k=16)
        comb = new("comb", A.f32(8 * 32))
        comb3 = comb.ap.rearrange("p (i e) -> p i e", i=8)
        Wgu = [new("Wgu%d" % s, A.bf16(16 * 512)) for s in range(2)]
        Wgu3 = [w.ap.rearrange("p (k n) -> p k n", k=16) for w in Wgu]
        Wd = [new("Wd%d" % s, A.bf16(2 * D)) for s in range(2)]
        Wd3 = [w.ap.rearrange("p (f n) -> p f n", f=2) for w in Wd]
        junk2 = new("junk2", A.bf16(D))
        hb2 = new("hb2", A.bf16(D))
        ss2 = new("ss2", A.f32(2))
        sg = [new("sg%d" % s, A.f32(256)) for s in range(2)]
        actb = [new("actb%d" % s, A.bf16(256)) for s in range(2)]
        actT = [new("actT%d" % s, A.bf16(256)) for s in range(2)]
        rt = new("rt", A.f32(36 + 8 * 6 + 16))
        lg = rt.ap[:, 0:36]
        oh = rt.ap[:, 36:40]
        ge = rt.ap[:, 40:44]
        esel = rt.ap[:, 44:52]
        m1k = rt.ap[:, 52:60]
        m2k = rt.ap[:, 60:68]
        es2 = rt.ap[:, 68:76]
        cm8 = rt.ap[:, 76:84]
        sc = rt.ap[:, 84:100]
        ot = new("ot", A.f32(D))

        for hp in range(NT // HT):
            for it in range(8):
                rows = slice(hp * HT + it * 128, hp * HT + (it + 1) * 128)
                R.dma("sp", acc[it].ap, x1s[rows, :], [], [acc[it]], acc[it])
                a_ = acc[it]
                act(junk2.ap, a_.ap, AF.Square, [a_], [junk2, ss2], accum=ss2.ap[:, 0:1])
                ts(ss2.ap[:, 1:2], ss2.ap[:, 0:1], 1.0 / D, EPS, ALU.mult, ALU.add, [ss2], [ss2])
                act(ss2.ap[:, 1:2], ss2.ap[:, 1:2], AF.Sqrt, [ss2], [ss2])
                R.op("dve", lambda e: e.reciprocal(out=ss2.ap[:, 1:2], in_=ss2.ap[:, 1:2]), [ss2], [ss2])
                stt(hb2.ap, a_.ap, ss2.ap[:, 1:2], nfwb.ap, ALU.mult, ALU.mult, [a_, ss2, nfwb], [hb2])
                transpose_rows(hb2, h2T, h2T3, it * 128)
                pr_ = next_ps()
                for k in range(16):
                    mm(pr_.ap[:, 0:36], h2T3[:, k, it * 128:(it + 1) * 128], wr3[:, k, :], k == 0, k == 15, [h2T, wr], [pr_])
                RT = [rt]
                tt(lg, pr_.ap[:, 0:36], brb.ap, ALU.add, [pr_, brb], RT)
                gm, ngm, gs_, gtop, m1, m2, dl, ee, w1, w2, c1, c2 = [sc[:, k:k + 1] for k in range(12)]
                R.op("dve", lambda e, o=gm, i_=lg[:, 0:4]: e.tensor_reduce(out=o, in_=i_, axis=AX.X, op=ALU.max), RT, RT)
                ts(ngm, gm, -1.0, None, ALU.mult, None, RT, RT)
                act(ge, lg[:, 0:4], AF.Exp, RT, RT, bias=ngm, accum=gs_)
                R.op("dve", lambda e, o=gtop, i_=gs_: e.reciprocal(out=o, in_=i_), RT, RT)
                ts(oh, lg[:, 0:4], gm, None, ALU.is_equal, None, RT, RT)
                ts(esel, lg[:, 4:12], oh[:, 0:1], None, ALU.mult, None, RT, RT)
                for g in range(1, 4):
                    stt(esel, lg[:, 4 + 8 * g:12 + 8 * g], oh[:, g:g + 1], esel, ALU.mult, ALU.add, RT, RT)
                R.op("dve", lambda e, o=m1, i_=esel: e.tensor_reduce(out=o, in_=i_, axis=AX.X, op=ALU.max), RT, RT)
                ts(m1k, esel, m1, None, ALU.is_equal, None, RT, RT)
                stt(es2, m1k, -1e30, esel, ALU.mult, ALU.add, RT, RT)
                R.op("dve", lambda e, o=m2, i_=es2: e.tensor_reduce(out=o, in_=i_, axis=AX.X, op=ALU.max), RT, RT)
                ts(m2k, es2, m2, None, ALU.is_equal, None, RT, RT)
                tt(dl, m2, m1, ALU.subtract, RT, RT)
                act(ee, dl, AF.Exp, RT, RT)
                ts(w1, ee, 1.0, None, ALU.add, None, RT, RT)
                R.op("dve", lambda e, o=w1, i_=w1: e.reciprocal(out=o, in_=i_), RT, RT)
                tt(w2, ee, w1, ALU.mult, RT, RT)
                tt(c1, w1, gtop, ALU.mult, RT, RT)
                tt(c2, w2, gtop, ALU.mult, RT, RT)
                ts(cm8, m1k, c1, None, ALU.mult, None, RT, RT)
                stt(cm8, m2k, c2, cm8, ALU.mult, ALU.add, RT, RT)
                for g in range(4):
                    ts(comb3[:, it, 8 * g:8 * g + 8], cm8, oh[:, g:g + 1], None, ALU.mult, None, RT, [comb])
            seq = [(e, it) for e in range(32) for it in range(8)]
            gu_ps = {}

            def load_expert(e):
                s = e % 2
                R.dma("pool", Wgu3[s][:, :, 0:256], weg[e].rearrange("(k p) n -> p k n", p=128), [], [Wgu[s]], Wgu[s])
                R.dma("pool", Wgu3[s][:, :, 256:512], weu[e].rearrange("(k p) n -> p k n", p=128), [], [Wgu[s]], Wgu[s])
                R.dma("pool", Wd3[s], wed[e].rearrange("(f p) n -> p f n", p=128), [], [Wd[s]], Wd[s])

            def emit_gu(idx):
                e, it = seq[idx]
                s = e % 2
                pg = ps[idx % 2]
                for k in range(16):
                    mm(pg.ap, h2T3[:, k, it * 128:(it + 1) * 128], Wgu3[s][:, k, :], k == 0, k == 15, [h2T, Wgu[s]], [pg])
                gu_ps[idx] = pg

            load_expert(0)
            emit_gu(0)
            for idx in range(len(seq)):
                e, it = seq[idx]
                if it == 0 and e + 1 < 32:
                    load_expert(e + 1)
                if idx + 1 < len(seq):
                    emit_gu(idx + 1)
                s = e % 2
                b2 = idx % 2
                pg = gu_ps.pop(idx)
                act(sg[b2].ap, pg.ap[:, 0:256], AF.Silu, [pg], [sg[b2]])
                stt(actb[b2].ap, sg[b2].ap, comb3[:, it, e:e + 1], pg.ap[:, 256:512], ALU.mult, ALU.mult,
                    [sg[b2], comb, pg], [actb[b2]])
                pt_ = ps[2 + b2]
                ptv = pt_.ap.bitcast(BF16)[:, 0:256].rearrange("p (f t) -> p f t", f=2)
                for f in range(2):
                    tr(ptv[:, f, :], actb[b2].ap[:, f * 128:(f + 1) * 128], [actb[b2], ident_b], [pt_])
                aT3 = actT[b2].ap.rearrange("p (f t) -> p f t", f=2)
                R.op("act", lambda e_, o=aT3, i_=ptv: e_.activation(out=o, in_=i_, func=AF.Copy), [pt_], [actT[b2]])
                for n in range(4):
                    pd = ps[4 + n]
                    for f in range(2):
                        mm(pd.ap, aT3[:, f, :], Wd3[s][:, f, n * 512:(n + 1) * 512], f == 0, f == 1, [actT[b2], Wd[s]], [pd])
                    tt(acc[it].ap[:, n * 512:(n + 1) * 512], acc[it].ap[:, n * 512:(n + 1) * 512], pd.ap, ALU.add,
                       [acc[it], pd], [acc[it]])
            for it in range(8):
                rows = slice(hp * HT + it * 128, hp * HT + (it + 1) * 128)
                a_ = acc[it]
                act(junk2.ap, a_.ap, AF.Square, [a_], [junk2, ss2], accum=ss2.ap[:, 0:1])
                ts(ss2.ap[:, 1:2], ss2.ap[:, 0:1], 1.0 / D, EPS, ALU.mult, ALU.add, [ss2], [ss2])
                act(ss2.ap[:, 1:2], ss2.ap[:, 1:2], AF.Sqrt, [ss2], [ss2])
                R.op("dve", lambda e: e.reciprocal(out=ss2.ap[:, 1:2], in_=ss2.ap[:, 1:2]), [ss2], [ss2])
                stt(ot.ap, a_.ap, ss2.ap[:, 1:2], nfinb.ap, ALU.mult, ALU.mult, [a_, ss2, nfinb], [ot])
                R.dma("sp", out[rows, :], ot.ap, [ot], [], ot)
        R.op("sp", None, [ot], [ot])
        R.emit(nc, es)
    return nc


_CACHE = {}


def _get(mode="fused"):
    if mode not in _CACHE:
        _CACHE[mode] = build(mode)
    return _CACHE[mode]


def kernel(**inp):
    f32 = np.float32
    g = {k: np.ascontiguousarray(np.asarray(v), dtype=f32) for k, v in inp.items()}
    x = g["x"].reshape(NCORE * NT, D)
    ident = np.eye(128, dtype=f32).astype(ml_dtypes.bfloat16)
    ramp = np.broadcast_to(np.arange(1, L + 1, dtype=f32)[None, :], (128, L)).copy()
    common = {
        "nmw": g["norm_mix_w"][0], "w_in": g["w_in"][0],
        "lam_re": g["s5_lam_re"][0], "lam_im": g["s5_lam_im"][0], "lstep": g["s5_log_step"][0],
        "b_re": g["s5_b_re"][0], "b_im": g["s5_b_im"][0],
        "conv_w": g["lru_conv_w"][0], "conv_b": g["lru_conv_b"][0],
        "w_a": g["lru_w_a"][0], "b_a": g["lru_b_a"][0], "w_x": g["lru_w_x"][0], "b_x": g["lru_b_x"][0],
        "lru_lam": g["lru_lambda"][0], "ident": ident, "ramp": ramp,
        "revc": (127.0 - np.arange(128, dtype=f32)).reshape(128, 1),
        "b_gate": g["b_gate"][0], "c_re": g["s5_c_re"][0], "c_im": g["s5_c_im"][0],
        "s5_d": g["s5_d"][0], "w_glu": g["s5_w_glu"][0], "b_glu": g["s5_b_glu"][0],
        "wps": g["w_proj_s5"][0], "wpl": g["w_proj_lru"][0], "w_out": g["w_out"][0],
        "nfw": g["norm_ffn_w"][0], "wrg": g["w_router_group"][0], "brg": g["b_router_group"][0],
        "wre": g["w_router_expert"][0], "bre": g["b_router_expert"][0],
        "weg": g["w_e_gate"][0], "weu": g["w_e_up"][0], "wed": g["w_e_down"][0], "nfin": g["norm_final_w"],
    }
    in_maps = []
    for c in range(NCORE):
        xp = np.zeros((NPB * TB, D), f32)
        if c > 0:
            xp[NPB * TB - c * NT:] = x[:c * NT]
        first_real = NPB - 4 * c
        keep = np.zeros((128, NPB), f32)
        keep[:, first_real:] = 1.0
        flag = np.zeros((128, NPB + 1), f32)
        flag[:, first_real] = 1.0
        in_maps.append(dict(common, x=x[c * NT:(c + 1) * NT], xp=xp, keepv=keep, flagv=flag))
    res = run_bass_kernel_spmd(_get(), in_maps, core_ids=list(range(NCORE)))
    outs = [np.asarray(r["out"], dtype=f32) for r in res.results]
    return np.concatenate(outs, axis=0).reshape(1, NCORE * NT, D)
```
